# Optimizing a Trainium2 kernel written in Bass

```python
import jax, jax.numpy as jnp
from jax import lax
import numpy as np

D_MODEL = 1024
BATCH = 32
SEQ = 2048
DEPTH = 4

GRID_W = 64
CTX_LEN = 256
HEAD_DIM = 64
GROUP_W = D_MODEL // 4
N_HEADS = GROUP_W // HEAD_DIM
N_KV_HEADS = N_HEADS // 2
GQ = N_HEADS // N_KV_HEADS
KV_W = N_KV_HEADS * HEAD_DIM
IN_W = 5 * GROUP_W + 2 * (GROUP_W + 2 * KV_W)
SHORT_CONV = 3
CONF_CONV = 31
WINDOW = 128
Q_BLOCK = 128
N_EXPERTS = 16
EXPERT_FF = D_MODEL
CAPACITY_FACTOR = 2
ROPE_THETA = 10000.0
EPS = 1e-6

kernel_name = "hybrid_headgroup_ec_moe_diffusion_trunk"


def rms_norm(x, g):
    xf = x.astype(jnp.float32)
    y = xf * lax.rsqrt(jnp.mean(xf * xf, axis=-1, keepdims=True) + EPS)
    return (y * g.astype(jnp.float32)).astype(x.dtype)


def layer_norm(x, g, b):
    xf = x.astype(jnp.float32)
    mu = jnp.mean(xf, axis=-1, keepdims=True)
    var = jnp.mean(jnp.square(xf - mu), axis=-1, keepdims=True)
    y = (xf - mu) * lax.rsqrt(var + EPS)
    return (y * g.astype(jnp.float32) + b.astype(jnp.float32)).astype(x.dtype)


def dwconv(x, w):
    k = w.shape[0]
    return lax.conv_general_dilated(x, w[:, None, :].astype(x.dtype), (1,), [(k // 2, k // 2)],
                                    dimension_numbers=('NWC', 'WIO', 'NWC'),
                                    feature_group_count=x.shape[-1])


def split_proj(p):
    sizes = (GROUP_W,) * 5 + (GROUP_W, KV_W, KV_W) * 2
    return jnp.split(p, np.cumsum(sizes)[:-1].tolist(), axis=-1)


def q_heads(t):
    return t.reshape(*t.shape[:-1], N_KV_HEADS, GQ, HEAD_DIM)


def kv_heads(t):
    return t.reshape(*t.shape[:-1], N_KV_HEADS, HEAD_DIM)


def short_conv_mixer(b, c, h, w):
    return b * dwconv(c * h, w)


def conformer_conv(a, g, w, bias, ln_g, ln_b):
    u = a * jax.nn.sigmoid(g)
    u = dwconv(u, w) + bias
    return jax.nn.silu(layer_norm(u, ln_g, ln_b))


def rope_tables(s):
    rows = s // GRID_W
    row = jnp.repeat(jnp.arange(rows), GRID_W).astype(jnp.float32)
    col = (jnp.arange(rows * GRID_W) % GRID_W).astype(jnp.float32)
    quarter = HEAD_DIM // 4
    inv = ROPE_THETA ** (-jnp.arange(quarter, dtype=jnp.float32) / quarter)
    ar = row[:, None] * inv
    ac = col[:, None] * inv
    ang = jnp.concatenate([ar, ar, ac, ac], axis=-1)
    return jnp.cos(ang), jnp.sin(ang)


def apply_rope(x, cos, sin):
    bshape = (cos.shape[0],) + (1,) * (x.ndim - 3) + (HEAD_DIM,)
    c = cos.reshape(bshape).astype(x.dtype)
    s = sin.reshape(bshape).astype(x.dtype)
    xs = x.reshape(*x.shape[:-1], 2, 2, HEAD_DIM // 4)
    rot = jnp.stack([-xs[..., 1, :], xs[..., 0, :]], axis=-2).reshape(x.shape)
    return x * c + rot * s


def gqa_attend(q, k, v, sink=None):
    s = jnp.einsum('bqkgd,bjkd->bkgqj', q, k).astype(jnp.float32) * (HEAD_DIM ** -0.5)
    if sink is not None:
        sl = jnp.broadcast_to(sink.reshape(N_KV_HEADS, GQ)[None, :, :, None, None].astype(jnp.float32),
                              s.shape[:-1] + (1,))
        s = jnp.concatenate([s, sl], axis=-1)
    p = jax.nn.softmax(s, axis=-1)
    if sink is not None:
        p = p[..., :-1]
    return jnp.einsum('bkgqj,bjkd->bqkgd', p.astype(v.dtype), v)


def window_attention(q, k, v, kc, vc, sink):
    bsz, s = q.shape[:2]
    nb = s // Q_BLOCK
    qb = q.reshape(bsz, nb, Q_BLOCK, N_KV_HEADS, GQ, HEAD_DIM)

    def bands(t):
        tp = jnp.pad(t, ((0, 0), (Q_BLOCK, Q_BLOCK), (0, 0), (0, 0)))
        tp = tp.reshape(bsz, nb + 2, Q_BLOCK, N_KV_HEADS, HEAD_DIM)
        return jnp.concatenate([tp[:, :-2], tp[:, 1:-1], tp[:, 2:]], axis=2)

    kw, vw = bands(k), bands(v)
    scale = HEAD_DIM ** -0.5
    s_loc = jnp.einsum('bnqkgd,bnjkd->bnkgqj', qb, kw).astype(jnp.float32) * scale
    s_ctx = jnp.einsum('bnqkgd,bckd->bnkgqc', qb, kc).astype(jnp.float32) * scale
    blk = jnp.arange(nb)[:, None, None] * Q_BLOCK
    qpos = blk + jnp.arange(Q_BLOCK)[None, :, None]
    kpos = blk - Q_BLOCK + jnp.arange(3 * Q_BLOCK)[None, None, :]
    valid = (kpos >= 0) & (kpos < s) & (jnp.abs(qpos - kpos) <= WINDOW)
    s_loc = jnp.where(valid[None, :, None, None], s_loc, -jnp.inf)
    sl = jnp.broadcast_to(sink.reshape(N_KV_HEADS, GQ)[None, None, :, :, None, None].astype(jnp.float32),
                          s_loc.shape[:-1] + (1,))
    p = jax.nn.softmax(jnp.concatenate([s_loc, s_ctx, sl], axis=-1), axis=-1)
    n_loc = 3 * Q_BLOCK
    p_loc = p[..., :n_loc].astype(v.dtype)
    p_ctx = p[..., n_loc:n_loc + kc.shape[1]].astype(v.dtype)
    o = (jnp.einsum('bnkgqj,bnjkd->bnqkgd', p_loc, vw)
         + jnp.einsum('bnkgqc,bckd->bnqkgd', p_ctx, vc))
    return o.reshape(bsz, s, GROUP_W)


def global_attention(q, k, v, kc, vc):
    bsz, s = q.shape[:2]
    nb = s // Q_BLOCK
    kall = jnp.concatenate([k, kc], axis=1)
    vall = jnp.concatenate([v, vc], axis=1)
    qb = jnp.moveaxis(q.reshape(bsz, nb, Q_BLOCK, N_KV_HEADS, GQ, HEAD_DIM), 1, 0)
    o = lax.map(lambda qblk: gqa_attend(qblk, kall, vall), qb)
    return jnp.moveaxis(o, 0, 1).reshape(bsz, s, GROUP_W)


def expert_choice_ffn(h, router, w_gate, w_up, w_down):
    bsz, t, d = h.shape
    cap = CAPACITY_FACTOR * t // N_EXPERTS
    aff = jax.nn.softmax((h @ router).astype(jnp.float32), axis=-1)
    gates, idx = lax.top_k(jnp.swapaxes(aff, 1, 2), cap)
    xs = jax.vmap(lambda hb, ib: hb[ib])(h, idx)
    a = jnp.einsum('becd,edf->becf', xs, w_gate)
    u = jnp.einsum('becd,edf->becf', xs, w_up)
    y = jnp.einsum('becf,efd->becd', jax.nn.silu(a) * u, w_down)
    y = y * gates[..., None].astype(y.dtype)
    return jax.vmap(lambda ib, yb: jnp.zeros((t, d), yb.dtype).at[ib.reshape(-1)].add(yb.reshape(-1, d)))(idx, y)


def layer(x, xc, mods, cmods, n1, n2, w_in, conv_a, conv_b, conv_b_bias, cln_g, cln_b, sink,
          qn_g, kn_g, w_out, router, w_gate, w_up, w_down, cos, sin, need_ctx):
    sh1, sc1, g1, sh2, sc2, g2 = [m[:, None] for m in jnp.split(mods, 6, axis=-1)]
    csh1, csc1, cg1, csh2, csc2, cg2 = [m[:, None] for m in jnp.split(cmods, 6, axis=-1)]
    bsz, s = x.shape[:2]
    lc = xc.shape[1]

    h = rms_norm(x, n1) * (1 + sc1) + sh1
    hc = rms_norm(xc, n1) * (1 + csc1) + csh1
    a_b, a_c, a_h, b_v, b_g, c_q, c_k, c_v, d_q, d_k, d_v = split_proj(h @ w_in)
    ca_b, ca_c, ca_h, cb_v, cb_g, cc_q, cc_k, cc_v, cd_q, cd_k, cd_v = split_proj(hc @ w_in)

    kc_c, vc_c = kv_heads(cc_k), kv_heads(cc_v)
    kc_d, vc_d = rms_norm(kv_heads(cd_k), kn_g), kv_heads(cd_v)

    ya = short_conv_mixer(a_b, a_c, a_h, conv_a)
    yb = conformer_conv(b_v, b_g, conv_b, conv_b_bias, cln_g, cln_b)
    yc = window_attention(apply_rope(q_heads(c_q), cos, sin), apply_rope(kv_heads(c_k), cos, sin),
                          kv_heads(c_v), kc_c, vc_c, sink)
    yd = global_attention(apply_rope(rms_norm(q_heads(d_q), qn_g), cos, sin),
                          apply_rope(rms_norm(kv_heads(d_k), kn_g), cos, sin),
                          kv_heads(d_v), kc_d, vc_d)
    x_new = x + g1 * (jnp.concatenate([ya, yb, yc, yd], axis=-1) @ w_out)
    h2 = rms_norm(x_new, n2) * (1 + sc2) + sh2
    x_new = x_new + g2 * expert_choice_ffn(h2, router, w_gate, w_up, w_down)

    if need_ctx:
        yca = short_conv_mixer(ca_b, ca_c, ca_h, conv_a)
        ycb = conformer_conv(cb_v, cb_g, conv_b, conv_b_bias, cln_g, cln_b)
        ycc = gqa_attend(q_heads(cc_q), kc_c, vc_c, sink).reshape(bsz, lc, GROUP_W)
        ycd = gqa_attend(rms_norm(q_heads(cd_q), qn_g), kc_d, vc_d).reshape(bsz, lc, GROUP_W)
        xc = xc + cg1 * (jnp.concatenate([yca, ycb, ycc, ycd], axis=-1) @ w_out)
        hc2 = rms_norm(xc, n2) * (1 + csc2) + csh2
        xc = xc + cg2 * expert_choice_ffn(hc2, router, w_gate, w_up, w_down)
    return x_new, xc


def setup_inputs(seed: int = 0) -> dict:
    key = jax.random.key(seed)
    ks = jax.random.split(key, 24)
    f32 = jnp.float32
    nrm = lambda k, shape, scale: jax.random.normal(k, shape, f32) * scale
    d = D_MODEL
    return {
        "x": nrm(ks[0], (BATCH, SEQ, d), 1.0),
        "c": nrm(ks[1], (BATCH, d), 1.0),
        "ctx": nrm(ks[2], (BATCH, CTX_LEN, d), 1.0),
        "c_ctx": nrm(ks[3], (d,), 1.0),
        "ada_w": nrm(ks[4], (DEPTH, d, 6 * d), 0.5 * d ** -0.5),
        "ada_b": nrm(ks[5], (DEPTH, 6 * d), 0.01),
        "norm1_g": 1.0 + nrm(ks[6], (DEPTH, d), 0.01),
        "norm2_g": 1.0 + nrm(ks[7], (DEPTH, d), 0.01),
        "w_in": nrm(ks[8], (DEPTH, d, IN_W), d ** -0.5),
        "conv_a_w": nrm(ks[9], (DEPTH, SHORT_CONV, GROUP_W), SHORT_CONV ** -0.5),
        "conv_b_w": nrm(ks[10], (DEPTH, CONF_CONV, GROUP_W), CONF_CONV ** -0.5),
        "conv_b_b": nrm(ks[11], (DEPTH, GROUP_W), 0.01),
        "conv_ln_g": 1.0 + nrm(ks[12], (DEPTH, GROUP_W), 0.01),
        "conv_ln_b": nrm(ks[13], (DEPTH, GROUP_W), 0.01),
        "sink": nrm(ks[14], (DEPTH, N_HEADS), 1.0),
        "q_norm_g": 1.0 + nrm(ks[15], (DEPTH, HEAD_DIM), 0.01),
        "k_norm_g": 1.0 + nrm(ks[16], (DEPTH, HEAD_DIM), 0.01),
        "w_out": nrm(ks[17], (DEPTH, d, d), d ** -0.5),
        "router_w": nrm(ks[18], (DEPTH, d, N_EXPERTS), d ** -0.5),
        "w_gate": nrm(ks[19], (DEPTH, N_EXPERTS, d, EXPERT_FF), d ** -0.5),
        "w_up": nrm(ks[20], (DEPTH, N_EXPERTS, d, EXPERT_FF), d ** -0.5),
        "w_down": nrm(ks[21], (DEPTH, N_EXPERTS, EXPERT_FF, d), EXPERT_FF ** -0.5),
        "final_g": 1.0 + nrm(ks[22], (d,), 0.01),
    }


def reference(x, c, ctx, c_ctx, ada_w, ada_b, norm1_g, norm2_g, w_in, conv_a_w, conv_b_w, conv_b_b,
              conv_ln_g, conv_ln_b, sink, q_norm_g, k_norm_g, w_out, router_w, w_gate, w_up, w_down,
              final_g):
    cos, sin = rope_tables(x.shape[1])
    sc = jax.nn.silu(c)
    scc = jax.nn.silu(c_ctx)[None]
    xc = ctx
    for i in range(DEPTH):
        mods = sc @ ada_w[i] + ada_b[i]
        cmods = scc @ ada_w[i] + ada_b[i]
        x, xc = layer(x, xc, mods, cmods, norm1_g[i], norm2_g[i], w_in[i], conv_a_w[i], conv_b_w[i],
                      conv_b_b[i], conv_ln_g[i], conv_ln_b[i], sink[i], q_norm_g[i], k_norm_g[i],
                      w_out[i], router_w[i], w_gate[i], w_up[i], w_down[i], cos, sin,
                      i < DEPTH - 1)
    return rms_norm(x, final_g)
```

```python
import numpy as np
import ml_dtypes
from contextlib import ExitStack
import concourse.bass as bass
import concourse.mybir as mybir
from concourse.bass_utils import run_bass_kernel_spmd

F32 = mybir.dt.float32
BF16 = mybir.dt.bfloat16
AF = mybir.ActivationFunctionType
ALU = mybir.AluOpType
AX = mybir.AxisListType

D = 1024
T = 2048
C = 256
NTOK = T + C
NTL = T // 128
NTT = NTOK // 128
NE = 16
CAP = 256
CAPC = 32
IN_W = 2304
EPS = 1e-6
N_CORES = 8
SAME_ENG_SYNC = True


class Buf:
    __slots__ = ("name", "w", "r")

    def __init__(self, name=""):
        self.name = name
        self.w = []
        self.r = {}


class Tl:
    def __init__(self, t, name=""):
        self.t = t
        self.b = Buf(name)

    def __getitem__(self, k):
        return self.t[k]


class Sync:
    W = 30000
    NDMA = 20

    def __init__(self, nc, es):
        self.nc = nc
        self.es = es
        self.engs = {"pe": nc.tensor, "act": nc.scalar, "dve": nc.vector, "pool": nc.gpsimd, "sp": nc.sync}
        self.cnt = {e: 0 for e in self.engs}
        self.sems = {e: [] for e in self.engs}
        self.known = {e: {} for e in self.engs}
        self.semid = {}
        self.dma_sems = [self._newsem("dma%d" % i) for i in range(self.NDMA)]
        self.dma_val = [0] * self.NDMA
        self.dma_i = 0
        self.n_wait_ins = 0
        self.dead = False

    def _newsem(self, name):
        s = self.es.enter_context(self.nc.semaphore(name))
        self.semid[id(s)] = len(self.semid)
        return s

    def _sid(self, s):
        return self.semid[id(s)]

    def _next_tok(self, eng):
        n = self.cnt[eng]
        k = n // self.W
        while len(self.sems[eng]) <= k:
            self.sems[eng].append(self._newsem("%s_m%d" % (eng, len(self.sems[eng]))))
        self.cnt[eng] = n + 1
        return (self.sems[eng][k], n % self.W + 1, eng)

    def _last_tok(self, eng):
        n = self.cnt[eng]
        if n == 0:
            return None
        n -= 1
        return (self.sems[eng][n // self.W], n % self.W + 1, eng)

    def _deps(self, r, w, add=False):
        toks = []
        for b in r:
            toks.extend(b.w)
        for b in w:
            if not add:
                toks.extend(b.w)
            toks.extend(b.r.values())
        return toks

    def _need(self, eng, toks):
        kn = self.known[eng]
        need = {}
        for (s, v, src) in toks:
            if src == eng and (eng == "pe" or not SAME_ENG_SYNC):
                continue
            sid = self._sid(s)
            if kn.get(sid, 0) >= v:
                continue
            if sid not in need or need[sid][1] < v:
                need[sid] = (s, v)
        return list(need.values())

    def _emit_waits(self, eng, waits, ins_fn):
        E = self.engs[eng]
        for (s, v) in waits[:-1]:
            E.wait_ge(s, v)
            self.n_wait_ins += 1
        ins = ins_fn(E)
        if waits:
            s, v = waits[-1]
            ins._wait_ge(s, v)
        kn = self.known[eng]
        for (s, v) in waits:
            sid = self._sid(s)
            if kn.get(sid, 0) < v:
                kn[sid] = v
        return ins

    def op(self, eng, fn, r=(), w=()):
        if self.dead:
            return None
        r = [x.b if isinstance(x, Tl) else x for x in r]
        w = [x.b if isinstance(x, Tl) else x for x in w]
        waits = self._need(eng, self._deps(r, w))
        ins = self._emit_waits(eng, waits, fn)
        tok = self._next_tok(eng)
        ins.then_inc(tok[0], 1)
        for b in r:
            b.r[eng] = tok
        for b in w:
            b.w = [tok]
            b.r = {}
        return ins

    def dma(self, eng, out, in_, r=(), w=(), add=False, **kw):
        if self.dead:
            return None
        r = [x.b if isinstance(x, Tl) else x for x in r]
        w = [x.b if isinstance(x, Tl) else x for x in w]
        toks = self._deps(r, w, add)
        i = self.dma_i
        self.dma_i = (i + 1) % self.NDMA
        s = self.dma_sems[i]
        if self.dma_val[i] > 0:
            toks.append((s, self.dma_val[i], "dma"))
        waits = self._need(eng, toks)
        ins = self._emit_waits(eng, waits, lambda E: E.dma_start(out=out, in_=in_, **kw))
        self.dma_val[i] += 16
        tok = (s, self.dma_val[i], "dma")
        ins.then_inc(s, 16)
        for b in r:
            b.r[("dma", i)] = tok
        for b in w:
            if add:
                b.w.append(tok)
            else:
                b.w = [tok]
                b.r = {}
        return ins

    def barrier(self, engines=None):
        toks = []
        for e in self.engs:
            t = self._last_tok(e)
            if t is not None:
                toks.append(t)
        for i in range(self.NDMA):
            if self.dma_val[i] > 0:
                toks.append((self.dma_sems[i], self.dma_val[i], "dma"))
        for e in (engines or list(self.engs)):
            kn = self.known[e]
            E = self.engs[e]
            for (s, v, src) in toks:
                if src == e:
                    continue
                sid = self._sid(s)
                if kn.get(sid, 0) >= v:
                    continue
                E.wait_ge(s, v)
                self.n_wait_ins += 1
                kn[sid] = v


class Pool:
    def __init__(self, tiles):
        self.tiles = tiles
        self.i = 0

    def next(self):
        t = self.tiles[self.i]
        self.i = (self.i + 1) % len(self.tiles)
        return t


class _Stop(Exception):
    pass


def build_program(NS=4, DEPTH=4, debug=False, stop=None):
    _sref = []

    _cur = [0]

    def chk(tag):
        if stop == tag or stop == "%s@%d" % (tag, _cur[0]):
            _sref[0].dead = True
    nc = bass.Bass("TRN2", target_bir_lowering=False)
    dt = lambda name, shape, dtype, kind="Internal": nc.dram_tensor(name, list(shape), dtype, kind=kind).ap()
    x_in = dt("x", [NS, T, D], F32, "ExternalInput")
    ctx_in = dt("ctx", [NS, C, D], F32, "ExternalInput")
    cT_in = dt("cT", [128, 8, 5], F32, "ExternalInput")
    ada_w = dt("ada_w", [DEPTH, D, 6 * D], F32, "ExternalInput")
    ada_b = dt("ada_b", [DEPTH, 6 * D], F32, "ExternalInput")
    n1rep = dt("n1rep", [DEPTH, 128, 8, 5], F32, "ExternalInput")
    n2rep = dt("n2rep", [DEPTH, 128, 8, 5], F32, "ExternalInput")
    w_in = dt("w_in", [DEPTH, D, IN_W], F32, "ExternalInput")
    convp = dt("convp", [DEPTH, 128, 2, 40], F32, "ExternalInput")
    sink_in = dt("sink", [1, DEPTH * 4], F32, "ExternalInput")
    qng = dt("qng", [DEPTH, 64], F32, "ExternalInput")
    kng = dt("kng", [DEPTH, 64], F32, "ExternalInput")
    w_out = dt("w_out", [DEPTH, D, D], F32, "ExternalInput")
    router = dt("router", [DEPTH, D, NE], F32, "ExternalInput")
    w_gate = dt("w_gate", [DEPTH, NE, D, D], F32, "ExternalInput")
    w_up = dt("w_up", [DEPTH, NE, D, D], F32, "ExternalInput")
    w_down = dt("w_down", [DEPTH, NE, D, D], F32, "ExternalInput")
    final_g = dt("final_g", [1, D], F32, "ExternalInput")
    c_identb = dt("c_identb", [128, 128], BF16, "ExternalInput")
    c_identf = dt("c_identf", [128, 128], F32, "ExternalInput")
    c_masks = dt("c_masks", [128, 2, 128], BF16, "ExternalInput")
    c_iota = dt("c_iota", [128, 258], F32, "ExternalInput")
    c_rope = dt("c_rope", [NTL, 128, 2, 384], F32, "ExternalInput")
    c_sel = dt("c_sel", [16, 16 * 128], F32, "ExternalInput")
    out = dt("out", [NS, T, D], F32, "ExternalOutput")
    dbg = {}
    if debug:
        dbg["xmid"] = dt("d_xmid", [NTT, 128, D], F32, "ExternalOutput")
        dbg["aff"] = dt("d_aff", [128, NTT, 64], F32, "ExternalOutput")
        dbg["yt"] = dt("d_yt", [8, 128, NTOK], BF16, "ExternalOutput")
        dbg["xres"] = dt("d_xres", [NTT, 128, D], F32, "ExternalOutput")
        dbg["slotg"] = dt("d_slotg", [64, 2, NTOK], F32, "ExternalOutput")
        dbg["qT"] = dt("d_qT", [128, 2, NTOK], BF16, "ExternalOutput")
        dbg["kT"] = dt("d_kT", [128, 1, NTOK], BF16, "ExternalOutput")
    MODS = dt("MODS", [DEPTH, 5, 6 * D], F32)
    XMID = dt("XMID", [NS, NTT, 128, D], F32)
    XRES = dt("XRES", [NS, NTT, 128, D], F32)
    XN2 = dt("XN2", [NS, NTT, 128, D], BF16)
    YT = dt("YT", [NS, 8, 128, NTOK], BF16)
    NSL = NS * CAP + NS * CAPC
    XSG = dt("XSG", [NE, 8, 128, NSL], BF16)
    YEX = dt("YEX", [NE, NSL, D], BF16)
    SLOTG = dt("SLOTG", [64, 2, NTOK], F32)

    with ExitStack() as es:
        S = Sync(nc, es)
        _sref.append(S)

        _uid = [0]

        def sb(name, shape, dtype, scope=es):
            _uid[0] += 1
            name = "%s_u%d" % (name, _uid[0])
            return Tl(scope.enter_context(nc.sbuf_tensor(name, list(shape), dtype)), name)

        def dma_k(eng, tile, src2d, ncols=None):
            for k in range(8):
                dst = tile[:, k, :] if ncols is None else tile[:, k, 0:ncols]
                S.dma(eng, dst, src2d[k * 128:(k + 1) * 128, :], w=[tile], add=(k > 0))

        psb = [Tl(es.enter_context(nc.psum_tensor("ps%d" % i, [128, 512], F32)), "ps%d" % i) for i in range(8)]
        PS_MM = Pool(psb[0:4])
        PS_TR = Pool(psb[4:6])
        PS_AC = Pool(psb[6:8])

        def bfv(ps):
            return ps.t.bitcast(BF16)

        identb = sb("identb", [128, 128], BF16)
        identf = sb("identf", [128, 128], F32)
        masks = sb("masks", [128, 2, 128], BF16)
        iota = sb("iota", [128, 258], F32)
        onesf = sb("onesf", [128, 128], F32)
        onesb = sb("onesb", [128, 128], BF16)
        modsT = sb("modsT", [128, DEPTH, 48, 5], F32)
        affTok = sb("affTok", [128, NTT, 64], F32)
        esink = sb("esink", [128, DEPTH * 4], F32)
        S.dma("sp", identb[:], c_identb[:, :], w=[identb])
        S.dma("sp", identf[:], c_identf[:, :], w=[identf])
        S.dma("sp", masks[:], c_masks[:, :, :], w=[masks])
        S.dma("sp", iota[:], c_iota[:, :], w=[iota])
        S.dma("sp", esink[:], sink_in[0:1, :].to_broadcast([128, DEPTH * 4]), w=[esink])
        S.op("dve", lambda e: e.memset(onesf[:], 1.0), w=[onesf])
        S.op("dve", lambda e: e.memset(onesb[:], 1.0), w=[onesb])
        S.op("dve", lambda e: e.memset(affTok[:], 0.0), w=[affTok])
        S.op("act", lambda e: e.activation(out=esink[:], in_=esink[:], func=AF.Exp), r=[esink], w=[esink])

        with ExitStack() as ps0:
            scT = sb("scT", [128, 8, 5], F32, ps0)
            adw = [sb("adw%d" % i, [128, 8, 512], F32, ps0) for i in range(2)]
            adb = [sb("adb%d" % i, [5, 512], F32, ps0) for i in range(2)]
            mrow = [sb("mrow%d" % i, [5, 512], F32, ps0) for i in range(2)]
            S.dma("sp", scT[:], cT_in[:, :, :], w=[scT])
            S.op("act", lambda e: e.activation(out=scT[:], in_=scT[:], func=AF.Silu), r=[scT], w=[scT])
            it = 0
            for l in range(DEPTH):
                for n in range(12):
                    wt, bt, mr = adw[it % 2], adb[it % 2], mrow[it % 2]
                    it += 1
                    dma_k("sp", wt, ada_w[l][:, n * 512:(n + 1) * 512])
                    S.dma("sp", bt[:], ada_b[l:l + 1, n * 512:(n + 1) * 512].to_broadcast([5, 512]), w=[bt])
                    ps = PS_MM.next()
                    for k in range(8):
                        S.op("pe", lambda e, k=k: e.matmul(ps[0:5, :], lhsT=scT[:, k, :], rhs=wt[:, k, :],
                                                          start=(k == 0), stop=(k == 7)), r=[scT, wt], w=[ps])
                    S.op("dve", lambda e: e.tensor_tensor(out=mr[:], in0=ps[0:5, :], in1=bt[:], op=ALU.add),
                         r=[ps, bt], w=[mr])
                    S.dma("sp", MODS[l][:, n * 512:(n + 1) * 512], mr[:], r=[mr])
                    pt = PS_TR.next()
                    for j in range(4):
                        S.op("pe", lambda e, j=j: e.transpose(pt[:, j * 5:(j + 1) * 5], mr[:, j * 128:(j + 1) * 128],
                                                              identf[0:5, 0:5]), r=[mr, identf], w=[pt])
                    S.op("act", lambda e: e.activation(
                        out=modsT[:, l, n * 4:(n + 1) * 4, :],
                        in_=pt[:, 0:20].rearrange("p (j s) -> p j s", s=5), func=AF.Copy), r=[pt], w=[modsT])
        S.barrier()

        try:
            chk("pro")
            def x_src(l, b, i):
                if l == 0:
                    return x_in[b, i * 128:(i + 1) * 128, :] if i < NTL else ctx_in[b, (i - NTL) * 128:(i - NTL + 1) * 128, :]
                return XRES[b, i]

            A1 = sb("A1", [128, 8, 5], F32)
            A2 = sb("A2", [128, 8, 5], F32)
            for l in range(DEPTH):
                _cur[0] = l
                with ExitStack() as p1:
                    YAG = Pool([sb("yag%d" % i, [128, 256], BF16, p1) for i in range(4)])
                    H2T = Pool([sb("h2t%d" % i, [128, 8, 128], BF16, p1) for i in range(2)])
                    YTB = [Buf("ytb%d" % i) for i in range(NS)]
                    nrep = sb("nrep", [128, 2, 8, 5], F32, p1)
                    cvp = sb("cvp", [128, 2, 40], F32, p1)
                    qg = sb("qg", [128, 64], F32, p1)
                    kg = sb("kg", [128, 64], F32, p1)
                    woutb = sb("woutb", [128, 8, D], BF16, p1)
                    rtb = sb("rtb", [128, 8, NE], BF16, p1)
                    hT = sb("hT", [128, 8, NTOK], BF16, p1)
                    xt = [sb("xt%d" % i, [128, D], F32, p1) for i in range(2)]
                    xnb = [sb("xnb%d" % i, [128, D], BF16, p1) for i in range(2)]
                    st = [sb("st%d" % i, [128, 8], F32, p1) for i in range(4)]
                    XT, XNB, ST = Pool(xt), Pool(xnb), Pool(st)
                    wch = [sb("wch%d" % i, [128, 8, 512], BF16, p1) for i in range(2)]
                    WCH = Pool(wch)
                    cw = [sb("cw%d" % i, [128, NTOK + 32], F32, p1) for i in range(3)]
                    upc = sb("upc", [128, C + 32], F32, p1)
                    ystg = [sb("ystg%d" % i, [128, NTOK], BF16, p1) for i in range(2)]
                    YSTG = Pool(ystg)
                    qT = sb("qT", [128, 2, NTOK], BF16, p1)
                    kT = sb("kT", [128, 1, NTOK], BF16, p1)
                    Vt = sb("Vt", [128, NTT, 2, 65], BF16, p1)
                    rope = [sb("rope%d" % i, [128, 2, 384], F32, p1) for i in range(2)]
                    ROPE = Pool(rope)
                    qk = [sb("qk%d" % i, [128, 384], F32, p1) for i in range(2)]
                    QK = Pool(qk)
                    qk2 = [sb("qkb%d" % i, [128, 384], F32, p1) for i in range(2)]
                    QK2 = Pool(qk2)
                    qkr = [sb("qkr%d" % i, [128, 384], BF16, p1) for i in range(2)]
                    QKR = Pool(qkr)
                    ptl = [sb("ptl%d" % i, [128, 640], BF16, p1) for i in range(3)]
                    PTL = Pool(ptl)
                    yat = [sb("yat%d" % i, [128, 256], BF16, p1) for i in range(2)]
                    YAT = Pool(yat)
                    g1b = sb("g1b", [128, D], F32, p1)
                    YTL = WCH
                    tmpf = [sb("tmpf%d" % i, [128, 512], F32, p1) for i in range(6)]
                    TMPF = Pool(tmpf)

                    S.dma("sp", nrep[:, 0], n1rep[l], w=[nrep])
                    S.dma("sp", nrep[:, 1], n2rep[l], w=[nrep])
                    S.dma("sp", cvp[:], convp[l], w=[cvp])
                    S.dma("sp", qg[:], qng[l:l + 1, :].to_broadcast([128, 64]), w=[qg])
                    S.dma("sp", kg[:], kng[l:l + 1, :].to_broadcast([128, 64]), w=[kg])
                    dma_k("pool", woutb, w_out[l])
                    S.dma("pool", rtb[:], router[l].rearrange("(k p) f -> p k f", p=128), w=[rtb])
                    S.op("dve", lambda e: e.scalar_tensor_tensor(out=A1[:], in0=modsT[:, l, 8:16, :], scalar=1.0,
                                                                 in1=nrep[:, 0], op0=ALU.add, op1=ALU.mult),
                         r=[modsT, nrep], w=[A1])
                    S.op("dve", lambda e: e.scalar_tensor_tensor(out=A2[:], in0=modsT[:, l, 32:40, :], scalar=1.0,
                                                                 in1=nrep[:, 1], op0=ALU.add, op1=ALU.mult),
                         r=[modsT, nrep], w=[A2])
                    S.op("dve", lambda e: e.memset(Vt[:], 1.0), w=[Vt])

                    def norm_to_T(src_tile, dstT, dcols, A, Bofs, j, xn_keep=None):
                        ss = ST.next()
                        xn = xn_keep if xn_keep is not None else XNB.next()
                        S.op("pool", lambda e: e.memset(ss[:], 0.0), w=[ss])
                        S.op("act", lambda e: e.activation(out=xn[:], in_=src_tile[:], func=AF.Square,
                                                           accum_out=ss[:, 0:1]), r=[src_tile], w=[xn, ss])
                        S.op("dve", lambda e: e.tensor_scalar(out=ss[:, 1:2], in0=ss[:, 0:1], scalar1=1.0 / D, scalar2=EPS,
                                                              op0=ALU.mult, op1=ALU.add), r=[ss], w=[ss])
                        S.op("dve", lambda e: e.reciprocal(out=ss[:, 2:3], in_=ss[:, 1:2]), r=[ss], w=[ss])
                        S.op("act", lambda e: e.activation(out=ss[:, 3:4], in_=ss[:, 2:3], func=AF.Sqrt), r=[ss], w=[ss])
                        S.op("act", lambda e: e.activation(out=xn[:], in_=src_tile[:], func=AF.Copy, scale=ss[:, 3:4]),
                             r=[src_tile, ss], w=[xn])
                        pt = PS_TR.next()
                        ptb = bfv(pt)
                        for c in range(8):
                            S.op("pe", lambda e, c=c: e.transpose(ptb[:, c * 128:(c + 1) * 128], xn[:, c * 128:(c + 1) * 128],
                                                                  identb[:]), r=[xn, identb], w=[pt])
                        for c in range(8):
                            if c % 2 == 0:
                                S.op("act", lambda e, c=c: e.activation(
                                    out=dstT[:, c, dcols], in_=ptb[:, c * 128:(c + 1) * 128], func=AF.Identity,
                                    scale=A[:, c, j:j + 1], bias=modsT[:, l, Bofs + c, j:j + 1]), r=[pt, A, modsT], w=[dstT])
                            else:
                                S.op("dve", lambda e, c=c: e.tensor_scalar(
                                    out=dstT[:, c, dcols], in0=ptb[:, c * 128:(c + 1) * 128],
                                    scalar1=A[:, c, j:j + 1], scalar2=modsT[:, l, Bofs + c, j:j + 1],
                                    op0=ALU.mult, op1=ALU.add), r=[pt, A, modsT], w=[dstT])
                        return xn

                    TCH = [(0, 512), (512, 512), (1024, 512), (1536, 512), (2048, 256)]
                    SEGS = [(0, T), (T, C)]

                    def colp(t):
                        return 16 + t if t < T else 16 + t + 0

                    for b in range(NS):
                        for i in range(NTT):
                            xtile = XT.next()
                            S.dma("sp", xtile[:], x_src(l, b, i), w=[xtile])
                            norm_to_T(xtile, hT, slice(i * 128, (i + 1) * 128), A1, 0, b if i < NTL else 4)

                        chk("p1_norm")
                        def inproj_fm(fc, consume):
                            wt = WCH.next()
                            dma_k("pool", wt, w_in[l][:, fc * 128:(fc + 1) * 128], 128)
                            for (t0, n) in TCH:
                                ps = PS_MM.next()
                                for k in range(8):
                                    S.op("pe", lambda e, k=k: e.matmul(ps[:, 0:n], lhsT=wt[:, k, 0:128], rhs=hT[:, k, t0:t0 + n],
                                                                      start=(k == 0), stop=(k == 7)), r=[wt, hT], w=[ps])
                                consume(ps, t0, n)

                        for h in range(2):
                            prod, cv = cw[0], cw[1]
                            S.op("pool", lambda e: e.memset(prod[:, 0:1], 0.0), w=[prod])
                            S.op("pool", lambda e: e.memset(prod[:, T + 1:T + 3], 0.0), w=[prod])
                            S.op("pool", lambda e: e.memset(prod[:, T + 3 + C:T + 4 + C], 0.0), w=[prod])

                            def offA(t0):
                                return 1 + t0 if t0 < T else T + 3 + (t0 - T)
                            inproj_fm(2 + h, lambda ps, t0, n: S.op(
                                "act", lambda e: e.activation(out=prod[:, offA(t0):offA(t0) + n], in_=ps[:, 0:n], func=AF.Copy),
                                r=[ps], w=[prod]))
                            inproj_fm(4 + h, lambda ps, t0, n: S.op(
                                "dve", lambda e: e.tensor_tensor(out=prod[:, offA(t0):offA(t0) + n], in0=ps[:, 0:n],
                                                                 in1=prod[:, offA(t0):offA(t0) + n], op=ALU.mult), r=[ps, prod], w=[prod]))
                            for (s0, sn) in SEGS:
                                o0 = offA(s0)
                                S.op("dve", lambda e: e.tensor_scalar(out=cv[:, s0:s0 + sn], in0=prod[:, o0 - 1:o0 - 1 + sn],
                                                                      scalar1=cvp[:, h, 0:1], scalar2=None, op0=ALU.mult),
                                     r=[prod, cvp], w=[cv])
                                for kk in (1, 2):
                                    S.op("dve", lambda e, kk=kk: e.scalar_tensor_tensor(
                                        out=cv[:, s0:s0 + sn], in0=prod[:, o0 - 1 + kk:o0 - 1 + kk + sn], scalar=cvp[:, h, kk:kk + 1],
                                        in1=cv[:, s0:s0 + sn], op0=ALU.mult, op1=ALU.add), r=[prod, cvp, cv], w=[cv])
                            ys = YSTG.next()
                            inproj_fm(0 + h, lambda ps, t0, n: S.op(
                                "dve", lambda e: e.tensor_tensor(out=ys[:, t0:t0 + n], in0=ps[:, 0:n], in1=cv[:, t0:t0 + n],
                                                                 op=ALU.mult), r=[ps, cv], w=[ys]))
                            S.dma("sp", YT[b, h], ys[:], r=[ys], w=[YTB[b]], add=True)
                        chk("p1_A")
                        ucs = [cw[1], cw[2]]
                        for h in range(2):
                            upad, uc = cw[0], ucs[h]
                            S.op("pool", lambda e: e.memset(upad[:, 0:15], 0.0), w=[upad])
                            S.op("pool", lambda e: e.memset(upad[:, 15 + T:30 + T], 0.0), w=[upad])
                            S.op("pool", lambda e: e.memset(upc[:, 0:15], 0.0), w=[upc])
                            S.op("pool", lambda e: e.memset(upc[:, 15 + C:30 + C], 0.0), w=[upc])

                            def dstB(t0, n):
                                return (upad, upad[:, 15 + t0:15 + t0 + n]) if t0 < T else (upc, upc[:, 15:15 + n])

                            def consB1(ps, t0, n):
                                tl, ap = dstB(t0, n)
                                S.op("act", lambda e: e.activation(out=ap, in_=ps[:, 0:n], func=AF.Sigmoid), r=[ps], w=[tl])

                            def consB2(ps, t0, n):
                                tl, ap = dstB(t0, n)
                                S.op("dve", lambda e: e.tensor_tensor(out=ap, in0=ps[:, 0:n], in1=ap, op=ALU.mult), r=[ps, tl], w=[tl])
                            inproj_fm(8 + h, consB1)
                            inproj_fm(6 + h, consB2)
                            for (src, s0, sn, eng) in ((upad, 0, T, "dve"), (upc, T, C, "dve")):
                                S.op(eng, lambda e: e.tensor_scalar(out=uc[:, s0:s0 + sn], in0=src[:, 0:sn],
                                                                    scalar1=cvp[:, h, 3:4], scalar2=cvp[:, h, 34:35],
                                                                    op0=ALU.mult, op1=ALU.add), r=[src, cvp], w=[uc])
                                for kk in range(1, 31):
                                    S.op(eng, lambda e, kk=kk: e.scalar_tensor_tensor(
                                        out=uc[:, s0:s0 + sn], in0=src[:, kk:kk + sn], scalar=cvp[:, h, 3 + kk:4 + kk],
                                        in1=uc[:, s0:s0 + sn], op0=ALU.mult, op1=ALU.add), r=[src, cvp, uc], w=[uc])
                        ysb = [YSTG.next(), YSTG.next()]
                        for (t0, n) in TCH:
                            p1s, p2s = PS_MM.next(), PS_MM.next()
                            for h in range(2):
                                S.op("pe", lambda e, h=h: e.matmul(p1s[:, 0:n], lhsT=onesf[:], rhs=ucs[h][:, t0:t0 + n],
                                                                  start=(h == 0), stop=(h == 1)), r=[onesf, ucs[h]], w=[p1s])
                            for h in range(2):
                                sqt = TMPF.next()
                                S.op("act", lambda e, h=h: e.activation(out=sqt[:, 0:n], in_=ucs[h][:, t0:t0 + n], func=AF.Square),
                                     r=[ucs[h]], w=[sqt])
                                S.op("pe", lambda e, h=h: e.matmul(p2s[:, 0:n], lhsT=onesf[:], rhs=sqt[:, 0:n],
                                                                  start=(h == 0), stop=(h == 1)), r=[onesf, sqt], w=[p2s])
                            mean, var, dd = TMPF.next(), TMPF.next(), TMPF.next()
                            S.op("act", lambda e: e.activation(out=mean[:, 0:n], in_=p1s[:, 0:n], func=AF.Copy, scale=1.0 / 256),
                                 r=[p1s], w=[mean])
                            S.op("dve", lambda e: e.tensor_tensor(out=var[:, 0:n], in0=mean[:, 0:n], in1=mean[:, 0:n], op=ALU.mult),
                                 r=[mean], w=[var])
                            S.op("dve", lambda e: e.scalar_tensor_tensor(out=var[:, 0:n], in0=p2s[:, 0:n], scalar=1.0 / 256,
                                                                         in1=var[:, 0:n], op0=ALU.mult, op1=ALU.subtract),
                                 r=[p2s, var], w=[var])
                            S.op("dve", lambda e: e.tensor_scalar(out=var[:, 0:n], in0=var[:, 0:n], scalar1=EPS, scalar2=None,
                                                                  op0=ALU.add), r=[var], w=[var])
                            S.op("dve", lambda e: e.reciprocal(out=var[:, 0:n], in_=var[:, 0:n]), r=[var], w=[var])
                            S.op("act", lambda e: e.activation(out=var[:, 0:n], in_=var[:, 0:n], func=AF.Sqrt), r=[var], w=[var])
                            for h in range(2):
                                S.op("pool", lambda e, h=h: e.tensor_tensor(out=dd[:, 0:n], in0=ucs[h][:, t0:t0 + n], in1=mean[:, 0:n],
                                                                            op=ALU.subtract), r=[ucs[h], mean], w=[dd])
                                S.op("pool", lambda e: e.tensor_tensor(out=dd[:, 0:n], in0=dd[:, 0:n], in1=var[:, 0:n], op=ALU.mult),
                                     r=[dd, var], w=[dd])
                                S.op("act", lambda e, h=h: e.activation(out=ysb[h][:, t0:t0 + n], in_=dd[:, 0:n], func=AF.Silu,
                                                                        scale=cvp[:, h, 35:36], bias=cvp[:, h, 36:37]),
                                     r=[dd, cvp], w=[ysb[h]])
                        for h in range(2):
                            S.dma("sp", YT[b, 2 + h], ysb[h][:], r=[ysb[h]], w=[YTB[b]], add=True)

                        chk("p1_B")
                        for grp in range(2):
                            wt = WCH.next()
                            c0 = 1280 + grp * 512
                            dma_k("pool", wt, w_in[l][:, c0:c0 + 512])
                            for i in range(NTT):
                                ps = PS_MM.next()
                                for k in range(8):
                                    S.op("pe", lambda e, k=k: e.matmul(ps[:, :], lhsT=hT[:, k, i * 128:(i + 1) * 128], rhs=wt[:, k, :],
                                                                      start=(k == 0), stop=(k == 7)), r=[hT, wt], w=[ps])
                                S.op("act", lambda e: e.activation(out=Vt[:, i, :, 0:64],
                                                                   in_=ps[:, 384:512].rearrange("p (a d) -> p a d", d=64),
                                                                   func=AF.Copy), r=[ps], w=[Vt])
                                q1 = QK.next()
                                S.op("act", lambda e: e.activation(out=q1[:], in_=ps[:, 0:384], func=AF.Copy), r=[ps], w=[q1])
                                if grp == 1:
                                    sq = QK2.next()
                                    ss = ST.next()
                                    S.op("dve", lambda e: e.tensor_tensor(out=sq[:], in0=q1[:], in1=q1[:], op=ALU.mult), r=[q1], w=[sq])
                                    S.op("dve", lambda e: e.tensor_reduce(out=ss[:, 0:6], in_=sq[:].rearrange("p (a d) -> p a d", d=64),
                                                                          axis=AX.X, op=ALU.add), r=[sq], w=[ss])
                                    S.op("dve", lambda e: e.tensor_scalar(out=ss[:, 0:6], in0=ss[:, 0:6], scalar1=1.0 / 64, scalar2=EPS,
                                                                          op0=ALU.mult, op1=ALU.add), r=[ss], w=[ss])
                                    S.op("dve", lambda e: e.reciprocal(out=ss[:, 0:6], in_=ss[:, 0:6]), r=[ss], w=[ss])
                                    S.op("act", lambda e: e.activation(out=ss[:, 0:6], in_=ss[:, 0:6], func=AF.Sqrt), r=[ss], w=[ss])
                                    for hh in range(6):
                                        gt = qg if hh < 4 else kg
                                        S.op("dve", lambda e, hh=hh, gt=gt: e.scalar_tensor_tensor(
                                            out=q1[:, hh * 64:(hh + 1) * 64], in0=q1[:, hh * 64:(hh + 1) * 64], scalar=ss[:, hh:hh + 1],
                                            in1=gt[:], op0=ALU.mult, op1=ALU.mult), r=[q1, ss, gt], w=[q1])
                                qr = QKR.next()
                                if i < NTL:
                                    rp = ROPE.next()
                                    S.dma("sp", rp[:], c_rope[i], w=[rp])
                                    t1, t2 = QK2.next(), QK2.next()
                                    v4 = lambda ap: ap.rearrange("p (a j q) -> p a j q", j=2, q=16)
                                    S.op("dve", lambda e: e.tensor_tensor(out=t1[:], in0=q1[:], in1=rp[:, 0, :], op=ALU.mult),
                                         r=[q1, rp], w=[t1])
                                    S.op("pool", lambda e: e.tensor_tensor(out=v4(t2[:])[:, :, 0, :], in0=v4(q1[:])[:, :, 1, :],
                                                                           in1=v4(rp[:, 1, :])[:, :, 0, :], op=ALU.mult), r=[q1, rp], w=[t2])
                                    S.op("pool", lambda e: e.tensor_tensor(out=v4(t2[:])[:, :, 1, :], in0=v4(q1[:])[:, :, 0, :],
                                                                           in1=v4(rp[:, 1, :])[:, :, 1, :], op=ALU.mult), r=[q1, rp], w=[t2])
                                    pq_o = lambda ap: ap[:, 0:256].rearrange("p (g k d) -> p k g d", g=2, k=2)
                                    pq_i = lambda ap: ap[:, 0:256].rearrange("p (k g d) -> p k g d", g=2, k=2)
                                    S.op("dve", lambda e: e.tensor_tensor(out=pq_o(qr[:]), in0=pq_i(t1[:]), in1=pq_i(t2[:]), op=ALU.add),
                                         r=[t1, t2], w=[qr])
                                    S.op("dve", lambda e: e.tensor_tensor(out=qr[:, 256:384], in0=t1[:, 256:384], in1=t2[:, 256:384],
                                                                          op=ALU.add), r=[t1, t2], w=[qr])
                                else:
                                    pq_o = lambda ap: ap[:, 0:256].rearrange("p (g k d) -> p k g d", g=2, k=2)
                                    pq_i = lambda ap: ap[:, 0:256].rearrange("p (k g d) -> p k g d", g=2, k=2)
                                    S.op("dve", lambda e: e.tensor_copy(out=pq_o(qr[:]), in_=pq_i(q1[:])), r=[q1], w=[qr])
                                    S.op("dve", lambda e: e.tensor_copy(out=qr[:, 256:384], in_=q1[:, 256:384]), r=[q1], w=[qr])
                                pt = PS_TR.next()
                                ptb = bfv(pt)
                                for hh in range(3):
                                    S.op("pe", lambda e, hh=hh: e.transpose(ptb[:, hh * 128:(hh + 1) * 128], qr[:, hh * 128:(hh + 1) * 128],
                                                                            identb[:]), r=[qr, identb], w=[pt])
                                S.op("act", lambda e: e.activation(out=qT[:, :, i * 128:(i + 1) * 128],
                                                                   in_=ptb[:, 0:256].rearrange("p (a t) -> p a t", t=128),
                                                                   func=AF.Copy), r=[pt], w=[qT])
                                S.op("dve", lambda e: e.tensor_copy(out=kT[:, 0, i * 128:(i + 1) * 128], in_=ptb[:, 256:384]),
                                     r=[pt], w=[kT])
                            if debug and grp == 1 and b == 0 and l == 0:
                                S.dma("sp", dbg["qT"][:, :, :], qT[:], r=[qT])
                                S.dma("sp", dbg["kT"][:, :, :], kT[:], r=[kT])
                            if grp == 0:
                                chk("p1_C0")
                            else:
                                chk("p1_D0")
                            yts = [YSTG.next(), YSTG.next()]

                            def normalize_head(pa, co, h, ya):
                                den = ST.next()
                                if grp == 0:
                                    S.op("act", lambda e: e.activation(out=den[:, 0:1], in_=pa[:, co + 64:co + 65], func=AF.Identity,
                                                                       bias=esink[:, l * 4 + h:l * 4 + h + 1]),
                                         r=[pa, esink], w=[den])
                                    S.op("dve", lambda e: e.reciprocal(out=den[:, 1:2], in_=den[:, 0:1]), r=[den], w=[den])
                                else:
                                    S.op("act", lambda e: e.activation(out=den[:, 0:1], in_=pa[:, co + 64:co + 65], func=AF.Copy),
                                         r=[pa], w=[den])
                                    S.op("dve", lambda e: e.reciprocal(out=den[:, 1:2], in_=den[:, 0:1]), r=[den], w=[den])
                                S.op("act", lambda e: e.activation(out=ya[:, h * 64:(h + 1) * 64], in_=pa[:, co:co + 64],
                                                                   func=AF.Copy, scale=den[:, 1:2]), r=[pa, den], w=[ya])

                            def finish_ya(ya, qb):
                                pt = PS_TR.next()
                                ptb = bfv(pt)
                                for c in range(2):
                                    S.op("pe", lambda e, c=c: e.transpose(ptb[:, c * 128:(c + 1) * 128], ya[:, c * 128:(c + 1) * 128],
                                                                          identb[:]), r=[ya, identb], w=[pt])
                                for c in range(2):
                                    S.op("dve", lambda e, c=c: e.tensor_copy(out=yts[c][:, qb * 128:(qb + 1) * 128],
                                                                             in_=ptb[:, c * 128:(c + 1) * 128]), r=[pt], w=[yts[c]])

                            for qb in range(NTT):
                                if qb >= NTL:
                                    kts = [NTL, NTL + 1]
                                elif grp == 0:
                                    kts = [m for m in (qb - 1, qb, qb + 1) if 0 <= m < NTL] + [NTL, NTL + 1]
                                else:
                                    kts = list(range(NTT))
                                nk = len(kts)
                                pacc = PS_AC.next()
                                ya = YAT.next()
                                for h in range(4):
                                    kh = h // 2
                                    for g0 in range(0, nk, 4):
                                        grpk = kts[g0:g0 + 4]
                                        ps = PS_MM.next()
                                        for ki, m in enumerate(grpk):
                                            S.op("pe", lambda e, m=m, ki=ki: e.matmul(
                                                ps[:, ki * 128:(ki + 1) * 128], lhsT=kT[kh * 64:(kh + 1) * 64, 0, m * 128:(m + 1) * 128],
                                                rhs=qT[kh * 64:(kh + 1) * 64, h % 2, qb * 128:(qb + 1) * 128], start=True, stop=True),
                                                r=[kT, qT], w=[ps])
                                        pt_ = PTL.next()
                                        nn_ = len(grpk) * 128
                                        S.op("act", lambda e: e.activation(out=pt_[:, 0:nn_], in_=ps[:, 0:nn_], func=AF.Exp, scale=0.125),
                                             r=[ps], w=[pt_])
                                        if grp == 0 and qb < NTL:
                                            for ki, m in enumerate(grpk):
                                                if (m == qb - 1 or m == qb + 1) and m < NTL:
                                                    mi = 1 if m == qb - 1 else 0
                                                    S.op("pool", lambda e, ki=ki, mi=mi: e.tensor_tensor(
                                                        out=pt_[:, ki * 128:(ki + 1) * 128], in0=pt_[:, ki * 128:(ki + 1) * 128],
                                                        in1=masks[:, mi, :], op=ALU.mult), r=[pt_, masks], w=[pt_])
                                        for ki, m in enumerate(grpk):
                                            S.op("pe", lambda e, ki=ki, m=m: e.matmul(
                                                pacc[:, h * 65:(h + 1) * 65], lhsT=pt_[:, ki * 128:(ki + 1) * 128], rhs=Vt[:, m, kh, :],
                                                start=(g0 + ki == 0), stop=(g0 + ki == nk - 1)), r=[pt_, Vt], w=[pacc])
                                for h in range(4):
                                    normalize_head(pacc, h * 65, h, ya)
                                finish_ya(ya, qb)
                                if grp == 1:
                                    chk("gQ_%d" % qb)
                            if grp == 0:
                                chk("p1_C")
                            for c in range(2):
                                S.dma("sp", YT[b, 4 + grp * 2 + c], yts[c][:], r=[yts[c]], w=[YTB[b]], add=True)

                        chk("p1_CD")
                        S.dma("sp", g1b[:], MODS[l][b:b + 1, 2048:3072].to_broadcast([128, D]), w=[g1b])
                        for (t0, n) in TCH:
                            if t0 >= T:
                                S.dma("sp", g1b[:], MODS[l][4:5, 2048:3072].to_broadcast([128, D]), w=[g1b])
                            yl = YTL.next()
                            for c in range(8):
                                S.dma("sp", yl[:, c, 0:n], YT[b, c][:, t0:t0 + n], r=[YTB[b]], w=[yl], add=(c > 0))
                            for i in range(t0 // 128, (t0 + n) // 128):
                                tt = i * 128 - t0
                                j = b if i < NTL else 4
                                gt = g1b
                                xtile = XT.next()
                                S.dma("sp", xtile[:], x_src(l, b, i), w=[xtile])
                                for half in range(2):
                                    ps = PS_MM.next()
                                    for k in range(8):
                                        S.op("pe", lambda e, k=k: e.matmul(ps[:, :], lhsT=yl[:, k, tt:tt + 128],
                                                                          rhs=woutb[:, k, half * 512:(half + 1) * 512],
                                                                          start=(k == 0), stop=(k == 7)), r=[yl, woutb], w=[ps])
                                    tm = TMPF.next()
                                    S.op("dve", lambda e: e.tensor_tensor(out=tm[:], in0=ps[:, :], in1=gt[:, half * 512:(half + 1) * 512],
                                                                          op=ALU.mult), r=[ps, gt], w=[tm])
                                    S.op("pool", lambda e: e.tensor_tensor(out=xtile[:, half * 512:(half + 1) * 512],
                                                                           in0=xtile[:, half * 512:(half + 1) * 512], in1=tm[:],
                                                                           op=ALU.add), r=[xtile, tm], w=[xtile])
                                S.dma("sp", XMID[b, i], xtile[:], r=[xtile])
                                if debug and b == 0 and l == 0:
                                    S.dma("sp", dbg["xmid"][i], xtile[:], r=[xtile])
                                xn = XNB.next()
                                h2 = H2T.next()
                                norm_to_T(xtile, h2, slice(0, 128), A2, 24, j, xn_keep=xn)
                                S.dma("sp", XN2[b, i], xn[:], r=[xn])
                                ps = PS_MM.next()
                                for k in range(8):
                                    S.op("pe", lambda e, k=k: e.matmul(ps[:, 0:NE], lhsT=h2[:, k, :], rhs=rtb[:, k, :],
                                                                      start=(k == 0), stop=(k == 7)), r=[h2, rtb], w=[ps])
                                ss = ST.next()
                                ex = TMPF.next()
                                S.op("pool", lambda e: e.memset(ss[:], 0.0), w=[ss])
                                S.op("act", lambda e: e.activation(out=ex[:, 0:NE], in_=ps[:, 0:NE], func=AF.Exp,
                                                                   accum_out=ss[:, 0:1]), r=[ps], w=[ex, ss])
                                S.op("dve", lambda e: e.reciprocal(out=ss[:, 1:2], in_=ss[:, 0:1]), r=[ss], w=[ss])
                                S.op("dve", lambda e: e.tensor_scalar(out=affTok[:, i, b * 16:(b + 1) * 16], in0=ex[:, 0:NE],
                                                                      scalar1=ss[:, 1:2], scalar2=None, op0=ALU.mult),
                                     r=[ex, ss], w=[affTok])
                    if debug and l == 0:
                        S.dma("sp", dbg["aff"][:, :, :], affTok[:], r=[affTok])
                        S.barrier()
                        for c in range(8):
                            t_ = YSTG.next()
                            S.dma("sp", t_[:], YT[0, c], w=[t_])
                            S.dma("sp", dbg["yt"][c], t_[:], r=[t_])
                    S.barrier()

                chk("p1")
                with ExitStack() as m1:
                    affT = sb("affT", [64, NTOK], F32, m1)
                    work = sb("work", [64, NTOK], F32, m1)
                    Gx = sb("Gx", [64, NTOK], F32, m1)
                    gtok = sb("gtok", [128, NTT, 64], F32, m1)
                    maskb = sb("maskb", [128, NTT, 64], BF16, m1)
                    slotTok = sb("slotTok", [128, NTT, 64], F32, m1)
                    MX = Pool([sb("mx%d" % i, [64, 8], F32, m1) for i in range(2)])
                    xn2 = sb("xn2", [128, NTT, D], BF16, m1)
                    PB = Pool([sb("pb%d" % i, [128, 256], BF16, m1) for i in range(NTL)])
                    PBC = Pool([sb("pbc%d" % i, [128, 32], BF16, m1) for i in range(2)])
                    XSTG = Pool([sb("xstg%d" % i, [128, 8, 256], BF16, m1) for i in range(2)])
                    XSTC = Pool([sb("xstc%d" % i, [128, 8, 32], BF16, m1) for i in range(2)])

                    def tr_in(dst, src_fn, rows_out, nt, ident_n, rbufs):
                        pass

                    for i0 in range(0, NTT, 4):
                        pt = PS_TR.next()
                        nn = min(4, NTT - i0)
                        for q in range(nn):
                            S.op("pe", lambda e, q=q: e.transpose(pt[0:64, q * 128:(q + 1) * 128], affTok[:, i0 + q, :], identf[:]),
                                 r=[affTok, identf], w=[pt])
                        S.op("act", lambda e: e.activation(out=affT[:, i0 * 128:(i0 + nn) * 128], in_=pt[0:64, 0:nn * 128], func=AF.Copy),
                             r=[pt], w=[affT])
                    S.op("dve", lambda e: e.tensor_copy(out=work[:], in_=affT[:]), r=[affT], w=[work])
                    for (s0, sn, rounds) in ((0, T, CAP // 8), (T, C, CAPC // 8)):
                        for r_ in range(rounds):
                            m8 = MX.next()
                            S.op("dve", lambda e: e.max(out=m8[:], in_=work[:, s0:s0 + sn]), r=[work], w=[m8])
                            S.op("dve", lambda e: e.match_replace(out=work[:, s0:s0 + sn], in_to_replace=m8[:],
                                                                  in_values=work[:, s0:s0 + sn], imm_value=0.0), r=[work, m8], w=[work])
                    S.op("dve", lambda e: e.tensor_tensor(out=Gx[:], in0=affT[:], in1=work[:], op=ALU.subtract), r=[affT, work], w=[Gx])
                    S.dma("sp", SLOTG[:, 1, :], Gx[:], r=[Gx])
                    for i0 in range(0, NTT, 4):
                        pt = PS_TR.next()
                        nn = min(4, NTT - i0)
                        for q in range(nn):
                            S.op("pe", lambda e, q=q: e.transpose(pt[:, q * 64:(q + 1) * 64], Gx[:, (i0 + q) * 128:(i0 + q + 1) * 128],
                                                                  identf[0:64, 0:64]), r=[Gx, identf], w=[pt])
                        S.op("act", lambda e: e.activation(out=gtok[:, i0:i0 + nn, :],
                                                           in_=pt[:, 0:nn * 64].rearrange("p (a c) -> p a c", c=64), func=AF.Copy),
                             r=[pt], w=[gtok])
                    S.op("dve", lambda e: e.tensor_scalar(out=maskb[:], in0=gtok[:], scalar1=0.0, scalar2=None, op0=ALU.is_gt),
                         r=[gtok], w=[maskb])
                    for i in range(NTT):
                        prev = list(range(0, i)) if i < NTL else list(range(NTL, i))
                        ps = PS_MM.next()
                        for ii, ip in enumerate(prev):
                            S.op("pe", lambda e, ip=ip, ii=ii: e.matmul(ps[:, 0:64], lhsT=onesb[:], rhs=maskb[:, ip, :],
                                                                        start=(ii == 0), stop=False), r=[onesb, maskb], w=[ps])
                        S.op("pe", lambda e: e.matmul(ps[:, 0:64], lhsT=masks[:, 0, :], rhs=maskb[:, i, :],
                                                      start=(len(prev) == 0), stop=True), r=[masks, maskb], w=[ps])
                        S.op("dve", lambda e: e.tensor_tensor(out=slotTok[:, i, :], in0=ps[:, 0:64], in1=maskb[:, i, :], op=ALU.mult),
                             r=[ps, maskb], w=[slotTok])
                    for i0 in range(0, NTT, 4):
                        pt = PS_TR.next()
                        nn = min(4, NTT - i0)
                        for q in range(nn):
                            S.op("pe", lambda e, q=q: e.transpose(pt[0:64, q * 128:(q + 1) * 128], slotTok[:, i0 + q, :], identf[:]),
                                 r=[slotTok, identf], w=[pt])
                        S.op("act", lambda e: e.activation(out=work[:, i0 * 128:(i0 + nn) * 128], in_=pt[0:64, 0:nn * 128], func=AF.Copy),
                             r=[pt], w=[work])
                    S.dma("sp", SLOTG[:, 0, :], work[:], r=[work])
                    if debug and l == 0:
                        S.dma("sp", dbg["slotg"][:, 0, :], work[:], r=[work])
                        S.dma("sp", dbg["slotg"][:, 1, :], Gx[:], r=[Gx])
                    chk("m1_topk")
                    for b in range(NS):
                        for i in range(NTT):
                            S.dma("sp", xn2[:, i, :], XN2[b, i], w=[xn2], add=(i > 0))
                        for ex_ in range(NE):
                            col = b * 16 + ex_
                            Ps = []
                            for i in range(NTL):
                                P = PB.next()
                                S.op("dve" if i % 2 == 0 else "pool", lambda e: e.tensor_scalar(
                                    out=P[:], in0=iota[:, 0:256], scalar1=slotTok[:, i, col:col + 1], scalar2=None, op0=ALU.is_equal),
                                    r=[iota, slotTok], w=[P])
                                Ps.append(P)
                            stg = XSTG.next()
                            for cg in range(2):
                                pss = [PS_MM.next() for _ in range(4)]
                                for i in range(NTL):
                                    for cc in range(4):
                                        c = cg * 4 + cc
                                        S.op("pe", lambda e, c=c, cc=cc: e.matmul(pss[cc][:, 0:256],
                                                                                  lhsT=xn2[:, i, c * 128:(c + 1) * 128], rhs=Ps[i][:],
                                                                                  start=(i == 0), stop=(i == NTL - 1)),
                                             r=[xn2, Ps[i]], w=[pss[cc]])
                                for cc in range(4):
                                    c = cg * 4 + cc
                                    src = pss[cc][:, 0:256]
                                    if c % 2 == 0:
                                        S.op("act", lambda e, c=c, src=src: e.activation(
                                            out=stg[:, c, :], in_=src, func=AF.Identity, scale=A2[:, c, b:b + 1],
                                            bias=modsT[:, l, 24 + c, b:b + 1]), r=[pss[cc], A2, modsT], w=[stg])
                                    else:
                                        S.op("dve", lambda e, c=c, src=src: e.tensor_scalar(
                                            out=stg[:, c, :], in0=src, scalar1=A2[:, c, b:b + 1], scalar2=modsT[:, l, 24 + c, b:b + 1],
                                            op0=ALU.mult, op1=ALU.add), r=[pss[cc], A2, modsT], w=[stg])
                            for c in range(8):
                                S.dma("sp", XSG[ex_, c][:, b * CAP:(b + 1) * CAP], stg[:, c, :], r=[stg])
                            psc = PS_AC.next()
                            Pcs = []
                            for i in range(NTL, NTT):
                                Pc = PBC.next()
                                S.op("pool", lambda e: e.tensor_scalar(out=Pc[:], in0=iota[:, 0:32], scalar1=slotTok[:, i, col:col + 1],
                                                                       scalar2=None, op0=ALU.is_equal), r=[iota, slotTok], w=[Pc])
                                Pcs.append(Pc)
                            for c in range(8):
                                for ii, i in enumerate(range(NTL, NTT)):
                                    S.op("pe", lambda e, c=c, i=i, ii=ii: e.matmul(psc[:, c * 32:(c + 1) * 32],
                                                                                   lhsT=xn2[:, i, c * 128:(c + 1) * 128], rhs=Pcs[ii][:],
                                                                                   start=(i == NTL), stop=(i == NTT - 1)),
                                         r=[xn2, Pcs[ii]], w=[psc])
                            stc = XSTC.next()
                            for c in range(8):
                                S.op("dve", lambda e, c=c: e.tensor_scalar(
                                    out=stc[:, c, :], in0=psc[:, c * 32:(c + 1) * 32], scalar1=A2[:, c, 4:5],
                                    scalar2=modsT[:, l, 24 + c, 4:5], op0=ALU.mult, op1=ALU.add), r=[psc, A2, modsT], w=[stc])
                            o0 = NS * CAP + b * CAPC
                            S.dma("sp", XSG[ex_][:, :, o0:o0 + CAPC].rearrange("c p s -> p c s"), stc[:], r=[stc])
                    S.barrier()

                chk("m1")
                with ExitStack() as m2:
                    WG = [sb("wg%d" % i, [128, 8, D], BF16, m2) for i in range(2)]
                    WU = [sb("wu%d" % i, [128, 8, D], BF16, m2) for i in range(2)]
                    WD = [sb("wd%d" % i, [128, 8, D], BF16, m2) for i in range(2)]
                    XS = [sb("xs%d" % i, [128, 8, NSL], BF16, m2) for i in range(2)]
                    actT = sb("actT", [128, 8, NSL], BF16, m2)
                    SA = Pool([sb("sa%d" % i, [128, 512], F32, m2) for i in range(3)])
                    YS = Pool([sb("ys%d" % i, [128, D], BF16, m2) for i in range(2)])
                    SCH = [(s0, min(512, NSL - s0)) for s0 in range(0, NSL, 512)]
                    STL = [(s0, min(128, NSL - s0)) for s0 in range(0, NSL, 128)]
                    for ex_ in range(NE):
                        wg, wu, wd, xs = WG[ex_ % 2], WU[ex_ % 2], WD[ex_ % 2], XS[ex_ % 2]
                        dma_k("pool", wg, w_gate[l, ex_])
                        dma_k("pool", wu, w_up[l, ex_])
                        dma_k("pool", wd, w_down[l, ex_])
                        for c in range(8):
                            S.dma("sp", xs[:, c, :], XSG[ex_, c], w=[xs], add=(c > 0))
                        for (s0, n) in SCH:
                            for fc in range(8):
                                pA, pU = PS_MM.next(), PS_MM.next()
                                for k in range(8):
                                    S.op("pe", lambda e, k=k: e.matmul(pA[:, 0:n], lhsT=wg[:, k, fc * 128:(fc + 1) * 128],
                                                                      rhs=xs[:, k, s0:s0 + n], start=(k == 0), stop=(k == 7)),
                                         r=[wg, xs], w=[pA])
                                for k in range(8):
                                    S.op("pe", lambda e, k=k: e.matmul(pU[:, 0:n], lhsT=wu[:, k, fc * 128:(fc + 1) * 128],
                                                                      rhs=xs[:, k, s0:s0 + n], start=(k == 0), stop=(k == 7)),
                                         r=[wu, xs], w=[pU])
                                sa = SA.next()
                                S.op("act", lambda e: e.activation(out=sa[:, 0:n], in_=pA[:, 0:n], func=AF.Silu), r=[pA], w=[sa])
                                S.op("dve", lambda e: e.tensor_tensor(out=actT[:, fc, s0:s0 + n], in0=sa[:, 0:n], in1=pU[:, 0:n],
                                                                      op=ALU.mult), r=[sa, pU], w=[actT])
                        for (s0, rows) in STL:
                            ys = YS.next()
                            for half in range(2):
                                ps = PS_MM.next()
                                for fc in range(8):
                                    S.op("pe", lambda e, fc=fc: e.matmul(ps[0:rows, :], lhsT=actT[:, fc, s0:s0 + rows],
                                                                        rhs=wd[:, fc, half * 512:(half + 1) * 512],
                                                                        start=(fc == 0), stop=(fc == 7)), r=[actT, wd], w=[ps])
                                if half == 0:
                                    S.op("act", lambda e: e.activation(out=ys[0:rows, 0:512], in_=ps[0:rows, :], func=AF.Copy),
                                         r=[ps], w=[ys])
                                else:
                                    S.op("dve", lambda e: e.tensor_copy(out=ys[0:rows, 512:1024], in_=ps[0:rows, :]), r=[ps], w=[ys])
                            S.dma("sp", YEX[ex_][s0:s0 + rows, :], ys[0:rows, :], r=[ys])
                    S.barrier()

                chk("m2")
                with ExitStack() as m3:
                    selb = sb("selb", [16, 16 * 128], F32, m3)
                    slot = sb("slot", [16, NTOK], F32, m3)
                    gg = sb("gg", [16, NTOK], F32, m3)
                    Yl = sb("Yl", [128, NE, 2, D], BF16, m3)
                    Yc = sb("Yc", [32, NE, D], BF16, m3)
                    PT = sb("PT", [128, NE, 2, 512], BF16, m3)
                    PTc = sb("PTc", [32, NE, 256], BF16, m3)
                    GB = Pool([sb("gb%d" % i, [128, 512], F32, m3) for i in range(3)])
                    g2b = sb("g2b", [128, D], F32, m3)
                    XT3 = Pool([sb("xt3_%d" % i, [128, D], F32, m3) for i in range(2)])
                    OT3 = Pool([sb("ot3_%d" % i, [128, D], F32, m3) for i in range(1)])
                    ST3 = Pool([sb("st3_%d" % i, [128, 8], F32, m3) for i in range(4)])
                    fgb = sb("fgb", [128, D], F32, m3)
                    S.dma("sp", fgb[:], final_g[0:1, :].to_broadcast([128, D]), w=[fgb])
                    S.dma("sp", selb[:], c_sel[:, :], w=[selb])
                    last = (l == DEPTH - 1)
                    for b in range(NS):
                        S.dma("sp", slot[:], SLOTG[b * 16:(b + 1) * 16, 0, :], w=[slot])
                        S.dma("sp", gg[:], SLOTG[b * 16:(b + 1) * 16, 1, :], w=[gg])
                        for ex_ in range(NE):
                            S.dma("sp", Yl[:, ex_], YEX[ex_][b * CAP:(b + 1) * CAP, :].rearrange("(k p) d -> p k d", p=128), w=[Yl],
                                  add=(ex_ > 0))
                        o0 = NS * CAP + b * CAPC
                        for ex_ in range(NE):
                            S.dma("sp", Yc[:, ex_, :], YEX[ex_][o0:o0 + CAPC, :], w=[Yc], add=(ex_ > 0))
                        S.dma("sp", g2b[:], MODS[l][b:b + 1, 5120:6144].to_broadcast([128, D]), w=[g2b])
                        for (t0, n) in TCH:
                            latent = t0 < T
                            if last and not latent:
                                continue
                            R = 128 if latent else 32
                            if not latent:
                                S.dma("sp", g2b[:], MODS[l][4:5, 5120:6144].to_broadcast([128, D]), w=[g2b])
                            for ex_ in range(NE):
                                psS, psG = PS_MM.next(), PS_MM.next()
                                S.op("pe", lambda e: e.matmul(psS[0:R, 0:n], lhsT=selb[:, ex_ * 128:ex_ * 128 + R], rhs=slot[:, t0:t0 + n],
                                                              start=True, stop=True), r=[selb, slot], w=[psS])
                                S.op("pe", lambda e: e.matmul(psG[0:R, 0:n], lhsT=selb[:, ex_ * 128:ex_ * 128 + R], rhs=gg[:, t0:t0 + n],
                                                              start=True, stop=True), r=[selb, gg], w=[psG])
                                gb = GB.next()
                                S.op("act", lambda e: e.activation(out=gb[0:R, 0:n], in_=psG[0:R, 0:n], func=AF.Copy), r=[psG], w=[gb])
                                if latent:
                                    for k in range(2):
                                        S.op("dve", lambda e, k=k: e.scalar_tensor_tensor(
                                            out=PT[:, ex_, k, 0:n], in0=psS[:, 0:n], scalar=iota[:, 256 + k:257 + k], in1=gb[:, 0:n],
                                            op0=ALU.is_equal, op1=ALU.mult), r=[psS, iota, gb], w=[PT])
                                else:
                                    S.op("dve", lambda e: e.scalar_tensor_tensor(
                                        out=PTc[:, ex_, 0:n], in0=psS[0:32, 0:n], scalar=iota[0:32, 256:257], in1=gb[0:32, 0:n],
                                        op0=ALU.is_equal, op1=ALU.mult), r=[psS, iota, gb], w=[PTc])
                            for i in range(t0 // 128, (t0 + n) // 128):
                                tt = i * 128 - t0
                                xtile = XT3.next()
                                S.dma("sp", xtile[:], XMID[b, i], w=[xtile])
                                gt = g2b
                                for half in range(2):
                                    ps = PS_AC.next()
                                    if latent:
                                        for ex_ in range(NE):
                                            for k in range(2):
                                                S.op("pe", lambda e, ex_=ex_, k=k: e.matmul(
                                                    ps[:, :], lhsT=PT[:, ex_, k, tt:tt + 128], rhs=Yl[:, ex_, k, half * 512:(half + 1) * 512],
                                                    start=(ex_ == 0 and k == 0), stop=(ex_ == NE - 1 and k == 1)), r=[PT, Yl], w=[ps])
                                    else:
                                        for ex_ in range(NE):
                                            S.op("pe", lambda e, ex_=ex_: e.matmul(
                                                ps[:, :], lhsT=PTc[:, ex_, tt:tt + 128], rhs=Yc[:, ex_, half * 512:(half + 1) * 512],
                                                start=(ex_ == 0), stop=(ex_ == NE - 1)), r=[PTc, Yc], w=[ps])
                                    tm = GB.next()
                                    S.op("dve", lambda e: e.tensor_tensor(out=tm[:], in0=ps[:, :], in1=gt[:, half * 512:(half + 1) * 512],
                                                                          op=ALU.mult), r=[ps, gt], w=[tm])
                                    S.op("pool", lambda e: e.tensor_tensor(out=xtile[:, half * 512:(half + 1) * 512],
                                                                           in0=xtile[:, half * 512:(half + 1) * 512], in1=tm[:],
                                                                           op=ALU.add), r=[xtile, tm], w=[xtile])
                                if debug and b == 0 and l == 0:
                                    S.dma("sp", dbg["xres"][i], xtile[:], r=[xtile])
                                if not last:
                                    S.dma("sp", XRES[b, i], xtile[:], r=[xtile])
                                else:
                                    ot = OT3.next()
                                    ss = ST3.next()
                                    S.op("pool", lambda e: e.memset(ss[:], 0.0), w=[ss])
                                    S.op("act", lambda e: e.activation(out=ot[:], in_=xtile[:], func=AF.Square, accum_out=ss[:, 0:1]),
                                         r=[xtile], w=[ot, ss])
                                    S.op("dve", lambda e: e.tensor_scalar(out=ss[:, 1:2], in0=ss[:, 0:1], scalar1=1.0 / D, scalar2=EPS,
                                                                          op0=ALU.mult, op1=ALU.add), r=[ss], w=[ss])
                                    S.op("dve", lambda e: e.reciprocal(out=ss[:, 2:3], in_=ss[:, 1:2]), r=[ss], w=[ss])
                                    S.op("act", lambda e: e.activation(out=ss[:, 3:4], in_=ss[:, 2:3], func=AF.Sqrt), r=[ss], w=[ss])
                                    S.op("dve", lambda e: e.scalar_tensor_tensor(out=ot[:], in0=xtile[:], scalar=ss[:, 3:4], in1=fgb[:],
                                                                                 op0=ALU.mult, op1=ALU.mult), r=[xtile, ss, fgb], w=[ot])
                                    S.dma("sp", out[b, i * 128:(i + 1) * 128, :], ot[:], r=[ot])
                    S.barrier()
        except _Stop:
            S.barrier()
        S.barrier(engines=["sp"])
        print("instr counts", S.cnt, "wait instrs", S.n_wait_ins, "ndma", sum(S.dma_val) // 16, flush=True)
    return nc


def _consts():
    identb = np.eye(128, dtype=np.float32).astype(ml_dtypes.bfloat16)
    identf = np.eye(128, dtype=np.float32)
    j = np.arange(128)[:, None]
    q = np.arange(128)[None, :]
    masks = np.stack([(j <= q), (j >= q)], axis=1).astype(np.float32).astype(ml_dtypes.bfloat16)
    iota = np.zeros((128, 258), np.float32)
    iota[:, 0:256] = np.arange(1, 257, dtype=np.float32)[None, :]
    iota[:, 256] = np.arange(1, 129)
    iota[:, 257] = np.arange(129, 257)
    pos = np.arange(T)
    row = (pos // 64).astype(np.float32)
    colp = (pos % 64).astype(np.float32)
    inv = (10000.0 ** (-np.arange(16, dtype=np.float32) / 16)).astype(np.float32)
    ar = row[:, None] * inv
    ac = colp[:, None] * inv
    ang = np.concatenate([ar, ar, ac, ac], axis=-1).astype(np.float32)
    cos = np.cos(ang).astype(np.float32)
    sin = np.sin(ang).astype(np.float32)
    sgn = np.tile(np.concatenate([-np.ones(16), np.ones(16)]), 2).astype(np.float32)
    ss = sin * sgn[None, :]
    cos6 = np.tile(cos, (1, 6))
    ss6 = np.tile(ss, (1, 6))
    rope = np.stack([cos6, ss6], axis=1).reshape(NTL, 128, 2, 384).astype(np.float32)
    sel = np.zeros((16, 16, 128), np.float32)
    for e in range(16):
        sel[e, e, :] = 1.0
    return dict(c_identb=identb, c_identf=identf, c_masks=masks, c_iota=iota, c_rope=rope,
                c_sel=sel.reshape(16, 16 * 128))


def _prep_shared(inp, DEPTH):
    f = lambda a: np.ascontiguousarray(np.asarray(a, dtype=np.float32))
    sh = {}
    sh["ada_w"] = f(inp["ada_w"][:DEPTH])
    sh["ada_b"] = f(inp["ada_b"][:DEPTH])
    rep = lambda g: f(np.repeat(np.asarray(g)[:DEPTH].reshape(DEPTH, 8, 128).transpose(0, 2, 1)[..., None], 5, axis=-1))
    sh["n1rep"] = rep(inp["norm1_g"])
    sh["n2rep"] = rep(inp["norm2_g"])
    sh["w_in"] = f(inp["w_in"][:DEPTH])
    cp = np.zeros((DEPTH, 128, 2, 40), np.float32)
    ca = np.asarray(inp["conv_a_w"])[:DEPTH]
    cb = np.asarray(inp["conv_b_w"])[:DEPTH]
    for h in range(2):
        cp[:, :, h, 0:3] = ca[:, :, h * 128:(h + 1) * 128].transpose(0, 2, 1)
        cp[:, :, h, 3:34] = cb[:, :, h * 128:(h + 1) * 128].transpose(0, 2, 1)
        cp[:, :, h, 34] = np.asarray(inp["conv_b_b"])[:DEPTH, h * 128:(h + 1) * 128]
        cp[:, :, h, 35] = np.asarray(inp["conv_ln_g"])[:DEPTH, h * 128:(h + 1) * 128]
        cp[:, :, h, 36] = np.asarray(inp["conv_ln_b"])[:DEPTH, h * 128:(h + 1) * 128]
    sh["convp"] = cp
    sh["sink"] = f(np.asarray(inp["sink"])[:DEPTH].reshape(1, DEPTH * 4))
    sh["qng"] = f(inp["q_norm_g"][:DEPTH])
    sh["kng"] = f(inp["k_norm_g"][:DEPTH])
    sh["w_out"] = f(inp["w_out"][:DEPTH])
    sh["router"] = f(inp["router_w"][:DEPTH])
    sh["w_gate"] = f(inp["w_gate"][:DEPTH])
    sh["w_up"] = f(inp["w_up"][:DEPTH])
    sh["w_down"] = f(inp["w_down"][:DEPTH])
    sh["final_g"] = f(np.asarray(inp["final_g"]).reshape(1, D))
    sh.update(_consts())
    return sh


def _core_inputs(inp, sh, b0, NS):
    m = dict(sh)
    m["x"] = np.ascontiguousarray(np.asarray(inp["x"][b0:b0 + NS], dtype=np.float32))
    m["ctx"] = np.ascontiguousarray(np.asarray(inp["ctx"][b0:b0 + NS], dtype=np.float32))
    call = np.zeros((5, D), np.float32)
    call[0:NS] = np.asarray(inp["c"][b0:b0 + NS])
    call[4] = np.asarray(inp["c_ctx"])
    m["cT"] = np.ascontiguousarray(call.reshape(5, 8, 128).transpose(2, 1, 0))
    return m


_NC_CACHE = {}


def kernel(**inputs):
    NS, DEPTH = 4, 4
    key = (NS, DEPTH)
    if key not in _NC_CACHE:
        _NC_CACHE[key] = build_program(NS, DEPTH)
    nc = _NC_CACHE[key]
    sh = _prep_shared(inputs, DEPTH)
    in_maps = [_core_inputs(inputs, sh, c * NS, NS) for c in range(N_CORES)]
    res = run_bass_kernel_spmd(nc, in_maps, core_ids=list(range(N_CORES)))
    outs = [np.asarray(r["out"]) for r in res.results]
    return np.concatenate(outs, axis=0).astype(np.float32)
```

```python
import numpy as np
import ml_dtypes
from contextlib import ExitStack
import concourse.bass as bass
import concourse.mybir as mybir
from concourse.bass_utils import run_bass_kernel_spmd

F32 = mybir.dt.float32
BF16 = mybir.dt.bfloat16
AF = mybir.ActivationFunctionType
ALU = mybir.AluOpType
AX = mybir.AxisListType

D = 1024
T = 2048
C = 256
NTOK = T + C
NTL = T // 128
NTT = NTOK // 128
NE = 16
CAP = 256
CAPC = 32
IN_W = 2304
EPS = 1e-6
N_CORES = 8
SAME_ENG_SYNC = True


class Buf:
    __slots__ = ("name", "w", "r")

    def __init__(self, name=""):
        self.name = name
        self.w = []
        self.r = {}


class Tl:
    def __init__(self, t, name=""):
        self.t = t
        self.b = Buf(name)

    def __getitem__(self, k):
        return self.t[k]


class Sync:
    W = 30000
    NDMA = 20

    def __init__(self, nc, es):
        self.nc = nc
        self.es = es
        self.engs = {"pe": nc.tensor, "act": nc.scalar, "dve": nc.vector, "pool": nc.gpsimd, "sp": nc.sync}
        self.cnt = {e: 0 for e in self.engs}
        self.sems = {e: [] for e in self.engs}
        self.known = {e: {} for e in self.engs}
        self.semid = {}
        self.dma_sems = [self._newsem("dma%d" % i) for i in range(self.NDMA)]
        self.dma_val = [0] * self.NDMA
        self.dma_i = 0
        self.n_wait_ins = 0
        self.dead = False

    def _newsem(self, name):
        s = self.es.enter_context(self.nc.semaphore(name))
        self.semid[id(s)] = len(self.semid)
        return s

    def _sid(self, s):
        return self.semid[id(s)]

    def _next_tok(self, eng):
        n = self.cnt[eng]
        k = n // self.W
        while len(self.sems[eng]) <= k:
            self.sems[eng].append(self._newsem("%s_m%d" % (eng, len(self.sems[eng]))))
        self.cnt[eng] = n + 1
        return (self.sems[eng][k], n % self.W + 1, eng)

    def _last_tok(self, eng):
        n = self.cnt[eng]
        if n == 0:
            return None
        n -= 1
        return (self.sems[eng][n // self.W], n % self.W + 1, eng)

    def _deps(self, r, w, add=False):
        toks = []
        for b in r:
            toks.extend(b.w)
        for b in w:
            if not add:
                toks.extend(b.w)
            toks.extend(b.r.values())
        return toks

    def _need(self, eng, toks):
        kn = self.known[eng]
        need = {}
        for (s, v, src) in toks:
            if src == eng and (eng == "pe" or not SAME_ENG_SYNC):
                continue
            sid = self._sid(s)
            if kn.get(sid, 0) >= v:
                continue
            if sid not in need or need[sid][1] < v:
                need[sid] = (s, v)
        return list(need.values())

    def _emit_waits(self, eng, waits, ins_fn):
        E = self.engs[eng]
        for (s, v) in waits[:-1]:
            E.wait_ge(s, v)
            self.n_wait_ins += 1
        ins = ins_fn(E)
        if waits:
            s, v = waits[-1]
            ins._wait_ge(s, v)
        kn = self.known[eng]
        for (s, v) in waits:
            sid = self._sid(s)
            if kn.get(sid, 0) < v:
                kn[sid] = v
        return ins

    def op(self, eng, fn, r=(), w=()):
        if self.dead:
            return None
        r = [x.b if isinstance(x, Tl) else x for x in r]
        w = [x.b if isinstance(x, Tl) else x for x in w]
        waits = self._need(eng, self._deps(r, w))
        ins = self._emit_waits(eng, waits, fn)
        tok = self._next_tok(eng)
        ins.then_inc(tok[0], 1)
        for b in r:
            b.r[eng] = tok
        for b in w:
            b.w = [tok]
            b.r = {}
        return ins

    def dma(self, eng, out, in_, r=(), w=(), add=False, **kw):
        if self.dead:
            return None
        r = [x.b if isinstance(x, Tl) else x for x in r]
        w = [x.b if isinstance(x, Tl) else x for x in w]
        toks = self._deps(r, w, add)
        i = self.dma_i
        self.dma_i = (i + 1) % self.NDMA
        s = self.dma_sems[i]
        if self.dma_val[i] > 0:
            toks.append((s, self.dma_val[i], "dma"))
        waits = self._need(eng, toks)
        ins = self._emit_waits(eng, waits, lambda E: E.dma_start(out=out, in_=in_, **kw))
        self.dma_val[i] += 16
        tok = (s, self.dma_val[i], "dma")
        ins.then_inc(s, 16)
        for b in r:
            b.r[("dma", i)] = tok
        for b in w:
            if add:
                b.w.append(tok)
            else:
                b.w = [tok]
                b.r = {}
        return ins

    def barrier(self, engines=None):
        toks = []
        for e in self.engs:
            t = self._last_tok(e)
            if t is not None:
                toks.append(t)
        for i in range(self.NDMA):
            if self.dma_val[i] > 0:
                toks.append((self.dma_sems[i], self.dma_val[i], "dma"))
        for e in (engines or list(self.engs)):
            kn = self.known[e]
            E = self.engs[e]
            for (s, v, src) in toks:
                if src == e:
                    continue
                sid = self._sid(s)
                if kn.get(sid, 0) >= v:
                    continue
                E.wait_ge(s, v)
                self.n_wait_ins += 1
                kn[sid] = v


class Pool:
    def __init__(self, tiles):
        self.tiles = tiles
        self.i = 0

    def next(self):
        t = self.tiles[self.i]
        self.i = (self.i + 1) % len(self.tiles)
        return t


class _Stop(Exception):
    pass


def build_program(NS=4, DEPTH=4, debug=False, stop=None):
    _sref = []

    _cur = [0]

    def chk(tag):
        if stop == tag or stop == "%s@%d" % (tag, _cur[0]):
            _sref[0].dead = True
    nc = bass.Bass("TRN2", target_bir_lowering=False)
    dt = lambda name, shape, dtype, kind="Internal": nc.dram_tensor(name, list(shape), dtype, kind=kind).ap()
    x_in = dt("x", [NS, T, D], F32, "ExternalInput")
    ctx_in = dt("ctx", [NS, C, D], F32, "ExternalInput")
    cT_in = dt("cT", [128, 8, 5], F32, "ExternalInput")
    ada_w = dt("ada_w", [DEPTH, D, 6 * D], F32, "ExternalInput")
    ada_b = dt("ada_b", [DEPTH, 6 * D], F32, "ExternalInput")
    n1rep = dt("n1rep", [DEPTH, 128, 8, 5], F32, "ExternalInput")
    n2rep = dt("n2rep", [DEPTH, 128, 8, 5], F32, "ExternalInput")
    w_in = dt("w_in", [DEPTH, D, IN_W], F32, "ExternalInput")
    convp = dt("convp", [DEPTH, 128, 2, 40], F32, "ExternalInput")
    sink_in = dt("sink", [1, DEPTH * 4], F32, "ExternalInput")
    qng = dt("qng", [DEPTH, 64], F32, "ExternalInput")
    kng = dt("kng", [DEPTH, 64], F32, "ExternalInput")
    w_out = dt("w_out", [DEPTH, D, D], F32, "ExternalInput")
    router = dt("router", [DEPTH, D, NE], F32, "ExternalInput")
    w_gate = dt("w_gate", [DEPTH, NE, D, D], F32, "ExternalInput")
    w_up = dt("w_up", [DEPTH, NE, D, D], F32, "ExternalInput")
    w_down = dt("w_down", [DEPTH, NE, D, D], F32, "ExternalInput")
    final_g = dt("final_g", [1, D], F32, "ExternalInput")
    c_identb = dt("c_identb", [128, 128], BF16, "ExternalInput")
    c_identf = dt("c_identf", [128, 128], F32, "ExternalInput")
    c_masks = dt("c_masks", [128, 2, 128], BF16, "ExternalInput")
    c_iota = dt("c_iota", [128, 258], F32, "ExternalInput")
    c_rope = dt("c_rope", [NTL, 128, 2, 384], F32, "ExternalInput")
    c_sel = dt("c_sel", [16, 16 * 128], F32, "ExternalInput")
    out = dt("out", [NS, T, D], F32, "ExternalOutput")
    dbg = {}
    if debug:
        dbg["xmid"] = dt("d_xmid", [NTT, 128, D], F32, "ExternalOutput")
        dbg["aff"] = dt("d_aff", [128, NTT, 64], F32, "ExternalOutput")
        dbg["yt"] = dt("d_yt", [8, 128, NTOK], BF16, "ExternalOutput")
        dbg["xres"] = dt("d_xres", [NTT, 128, D], F32, "ExternalOutput")
        dbg["slotg"] = dt("d_slotg", [64, 2, NTOK], F32, "ExternalOutput")
        dbg["qT"] = dt("d_qT", [128, 2, NTOK], BF16, "ExternalOutput")
        dbg["kT"] = dt("d_kT", [128, 1, NTOK], BF16, "ExternalOutput")
    MODS = dt("MODS", [DEPTH, 5, 6 * D], F32)
    XMID = dt("XMID", [NS, NTT, 128, D], F32)
    XRES = dt("XRES", [NS, NTT, 128, D], F32)
    XN2 = dt("XN2", [NS, NTT, 128, D], BF16)
    YT = dt("YT", [NS, 8, 128, NTOK], BF16)
    NSL = NS * CAP + NS * CAPC
    XSG = dt("XSG", [NE, 8, 128, NSL], BF16)
    YEX = dt("YEX", [NE, NSL, D], BF16)
    SLOTG = dt("SLOTG", [64, 2, NTOK], F32)

    with ExitStack() as es:
        S = Sync(nc, es)
        _sref.append(S)

        _uid = [0]

        def sb(name, shape, dtype, scope=es):
            _uid[0] += 1
            name = "%s_u%d" % (name, _uid[0])
            return Tl(scope.enter_context(nc.sbuf_tensor(name, list(shape), dtype)), name)

        def dma_k(eng, tile, src2d, ncols=None):
            for k in range(8):
                dst = tile[:, k, :] if ncols is None else tile[:, k, 0:ncols]
                S.dma(eng, dst, src2d[k * 128:(k + 1) * 128, :], w=[tile], add=(k > 0))

        psb = [Tl(es.enter_context(nc.psum_tensor("ps%d" % i, [128, 512], F32)), "ps%d" % i) for i in range(8)]
        PS_MM = Pool(psb[0:4])
        PS_TR = Pool(psb[4:6])
        PS_AC = Pool(psb[6:8])

        def bfv(ps):
            return ps.t.bitcast(BF16)

        identb = sb("identb", [128, 128], BF16)
        identf = sb("identf", [128, 128], F32)
        masks = sb("masks", [128, 2, 128], BF16)
        iota = sb("iota", [128, 258], F32)
        onesf = sb("onesf", [128, 128], F32)
        onesb = sb("onesb", [128, 128], BF16)
        modsT = sb("modsT", [128, DEPTH, 48, 5], F32)
        affTok = sb("affTok", [128, NTT, 64], F32)
        esink = sb("esink", [128, DEPTH * 4], F32)
        S.dma("sp", identb[:], c_identb[:, :], w=[identb])
        S.dma("sp", identf[:], c_identf[:, :], w=[identf])
        S.dma("sp", masks[:], c_masks[:, :, :], w=[masks])
        S.dma("sp", iota[:], c_iota[:, :], w=[iota])
        S.dma("sp", esink[:], sink_in[0:1, :].to_broadcast([128, DEPTH * 4]), w=[esink])
        S.op("dve", lambda e: e.memset(onesf[:], 1.0), w=[onesf])
        S.op("dve", lambda e: e.memset(onesb[:], 1.0), w=[onesb])
        S.op("dve", lambda e: e.memset(affTok[:], 0.0), w=[affTok])
        S.op("act", lambda e: e.activation(out=esink[:], in_=esink[:], func=AF.Exp), r=[esink], w=[esink])

        with ExitStack() as ps0:
            scT = sb("scT", [128, 8, 5], F32, ps0)
            adw = [sb("adw%d" % i, [128, 8, 512], F32, ps0) for i in range(2)]
            adb = [sb("adb%d" % i, [5, 512], F32, ps0) for i in range(2)]
            mrow = [sb("mrow%d" % i, [5, 512], F32, ps0) for i in range(2)]
            S.dma("sp", scT[:], cT_in[:, :, :], w=[scT])
            S.op("act", lambda e: e.activation(out=scT[:], in_=scT[:], func=AF.Silu), r=[scT], w=[scT])
            it = 0
            for l in range(DEPTH):
                for n in range(12):
                    wt, bt, mr = adw[it % 2], adb[it % 2], mrow[it % 2]
                    it += 1
                    dma_k("sp", wt, ada_w[l][:, n * 512:(n + 1) * 512])
                    S.dma("sp", bt[:], ada_b[l:l + 1, n * 512:(n + 1) * 512].to_broadcast([5, 512]), w=[bt])
                    ps = PS_MM.next()
                    for k in range(8):
                        S.op("pe", lambda e, k=k: e.matmul(ps[0:5, :], lhsT=scT[:, k, :], rhs=wt[:, k, :],
                                                          start=(k == 0), stop=(k == 7)), r=[scT, wt], w=[ps])
                    S.op("dve", lambda e: e.tensor_tensor(out=mr[:], in0=ps[0:5, :], in1=bt[:], op=ALU.add),
                         r=[ps, bt], w=[mr])
                    S.dma("sp", MODS[l][:, n * 512:(n + 1) * 512], mr[:], r=[mr])
                    pt = PS_TR.next()
                    for j in range(4):
                        S.op("pe", lambda e, j=j: e.transpose(pt[:, j * 5:(j + 1) * 5], mr[:, j * 128:(j + 1) * 128],
                                                              identf[0:5, 0:5]), r=[mr, identf], w=[pt])
                    S.op("act", lambda e: e.activation(
                        out=modsT[:, l, n * 4:(n + 1) * 4, :],
                        in_=pt[:, 0:20].rearrange("p (j s) -> p j s", s=5), func=AF.Copy), r=[pt], w=[modsT])
        S.barrier()

        try:
            chk("pro")
            def x_src(l, b, i):
                if l == 0:
                    return x_in[b, i * 128:(i + 1) * 128, :] if i < NTL else ctx_in[b, (i - NTL) * 128:(i - NTL + 1) * 128, :]
                return XRES[b, i]

            A1 = sb("A1", [128, 8, 5], F32)
            A2 = sb("A2", [128, 8, 5], F32)
            for l in range(DEPTH):
                _cur[0] = l
                with ExitStack() as p1:
                    YAG = Pool([sb("yag%d" % i, [128, 256], BF16, p1) for i in range(4)])
                    H2T = Pool([sb("h2t%d" % i, [128, 8, 128], BF16, p1) for i in range(2)])
                    YTB = [Buf("ytb%d" % i) for i in range(NS)]
                    nrep = sb("nrep", [128, 2, 8, 5], F32, p1)
                    cvp = sb("cvp", [128, 2, 40], F32, p1)
                    qg = sb("qg", [128, 64], F32, p1)
                    kg = sb("kg", [128, 64], F32, p1)
                    woutb = sb("woutb", [128, 8, D], BF16, p1)
                    rtb = sb("rtb", [128, 8, NE], BF16, p1)
                    hT = sb("hT", [128, 8, NTOK], BF16, p1)
                    xt = [sb("xt%d" % i, [128, D], F32, p1) for i in range(2)]
                    xnb = [sb("xnb%d" % i, [128, D], BF16, p1) for i in range(2)]
                    st = [sb("st%d" % i, [128, 8], F32, p1) for i in range(4)]
                    XT, XNB, ST = Pool(xt), Pool(xnb), Pool(st)
                    wch = [sb("wch%d" % i, [128, 8, 512], BF16, p1) for i in range(2)]
                    WCH = Pool(wch)
                    cw = [sb("cw%d" % i, [128, NTOK + 32], F32, p1) for i in range(3)]
                    upc = sb("upc", [128, C + 32], F32, p1)
                    ystg = [sb("ystg%d" % i, [128, NTOK], BF16, p1) for i in range(2)]
                    YSTG = Pool(ystg)
                    qT = sb("qT", [128, 2, NTOK], BF16, p1)
                    kT = sb("kT", [128, 1, NTOK], BF16, p1)
                    Vt = sb("Vt", [128, NTT, 2, 65], BF16, p1)
                    rope = [sb("rope%d" % i, [128, 2, 384], F32, p1) for i in range(2)]
                    ROPE = Pool(rope)
                    qk = [sb("qk%d" % i, [128, 384], F32, p1) for i in range(2)]
                    QK = Pool(qk)
                    qk2 = [sb("qkb%d" % i, [128, 384], F32, p1) for i in range(2)]
                    QK2 = Pool(qk2)
                    qkr = [sb("qkr%d" % i, [128, 384], BF16, p1) for i in range(2)]
                    QKR = Pool(qkr)
                    ptl = [sb("ptl%d" % i, [128, 512], BF16, p1) for i in range(4)]
                    PTL = Pool(ptl)
                    yat = [sb("yat%d" % i, [128, 256], BF16, p1) for i in range(2)]
                    YAT = Pool(yat)
                    g1b = sb("g1b", [128, D], F32, p1)
                    YTL = WCH
                    tmpf = [sb("tmpf%d" % i, [128, 512], F32, p1) for i in range(6)]
                    TMPF = Pool(tmpf)

                    S.dma("sp", nrep[:, 0], n1rep[l], w=[nrep])
                    S.dma("sp", nrep[:, 1], n2rep[l], w=[nrep])
                    S.dma("sp", cvp[:], convp[l], w=[cvp])
                    S.dma("sp", qg[:], qng[l:l + 1, :].to_broadcast([128, 64]), w=[qg])
                    S.dma("sp", kg[:], kng[l:l + 1, :].to_broadcast([128, 64]), w=[kg])
                    dma_k("pool", woutb, w_out[l])
                    S.dma("pool", rtb[:], router[l].rearrange("(k p) f -> p k f", p=128), w=[rtb])
                    S.op("dve", lambda e: e.scalar_tensor_tensor(out=A1[:], in0=modsT[:, l, 8:16, :], scalar=1.0,
                                                                 in1=nrep[:, 0], op0=ALU.add, op1=ALU.mult),
                         r=[modsT, nrep], w=[A1])
                    S.op("dve", lambda e: e.scalar_tensor_tensor(out=A2[:], in0=modsT[:, l, 32:40, :], scalar=1.0,
                                                                 in1=nrep[:, 1], op0=ALU.add, op1=ALU.mult),
                         r=[modsT, nrep], w=[A2])
                    S.op("dve", lambda e: e.memset(Vt[:], 1.0), w=[Vt])

                    def norm_to_T(src_tile, dstT, dcols, A, Bofs, j, xn_keep=None):
                        ss = ST.next()
                        xn = xn_keep if xn_keep is not None else XNB.next()
                        S.op("pool", lambda e: e.memset(ss[:], 0.0), w=[ss])
                        S.op("act", lambda e: e.activation(out=xn[:], in_=src_tile[:], func=AF.Square,
                                                           accum_out=ss[:, 0:1]), r=[src_tile], w=[xn, ss])
                        S.op("dve", lambda e: e.tensor_scalar(out=ss[:, 1:2], in0=ss[:, 0:1], scalar1=1.0 / D, scalar2=EPS,
                                                              op0=ALU.mult, op1=ALU.add), r=[ss], w=[ss])
                        S.op("dve", lambda e: e.reciprocal(out=ss[:, 2:3], in_=ss[:, 1:2]), r=[ss], w=[ss])
                        S.op("act", lambda e: e.activation(out=ss[:, 3:4], in_=ss[:, 2:3], func=AF.Sqrt), r=[ss], w=[ss])
                        S.op("act", lambda e: e.activation(out=xn[:], in_=src_tile[:], func=AF.Copy, scale=ss[:, 3:4]),
                             r=[src_tile, ss], w=[xn])
                        pt = PS_TR.next()
                        ptb = bfv(pt)
                        for c in range(8):
                            S.op("pe", lambda e, c=c: e.transpose(ptb[:, c * 128:(c + 1) * 128], xn[:, c * 128:(c + 1) * 128],
                                                                  identb[:]), r=[xn, identb], w=[pt])
                        for c in range(8):
                            if c % 2 == 0:
                                S.op("act", lambda e, c=c: e.activation(
                                    out=dstT[:, c, dcols], in_=ptb[:, c * 128:(c + 1) * 128], func=AF.Identity,
                                    scale=A[:, c, j:j + 1], bias=modsT[:, l, Bofs + c, j:j + 1]), r=[pt, A, modsT], w=[dstT])
                            else:
                                S.op("dve", lambda e, c=c: e.tensor_scalar(
                                    out=dstT[:, c, dcols], in0=ptb[:, c * 128:(c + 1) * 128],
                                    scalar1=A[:, c, j:j + 1], scalar2=modsT[:, l, Bofs + c, j:j + 1],
                                    op0=ALU.mult, op1=ALU.add), r=[pt, A, modsT], w=[dstT])
                        return xn

                    TCH = [(0, 512), (512, 512), (1024, 512), (1536, 512), (2048, 256)]
                    SEGS = [(0, T), (T, C)]

                    def colp(t):
                        return 16 + t if t < T else 16 + t + 0

                    for b in range(NS):
                        for i in range(NTT):
                            xtile = XT.next()
                            S.dma("sp", xtile[:], x_src(l, b, i), w=[xtile])
                            norm_to_T(xtile, hT, slice(i * 128, (i + 1) * 128), A1, 0, b if i < NTL else 4)

                        chk("p1_norm")
                        def inproj_fm(fc, consume):
                            wt = WCH.next()
                            dma_k("pool", wt, w_in[l][:, fc * 128:(fc + 1) * 128], 128)
                            for (t0, n) in TCH:
                                ps = PS_MM.next()
                                for k in range(8):
                                    S.op("pe", lambda e, k=k: e.matmul(ps[:, 0:n], lhsT=wt[:, k, 0:128], rhs=hT[:, k, t0:t0 + n],
                                                                      start=(k == 0), stop=(k == 7)), r=[wt, hT], w=[ps])
                                consume(ps, t0, n)

                        for h in range(2):
                            prod, cv = cw[0], cw[1]
                            S.op("pool", lambda e: e.memset(prod[:, 0:1], 0.0), w=[prod])
                            S.op("pool", lambda e: e.memset(prod[:, T + 1:T + 3], 0.0), w=[prod])
                            S.op("pool", lambda e: e.memset(prod[:, T + 3 + C:T + 4 + C], 0.0), w=[prod])

                            def offA(t0):
                                return 1 + t0 if t0 < T else T + 3 + (t0 - T)
                            inproj_fm(2 + h, lambda ps, t0, n: S.op(
                                "act", lambda e: e.activation(out=prod[:, offA(t0):offA(t0) + n], in_=ps[:, 0:n], func=AF.Copy),
                                r=[ps], w=[prod]))
                            inproj_fm(4 + h, lambda ps, t0, n: S.op(
                                "dve", lambda e: e.tensor_tensor(out=prod[:, offA(t0):offA(t0) + n], in0=ps[:, 0:n],
                                                                 in1=prod[:, offA(t0):offA(t0) + n], op=ALU.mult), r=[ps, prod], w=[prod]))
                            for (s0, sn) in SEGS:
                                o0 = offA(s0)
                                S.op("dve", lambda e: e.tensor_scalar(out=cv[:, s0:s0 + sn], in0=prod[:, o0 - 1:o0 - 1 + sn],
                                                                      scalar1=cvp[:, h, 0:1], scalar2=None, op0=ALU.mult),
                                     r=[prod, cvp], w=[cv])
                                for kk in (1, 2):
                                    S.op("dve", lambda e, kk=kk: e.scalar_tensor_tensor(
                                        out=cv[:, s0:s0 + sn], in0=prod[:, o0 - 1 + kk:o0 - 1 + kk + sn], scalar=cvp[:, h, kk:kk + 1],
                                        in1=cv[:, s0:s0 + sn], op0=ALU.mult, op1=ALU.add), r=[prod, cvp, cv], w=[cv])
                            ys = YSTG.next()
                            inproj_fm(0 + h, lambda ps, t0, n: S.op(
                                "dve", lambda e: e.tensor_tensor(out=ys[:, t0:t0 + n], in0=ps[:, 0:n], in1=cv[:, t0:t0 + n],
                                                                 op=ALU.mult), r=[ps, cv], w=[ys]))
                            S.dma("sp", YT[b, h], ys[:], r=[ys], w=[YTB[b]], add=True)
                        chk("p1_A")
                        ucs = [cw[1], cw[2]]
                        for h in range(2):
                            upad, uc = cw[0], ucs[h]
                            S.op("pool", lambda e: e.memset(upad[:, 0:15], 0.0), w=[upad])
                            S.op("pool", lambda e: e.memset(upad[:, 15 + T:30 + T], 0.0), w=[upad])
                            S.op("pool", lambda e: e.memset(upc[:, 0:15], 0.0), w=[upc])
                            S.op("pool", lambda e: e.memset(upc[:, 15 + C:30 + C], 0.0), w=[upc])

                            def dstB(t0, n):
                                return (upad, upad[:, 15 + t0:15 + t0 + n]) if t0 < T else (upc, upc[:, 15:15 + n])

                            def consB1(ps, t0, n):
                                tl, ap = dstB(t0, n)
                                S.op("act", lambda e: e.activation(out=ap, in_=ps[:, 0:n], func=AF.Sigmoid), r=[ps], w=[tl])

                            def consB2(ps, t0, n):
                                tl, ap = dstB(t0, n)
                                S.op("dve", lambda e: e.tensor_tensor(out=ap, in0=ps[:, 0:n], in1=ap, op=ALU.mult), r=[ps, tl], w=[tl])
                            inproj_fm(8 + h, consB1)
                            inproj_fm(6 + h, consB2)
                            for (src, s0, sn, eng) in ((upad, 0, T, "dve"), (upc, T, C, "dve")):
                                S.op(eng, lambda e: e.tensor_scalar(out=uc[:, s0:s0 + sn], in0=src[:, 0:sn],
                                                                    scalar1=cvp[:, h, 3:4], scalar2=cvp[:, h, 34:35],
                                                                    op0=ALU.mult, op1=ALU.add), r=[src, cvp], w=[uc])
                                for kk in range(1, 31):
                                    S.op(eng, lambda e, kk=kk: e.scalar_tensor_tensor(
                                        out=uc[:, s0:s0 + sn], in0=src[:, kk:kk + sn], scalar=cvp[:, h, 3 + kk:4 + kk],
                                        in1=uc[:, s0:s0 + sn], op0=ALU.mult, op1=ALU.add), r=[src, cvp, uc], w=[uc])
                        ysb = [YSTG.next(), YSTG.next()]
                        for (t0, n) in TCH:
                            p1s, p2s = PS_MM.next(), PS_MM.next()
                            for h in range(2):
                                S.op("pe", lambda e, h=h: e.matmul(p1s[:, 0:n], lhsT=onesf[:], rhs=ucs[h][:, t0:t0 + n],
                                                                  start=(h == 0), stop=(h == 1)), r=[onesf, ucs[h]], w=[p1s])
                            for h in range(2):
                                sqt = TMPF.next()
                                S.op("act", lambda e, h=h: e.activation(out=sqt[:, 0:n], in_=ucs[h][:, t0:t0 + n], func=AF.Square),
                                     r=[ucs[h]], w=[sqt])
                                S.op("pe", lambda e, h=h: e.matmul(p2s[:, 0:n], lhsT=onesf[:], rhs=sqt[:, 0:n],
                                                                  start=(h == 0), stop=(h == 1)), r=[onesf, sqt], w=[p2s])
                            mean, var, dd = TMPF.next(), TMPF.next(), TMPF.next()
                            S.op("act", lambda e: e.activation(out=mean[:, 0:n], in_=p1s[:, 0:n], func=AF.Copy, scale=1.0 / 256),
                                 r=[p1s], w=[mean])
                            S.op("dve", lambda e: e.tensor_tensor(out=var[:, 0:n], in0=mean[:, 0:n], in1=mean[:, 0:n], op=ALU.mult),
                                 r=[mean], w=[var])
                            S.op("dve", lambda e: e.scalar_tensor_tensor(out=var[:, 0:n], in0=p2s[:, 0:n], scalar=1.0 / 256,
                                                                         in1=var[:, 0:n], op0=ALU.mult, op1=ALU.subtract),
                                 r=[p2s, var], w=[var])
                            S.op("dve", lambda e: e.tensor_scalar(out=var[:, 0:n], in0=var[:, 0:n], scalar1=EPS, scalar2=None,
                                                                  op0=ALU.add), r=[var], w=[var])
                            S.op("dve", lambda e: e.reciprocal(out=var[:, 0:n], in_=var[:, 0:n]), r=[var], w=[var])
                            S.op("act", lambda e: e.activation(out=var[:, 0:n], in_=var[:, 0:n], func=AF.Sqrt), r=[var], w=[var])
                            for h in range(2):
                                S.op("pool", lambda e, h=h: e.tensor_tensor(out=dd[:, 0:n], in0=ucs[h][:, t0:t0 + n], in1=mean[:, 0:n],
                                                                            op=ALU.subtract), r=[ucs[h], mean], w=[dd])
                                S.op("pool", lambda e: e.tensor_tensor(out=dd[:, 0:n], in0=dd[:, 0:n], in1=var[:, 0:n], op=ALU.mult),
                                     r=[dd, var], w=[dd])
                                S.op("act", lambda e, h=h: e.activation(out=ysb[h][:, t0:t0 + n], in_=dd[:, 0:n], func=AF.Silu,
                                                                        scale=cvp[:, h, 35:36], bias=cvp[:, h, 36:37]),
                                     r=[dd, cvp], w=[ysb[h]])
                        for h in range(2):
                            S.dma("sp", YT[b, 2 + h], ysb[h][:], r=[ysb[h]], w=[YTB[b]], add=True)

                        chk("p1_B")
                        for grp in range(2):
                            wt = WCH.next()
                            c0 = 1280 + grp * 512
                            dma_k("pool", wt, w_in[l][:, c0:c0 + 512])
                            for i in range(NTT):
                                ps = PS_MM.next()
                                for k in range(8):
                                    S.op("pe", lambda e, k=k: e.matmul(ps[:, :], lhsT=hT[:, k, i * 128:(i + 1) * 128], rhs=wt[:, k, :],
                                                                      start=(k == 0), stop=(k == 7)), r=[hT, wt], w=[ps])
                                S.op("act", lambda e: e.activation(out=Vt[:, i, :, 0:64],
                                                                   in_=ps[:, 384:512].rearrange("p (a d) -> p a d", d=64),
                                                                   func=AF.Copy), r=[ps], w=[Vt])
                                q1 = QK.next()
                                S.op("act", lambda e: e.activation(out=q1[:], in_=ps[:, 0:384], func=AF.Copy), r=[ps], w=[q1])
                                if grp == 1:
                                    sq = QK2.next()
                                    ss = ST.next()
                                    S.op("dve", lambda e: e.tensor_tensor(out=sq[:], in0=q1[:], in1=q1[:], op=ALU.mult), r=[q1], w=[sq])
                                    S.op("dve", lambda e: e.tensor_reduce(out=ss[:, 0:6], in_=sq[:].rearrange("p (a d) -> p a d", d=64),
                                                                          axis=AX.X, op=ALU.add), r=[sq], w=[ss])
                                    S.op("dve", lambda e: e.tensor_scalar(out=ss[:, 0:6], in0=ss[:, 0:6], scalar1=1.0 / 64, scalar2=EPS,
                                                                          op0=ALU.mult, op1=ALU.add), r=[ss], w=[ss])
                                    S.op("dve", lambda e: e.reciprocal(out=ss[:, 0:6], in_=ss[:, 0:6]), r=[ss], w=[ss])
                                    S.op("act", lambda e: e.activation(out=ss[:, 0:6], in_=ss[:, 0:6], func=AF.Sqrt), r=[ss], w=[ss])
                                    for hh in range(6):
                                        gt = qg if hh < 4 else kg
                                        S.op("dve", lambda e, hh=hh, gt=gt: e.scalar_tensor_tensor(
                                            out=q1[:, hh * 64:(hh + 1) * 64], in0=q1[:, hh * 64:(hh + 1) * 64], scalar=ss[:, hh:hh + 1],
                                            in1=gt[:], op0=ALU.mult, op1=ALU.mult), r=[q1, ss, gt], w=[q1])
                                qr = QKR.next()
                                if i < NTL:
                                    rp = ROPE.next()
                                    S.dma("sp", rp[:], c_rope[i], w=[rp])
                                    t1, t2 = QK2.next(), QK2.next()
                                    v4 = lambda ap: ap.rearrange("p (a j q) -> p a j q", j=2, q=16)
                                    S.op("dve", lambda e: e.tensor_tensor(out=t1[:], in0=q1[:], in1=rp[:, 0, :], op=ALU.mult),
                                         r=[q1, rp], w=[t1])
                                    S.op("pool", lambda e: e.tensor_tensor(out=v4(t2[:])[:, :, 0, :], in0=v4(q1[:])[:, :, 1, :],
                                                                           in1=v4(rp[:, 1, :])[:, :, 0, :], op=ALU.mult), r=[q1, rp], w=[t2])
                                    S.op("pool", lambda e: e.tensor_tensor(out=v4(t2[:])[:, :, 1, :], in0=v4(q1[:])[:, :, 0, :],
                                                                           in1=v4(rp[:, 1, :])[:, :, 1, :], op=ALU.mult), r=[q1, rp], w=[t2])
                                    pq_o = lambda ap: ap[:, 0:256].rearrange("p (g k d) -> p k g d", g=2, k=2)
                                    pq_i = lambda ap: ap[:, 0:256].rearrange("p (k g d) -> p k g d", g=2, k=2)
                                    S.op("dve", lambda e: e.tensor_tensor(out=pq_o(qr[:]), in0=pq_i(t1[:]), in1=pq_i(t2[:]), op=ALU.add),
                                         r=[t1, t2], w=[qr])
                                    S.op("dve", lambda e: e.tensor_tensor(out=qr[:, 256:384], in0=t1[:, 256:384], in1=t2[:, 256:384],
                                                                          op=ALU.add), r=[t1, t2], w=[qr])
                                else:
                                    pq_o = lambda ap: ap[:, 0:256].rearrange("p (g k d) -> p k g d", g=2, k=2)
                                    pq_i = lambda ap: ap[:, 0:256].rearrange("p (k g d) -> p k g d", g=2, k=2)
                                    S.op("dve", lambda e: e.tensor_copy(out=pq_o(qr[:]), in_=pq_i(q1[:])), r=[q1], w=[qr])
                                    S.op("dve", lambda e: e.tensor_copy(out=qr[:, 256:384], in_=q1[:, 256:384]), r=[q1], w=[qr])
                                pt = PS_TR.next()
                                ptb = bfv(pt)
                                for hh in range(3):
                                    S.op("pe", lambda e, hh=hh: e.transpose(ptb[:, hh * 128:(hh + 1) * 128], qr[:, hh * 128:(hh + 1) * 128],
                                                                            identb[:]), r=[qr, identb], w=[pt])
                                S.op("act", lambda e: e.activation(out=qT[:, :, i * 128:(i + 1) * 128],
                                                                   in_=ptb[:, 0:256].rearrange("p (a t) -> p a t", t=128),
                                                                   func=AF.Copy), r=[pt], w=[qT])
                                S.op("dve", lambda e: e.tensor_copy(out=kT[:, 0, i * 128:(i + 1) * 128], in_=ptb[:, 256:384]),
                                     r=[pt], w=[kT])
                            if debug and grp == 1 and b == 0 and l == 0:
                                S.dma("sp", dbg["qT"][:, :, :], qT[:], r=[qT])
                                S.dma("sp", dbg["kT"][:, :, :], kT[:], r=[kT])
                            if grp == 0:
                                chk("p1_C0")
                            else:
                                chk("p1_D0")
                            yts = [YSTG.next(), YSTG.next()]

                            def normalize_head(pa, co, h, ya):
                                den = ST.next()
                                if grp == 0:
                                    S.op("act", lambda e: e.activation(out=den[:, 0:1], in_=pa[:, co + 64:co + 65], func=AF.Identity,
                                                                       bias=esink[:, l * 4 + h:l * 4 + h + 1]),
                                         r=[pa, esink], w=[den])
                                    S.op("dve", lambda e: e.reciprocal(out=den[:, 1:2], in_=den[:, 0:1]), r=[den], w=[den])
                                else:
                                    S.op("act", lambda e: e.activation(out=den[:, 0:1], in_=pa[:, co + 64:co + 65], func=AF.Copy),
                                         r=[pa], w=[den])
                                    S.op("dve", lambda e: e.reciprocal(out=den[:, 1:2], in_=den[:, 0:1]), r=[den], w=[den])
                                S.op("act", lambda e: e.activation(out=ya[:, h * 64:(h + 1) * 64], in_=pa[:, co:co + 64],
                                                                   func=AF.Copy, scale=den[:, 1:2]), r=[pa, den], w=[ya])

                            def finish_ya(ya, qb):
                                pt = PS_TR.next()
                                ptb = bfv(pt)
                                for c in range(2):
                                    S.op("pe", lambda e, c=c: e.transpose(ptb[:, c * 128:(c + 1) * 128], ya[:, c * 128:(c + 1) * 128],
                                                                          identb[:]), r=[ya, identb], w=[pt])
                                for c in range(2):
                                    S.op("dve", lambda e, c=c: e.tensor_copy(out=yts[c][:, qb * 128:(qb + 1) * 128],
                                                                             in_=ptb[:, c * 128:(c + 1) * 128]), r=[pt], w=[yts[c]])

                            items = []
                            for qb in range(NTT):
                                if qb >= NTL:
                                    kts = [NTL, NTL + 1]
                                elif grp == 0:
                                    kts = [m for m in (qb - 1, qb, qb + 1) if 0 <= m < NTL] + [NTL, NTL + 1]
                                else:
                                    kts = list(range(NTT))
                                nk = len(kts)
                                for h in range(4):
                                    for g0 in range(0, nk, 4):
                                        items.append((qb, h, g0, kts[g0:g0 + 4], nk))

                            def emit_st(item):
                                qb, h, g0, grpk, nk = item
                                kh = h // 2
                                ps = PS_MM.next()
                                for ki, m in enumerate(grpk):
                                    S.op("pe", lambda e, m=m, ki=ki: e.matmul(
                                        ps[:, ki * 128:(ki + 1) * 128], lhsT=kT[kh * 64:(kh + 1) * 64, 0, m * 128:(m + 1) * 128],
                                        rhs=qT[kh * 64:(kh + 1) * 64, h % 2, qb * 128:(qb + 1) * 128], start=True, stop=True),
                                        r=[kT, qT], w=[ps])
                                pt_ = PTL.next()
                                nn_ = len(grpk) * 128
                                S.op("act", lambda e: e.activation(out=pt_[:, 0:nn_], in_=ps[:, 0:nn_], func=AF.Exp, scale=0.125),
                                     r=[ps], w=[pt_])
                                if grp == 0 and qb < NTL:
                                    for ki, m in enumerate(grpk):
                                        if (m == qb - 1 or m == qb + 1) and m < NTL:
                                            mi = 1 if m == qb - 1 else 0
                                            S.op("pool", lambda e, ki=ki, mi=mi: e.tensor_tensor(
                                                out=pt_[:, ki * 128:(ki + 1) * 128], in0=pt_[:, ki * 128:(ki + 1) * 128],
                                                in1=masks[:, mi, :], op=ALU.mult), r=[pt_, masks], w=[pt_])
                                return pt_

                            cur = {}

                            def emit_pv(item, pt_):
                                qb, h, g0, grpk, nk = item
                                kh = h // 2
                                if h == 0 and g0 == 0:
                                    cur["pacc"] = PS_AC.next()
                                    cur["ya"] = YAT.next()
                                pacc = cur["pacc"]
                                for ki, m in enumerate(grpk):
                                    S.op("pe", lambda e, ki=ki, m=m: e.matmul(
                                        pacc[:, h * 65:(h + 1) * 65], lhsT=pt_[:, ki * 128:(ki + 1) * 128], rhs=Vt[:, m, kh, :],
                                        start=(g0 + ki == 0), stop=(g0 + ki == nk - 1)), r=[pt_, Vt], w=[pacc])
                                if h == 3 and g0 + len(grpk) == nk:
                                    for hh in range(4):
                                        normalize_head(pacc, hh * 65, hh, cur["ya"])
                                    finish_ya(cur["ya"], qb)

                            pend = emit_st(items[0])
                            for k_ in range(len(items)):
                                nxt = emit_st(items[k_ + 1]) if k_ + 1 < len(items) else None
                                emit_pv(items[k_], pend)
                                pend = nxt
                            if grp == 0:
                                chk("p1_C")
                            for c in range(2):
                                S.dma("sp", YT[b, 4 + grp * 2 + c], yts[c][:], r=[yts[c]], w=[YTB[b]], add=True)

                        chk("p1_CD")
                        S.dma("sp", g1b[:], MODS[l][b:b + 1, 2048:3072].to_broadcast([128, D]), w=[g1b])
                        for (t0, n) in TCH:
                            if t0 >= T:
                                S.dma("sp", g1b[:], MODS[l][4:5, 2048:3072].to_broadcast([128, D]), w=[g1b])
                            yl = YTL.next()
                            for c in range(8):
                                S.dma("sp", yl[:, c, 0:n], YT[b, c][:, t0:t0 + n], r=[YTB[b]], w=[yl], add=(c > 0))
                            for i in range(t0 // 128, (t0 + n) // 128):
                                tt = i * 128 - t0
                                j = b if i < NTL else 4
                                gt = g1b
                                xtile = XT.next()
                                S.dma("sp", xtile[:], x_src(l, b, i), w=[xtile])
                                for half in range(2):
                                    ps = PS_MM.next()
                                    for k in range(8):
                                        S.op("pe", lambda e, k=k: e.matmul(ps[:, :], lhsT=yl[:, k, tt:tt + 128],
                                                                          rhs=woutb[:, k, half * 512:(half + 1) * 512],
                                                                          start=(k == 0), stop=(k == 7)), r=[yl, woutb], w=[ps])
                                    tm = TMPF.next()
                                    S.op("dve", lambda e: e.tensor_tensor(out=tm[:], in0=ps[:, :], in1=gt[:, half * 512:(half + 1) * 512],
                                                                          op=ALU.mult), r=[ps, gt], w=[tm])
                                    S.op("pool", lambda e: e.tensor_tensor(out=xtile[:, half * 512:(half + 1) * 512],
                                                                           in0=xtile[:, half * 512:(half + 1) * 512], in1=tm[:],
                                                                           op=ALU.add), r=[xtile, tm], w=[xtile])
                                S.dma("sp", XMID[b, i], xtile[:], r=[xtile])
                                if debug and b == 0 and l == 0:
                                    S.dma("sp", dbg["xmid"][i], xtile[:], r=[xtile])
                                xn = XNB.next()
                                h2 = H2T.next()
                                norm_to_T(xtile, h2, slice(0, 128), A2, 24, j, xn_keep=xn)
                                S.dma("sp", XN2[b, i], xn[:], r=[xn])
                                ps = PS_MM.next()
                                for k in range(8):
                                    S.op("pe", lambda e, k=k: e.matmul(ps[:, 0:NE], lhsT=h2[:, k, :], rhs=rtb[:, k, :],
                                                                      start=(k == 0), stop=(k == 7)), r=[h2, rtb], w=[ps])
                                ss = ST.next()
                                ex = TMPF.next()
                                S.op("pool", lambda e: e.memset(ss[:], 0.0), w=[ss])
                                S.op("act", lambda e: e.activation(out=ex[:, 0:NE], in_=ps[:, 0:NE], func=AF.Exp,
                                                                   accum_out=ss[:, 0:1]), r=[ps], w=[ex, ss])
                                S.op("dve", lambda e: e.reciprocal(out=ss[:, 1:2], in_=ss[:, 0:1]), r=[ss], w=[ss])
                                S.op("dve", lambda e: e.tensor_scalar(out=affTok[:, i, b * 16:(b + 1) * 16], in0=ex[:, 0:NE],
                                                                      scalar1=ss[:, 1:2], scalar2=None, op0=ALU.mult),
                                     r=[ex, ss], w=[affTok])
                    if debug and l == 0:
                        S.dma("sp", dbg["aff"][:, :, :], affTok[:], r=[affTok])
                        S.barrier()
                        for c in range(8):
                            t_ = YSTG.next()
                            S.dma("sp", t_[:], YT[0, c], w=[t_])
                            S.dma("sp", dbg["yt"][c], t_[:], r=[t_])
                    S.barrier()

                chk("p1")
                with ExitStack() as m1:
                    affT = sb("affT", [64, NTOK], F32, m1)
                    work = sb("work", [64, NTOK], F32, m1)
                    Gx = sb("Gx", [64, NTOK], F32, m1)
                    gtok = sb("gtok", [128, NTT, 64], F32, m1)
                    maskb = sb("maskb", [128, NTT, 64], BF16, m1)
                    slotTok = sb("slotTok", [128, NTT, 64], F32, m1)
                    MX = Pool([sb("mx%d" % i, [64, 8], F32, m1) for i in range(2)])
                    xn2 = sb("xn2", [128, NTT, D], BF16, m1)
                    PB = Pool([sb("pb%d" % i, [128, 256], BF16, m1) for i in range(NTL)])
                    PBC = Pool([sb("pbc%d" % i, [128, 32], BF16, m1) for i in range(2)])
                    XSTG = Pool([sb("xstg%d" % i, [128, 8, 256], BF16, m1) for i in range(2)])
                    XSTC = Pool([sb("xstc%d" % i, [128, 8, 32], BF16, m1) for i in range(2)])

                    def tr_in(dst, src_fn, rows_out, nt, ident_n, rbufs):
                        pass

                    for i0 in range(0, NTT, 4):
                        pt = PS_TR.next()
                        nn = min(4, NTT - i0)
                        for q in range(nn):
                            S.op("pe", lambda e, q=q: e.transpose(pt[0:64, q * 128:(q + 1) * 128], affTok[:, i0 + q, :], identf[:]),
                                 r=[affTok, identf], w=[pt])
                        S.op("act", lambda e: e.activation(out=affT[:, i0 * 128:(i0 + nn) * 128], in_=pt[0:64, 0:nn * 128], func=AF.Copy),
                             r=[pt], w=[affT])
                    S.op("dve", lambda e: e.tensor_copy(out=work[:], in_=affT[:]), r=[affT], w=[work])
                    for (s0, sn, rounds) in ((0, T, CAP // 8), (T, C, CAPC // 8)):
                        for r_ in range(rounds):
                            m8 = MX.next()
                            S.op("dve", lambda e: e.max(out=m8[:], in_=work[:, s0:s0 + sn]), r=[work], w=[m8])
                            S.op("dve", lambda e: e.match_replace(out=work[:, s0:s0 + sn], in_to_replace=m8[:],
                                                                  in_values=work[:, s0:s0 + sn], imm_value=0.0), r=[work, m8], w=[work])
                    S.op("dve", lambda e: e.tensor_tensor(out=Gx[:], in0=affT[:], in1=work[:], op=ALU.subtract), r=[affT, work], w=[Gx])
                    S.dma("sp", SLOTG[:, 1, :], Gx[:], r=[Gx])
                    for i0 in range(0, NTT, 4):
                        pt = PS_TR.next()
                        nn = min(4, NTT - i0)
                        for q in range(nn):
                            S.op("pe", lambda e, q=q: e.transpose(pt[:, q * 64:(q + 1) * 64], Gx[:, (i0 + q) * 128:(i0 + q + 1) * 128],
                                                                  identf[0:64, 0:64]), r=[Gx, identf], w=[pt])
                        S.op("act", lambda e: e.activation(out=gtok[:, i0:i0 + nn, :],
                                                           in_=pt[:, 0:nn * 64].rearrange("p (a c) -> p a c", c=64), func=AF.Copy),
                             r=[pt], w=[gtok])
                    S.op("dve", lambda e: e.tensor_scalar(out=maskb[:], in0=gtok[:], scalar1=0.0, scalar2=None, op0=ALU.is_gt),
                         r=[gtok], w=[maskb])
                    for i in range(NTT):
                        prev = list(range(0, i)) if i < NTL else list(range(NTL, i))
                        ps = PS_MM.next()
                        for ii, ip in enumerate(prev):
                            S.op("pe", lambda e, ip=ip, ii=ii: e.matmul(ps[:, 0:64], lhsT=onesb[:], rhs=maskb[:, ip, :],
                                                                        start=(ii == 0), stop=False), r=[onesb, maskb], w=[ps])
                        S.op("pe", lambda e: e.matmul(ps[:, 0:64], lhsT=masks[:, 0, :], rhs=maskb[:, i, :],
                                                      start=(len(prev) == 0), stop=True), r=[masks, maskb], w=[ps])
                        S.op("dve", lambda e: e.tensor_tensor(out=slotTok[:, i, :], in0=ps[:, 0:64], in1=maskb[:, i, :], op=ALU.mult),
                             r=[ps, maskb], w=[slotTok])
                    for i0 in range(0, NTT, 4):
                        pt = PS_TR.next()
                        nn = min(4, NTT - i0)
                        for q in range(nn):
                            S.op("pe", lambda e, q=q: e.transpose(pt[0:64, q * 128:(q + 1) * 128], slotTok[:, i0 + q, :], identf[:]),
                                 r=[slotTok, identf], w=[pt])
                        S.op("act", lambda e: e.activation(out=work[:, i0 * 128:(i0 + nn) * 128], in_=pt[0:64, 0:nn * 128], func=AF.Copy),
                             r=[pt], w=[work])
                    S.dma("sp", SLOTG[:, 0, :], work[:], r=[work])
                    if debug and l == 0:
                        S.dma("sp", dbg["slotg"][:, 0, :], work[:], r=[work])
                        S.dma("sp", dbg["slotg"][:, 1, :], Gx[:], r=[Gx])
                    chk("m1_topk")
                    for b in range(NS):
                        for i in range(NTT):
                            S.dma("sp", xn2[:, i, :], XN2[b, i], w=[xn2], add=(i > 0))
                        for ex_ in range(NE):
                            col = b * 16 + ex_
                            Ps = []
                            for i in range(NTL):
                                P = PB.next()
                                S.op("dve" if i % 2 == 0 else "pool", lambda e: e.tensor_scalar(
                                    out=P[:], in0=iota[:, 0:256], scalar1=slotTok[:, i, col:col + 1], scalar2=None, op0=ALU.is_equal),
                                    r=[iota, slotTok], w=[P])
                                Ps.append(P)
                            stg = XSTG.next()
                            for cg in range(4):
                                pss = [PS_MM.next() for _ in range(2)]
                                for i in range(NTL):
                                    for cc in range(2):
                                        c = cg * 2 + cc
                                        S.op("pe", lambda e, c=c, cc=cc: e.matmul(pss[cc][:, 0:256],
                                                                                  lhsT=xn2[:, i, c * 128:(c + 1) * 128], rhs=Ps[i][:],
                                                                                  start=(i == 0), stop=(i == NTL - 1)),
                                             r=[xn2, Ps[i]], w=[pss[cc]])
                                for cc in range(2):
                                    c = cg * 2 + cc
                                    src = pss[cc][:, 0:256]
                                    if c % 2 == 0:
                                        S.op("act", lambda e, c=c, src=src: e.activation(
                                            out=stg[:, c, :], in_=src, func=AF.Identity, scale=A2[:, c, b:b + 1],
                                            bias=modsT[:, l, 24 + c, b:b + 1]), r=[pss[cc], A2, modsT], w=[stg])
                                    else:
                                        S.op("dve", lambda e, c=c, src=src: e.tensor_scalar(
                                            out=stg[:, c, :], in0=src, scalar1=A2[:, c, b:b + 1], scalar2=modsT[:, l, 24 + c, b:b + 1],
                                            op0=ALU.mult, op1=ALU.add), r=[pss[cc], A2, modsT], w=[stg])
                            for c in range(8):
                                S.dma("sp", XSG[ex_, c][:, b * CAP:(b + 1) * CAP], stg[:, c, :], r=[stg])
                            psc = PS_AC.next()
                            Pcs = []
                            for i in range(NTL, NTT):
                                Pc = PBC.next()
                                S.op("pool", lambda e: e.tensor_scalar(out=Pc[:], in0=iota[:, 0:32], scalar1=slotTok[:, i, col:col + 1],
                                                                       scalar2=None, op0=ALU.is_equal), r=[iota, slotTok], w=[Pc])
                                Pcs.append(Pc)
                            for c in range(8):
                                for ii, i in enumerate(range(NTL, NTT)):
                                    S.op("pe", lambda e, c=c, i=i, ii=ii: e.matmul(psc[:, c * 32:(c + 1) * 32],
                                                                                   lhsT=xn2[:, i, c * 128:(c + 1) * 128], rhs=Pcs[ii][:],
                                                                                   start=(i == NTL), stop=(i == NTT - 1)),
                                         r=[xn2, Pcs[ii]], w=[psc])
                            stc = XSTC.next()
                            for c in range(8):
                                S.op("dve", lambda e, c=c: e.tensor_scalar(
                                    out=stc[:, c, :], in0=psc[:, c * 32:(c + 1) * 32], scalar1=A2[:, c, 4:5],
                                    scalar2=modsT[:, l, 24 + c, 4:5], op0=ALU.mult, op1=ALU.add), r=[psc, A2, modsT], w=[stc])
                            o0 = NS * CAP + b * CAPC
                            S.dma("sp", XSG[ex_][:, :, o0:o0 + CAPC].rearrange("c p s -> p c s"), stc[:], r=[stc])
                    S.barrier()

                chk("m1")
                with ExitStack() as m2:
                    WG = [sb("wg%d" % i, [128, 8, D], BF16, m2) for i in range(2)]
                    WU = [sb("wu%d" % i, [128, 8, D], BF16, m2) for i in range(2)]
                    WD = [sb("wd%d" % i, [128, 8, D], BF16, m2) for i in range(2)]
                    XS = [sb("xs%d" % i, [128, 8, NSL], BF16, m2) for i in range(2)]
                    actT = sb("actT", [128, 8, NSL], BF16, m2)
                    SA = Pool([sb("sa%d" % i, [128, 512], F32, m2) for i in range(3)])
                    YS = Pool([sb("ys%d" % i, [128, D], BF16, m2) for i in range(2)])
                    SCH = [(s0, min(512, NSL - s0)) for s0 in range(0, NSL, 512)]
                    STL = [(s0, min(128, NSL - s0)) for s0 in range(0, NSL, 128)]
                    for ex_ in range(NE):
                        wg, wu, wd, xs = WG[ex_ % 2], WU[ex_ % 2], WD[ex_ % 2], XS[ex_ % 2]
                        dma_k("pool", wg, w_gate[l, ex_])
                        dma_k("pool", wu, w_up[l, ex_])
                        dma_k("pool", wd, w_down[l, ex_])
                        for c in range(8):
                            S.dma("sp", xs[:, c, :], XSG[ex_, c], w=[xs], add=(c > 0))
                        for (s0, n) in SCH:
                            for fc in range(8):
                                pA, pU = PS_MM.next(), PS_MM.next()
                                for k in range(8):
                                    S.op("pe", lambda e, k=k: e.matmul(pA[:, 0:n], lhsT=wg[:, k, fc * 128:(fc + 1) * 128],
                                                                      rhs=xs[:, k, s0:s0 + n], start=(k == 0), stop=(k == 7)),
                                         r=[wg, xs], w=[pA])
                                for k in range(8):
                                    S.op("pe", lambda e, k=k: e.matmul(pU[:, 0:n], lhsT=wu[:, k, fc * 128:(fc + 1) * 128],
                                                                      rhs=xs[:, k, s0:s0 + n], start=(k == 0), stop=(k == 7)),
                                         r=[wu, xs], w=[pU])
                                sa = SA.next()
                                S.op("act", lambda e: e.activation(out=sa[:, 0:n], in_=pA[:, 0:n], func=AF.Silu), r=[pA], w=[sa])
                                S.op("dve", lambda e: e.tensor_tensor(out=actT[:, fc, s0:s0 + n], in0=sa[:, 0:n], in1=pU[:, 0:n],
                                                                      op=ALU.mult), r=[sa, pU], w=[actT])
                        for (s0, rows) in STL:
                            ys = YS.next()
                            for half in range(2):
                                ps = PS_MM.next()
                                for fc in range(8):
                                    S.op("pe", lambda e, fc=fc: e.matmul(ps[0:rows, :], lhsT=actT[:, fc, s0:s0 + rows],
                                                                        rhs=wd[:, fc, half * 512:(half + 1) * 512],
                                                                        start=(fc == 0), stop=(fc == 7)), r=[actT, wd], w=[ps])
                                if half == 0:
                                    S.op("act", lambda e: e.activation(out=ys[0:rows, 0:512], in_=ps[0:rows, :], func=AF.Copy),
                                         r=[ps], w=[ys])
                                else:
                                    S.op("dve", lambda e: e.tensor_copy(out=ys[0:rows, 512:1024], in_=ps[0:rows, :]), r=[ps], w=[ys])
                            S.dma("sp", YEX[ex_][s0:s0 + rows, :], ys[0:rows, :], r=[ys])
                    S.barrier()

                chk("m2")
                with ExitStack() as m3:
                    selb = sb("selb", [16, 16 * 128], F32, m3)
                    slot = sb("slot", [16, NTOK], F32, m3)
                    gg = sb("gg", [16, NTOK], F32, m3)
                    Yl = sb("Yl", [128, NE, 2, D], BF16, m3)
                    Yc = sb("Yc", [32, NE, D], BF16, m3)
                    PT = sb("PT", [128, NE, 2, 512], BF16, m3)
                    PTc = sb("PTc", [32, NE, 256], BF16, m3)
                    GB = Pool([sb("gb%d" % i, [128, 512], F32, m3) for i in range(3)])
                    g2b = sb("g2b", [128, D], F32, m3)
                    XT3 = Pool([sb("xt3_%d" % i, [128, D], F32, m3) for i in range(2)])
                    OT3 = Pool([sb("ot3_%d" % i, [128, D], F32, m3) for i in range(1)])
                    ST3 = Pool([sb("st3_%d" % i, [128, 8], F32, m3) for i in range(4)])
                    fgb = sb("fgb", [128, D], F32, m3)
                    S.dma("sp", fgb[:], final_g[0:1, :].to_broadcast([128, D]), w=[fgb])
                    S.dma("sp", selb[:], c_sel[:, :], w=[selb])
                    last = (l == DEPTH - 1)
                    for b in range(NS):
                        S.dma("sp", slot[:], SLOTG[b * 16:(b + 1) * 16, 0, :], w=[slot])
                        S.dma("sp", gg[:], SLOTG[b * 16:(b + 1) * 16, 1, :], w=[gg])
                        for ex_ in range(NE):
                            S.dma("sp", Yl[:, ex_], YEX[ex_][b * CAP:(b + 1) * CAP, :].rearrange("(k p) d -> p k d", p=128), w=[Yl],
                                  add=(ex_ > 0))
                        o0 = NS * CAP + b * CAPC
                        for ex_ in range(NE):
                            S.dma("sp", Yc[:, ex_, :], YEX[ex_][o0:o0 + CAPC, :], w=[Yc], add=(ex_ > 0))
                        S.dma("sp", g2b[:], MODS[l][b:b + 1, 5120:6144].to_broadcast([128, D]), w=[g2b])
                        for (t0, n) in TCH:
                            latent = t0 < T
                            if last and not latent:
                                continue
                            R = 128 if latent else 32
                            if not latent:
                                S.dma("sp", g2b[:], MODS[l][4:5, 5120:6144].to_broadcast([128, D]), w=[g2b])
                            for ex_ in range(NE):
                                psS, psG = PS_MM.next(), PS_MM.next()
                                S.op("pe", lambda e: e.matmul(psS[0:R, 0:n], lhsT=selb[:, ex_ * 128:ex_ * 128 + R], rhs=slot[:, t0:t0 + n],
                                                              start=True, stop=True), r=[selb, slot], w=[psS])
                                S.op("pe", lambda e: e.matmul(psG[0:R, 0:n], lhsT=selb[:, ex_ * 128:ex_ * 128 + R], rhs=gg[:, t0:t0 + n],
                                                              start=True, stop=True), r=[selb, gg], w=[psG])
                                gb = GB.next()
                                S.op("act", lambda e: e.activation(out=gb[0:R, 0:n], in_=psG[0:R, 0:n], func=AF.Copy), r=[psG], w=[gb])
                                if latent:
                                    for k in range(2):
                                        S.op("dve", lambda e, k=k: e.scalar_tensor_tensor(
                                            out=PT[:, ex_, k, 0:n], in0=psS[:, 0:n], scalar=iota[:, 256 + k:257 + k], in1=gb[:, 0:n],
                                            op0=ALU.is_equal, op1=ALU.mult), r=[psS, iota, gb], w=[PT])
                                else:
                                    S.op("dve", lambda e: e.scalar_tensor_tensor(
                                        out=PTc[:, ex_, 0:n], in0=psS[0:32, 0:n], scalar=iota[0:32, 256:257], in1=gb[0:32, 0:n],
                                        op0=ALU.is_equal, op1=ALU.mult), r=[psS, iota, gb], w=[PTc])
                            for i in range(t0 // 128, (t0 + n) // 128):
                                tt = i * 128 - t0
                                xtile = XT3.next()
                                S.dma("sp", xtile[:], XMID[b, i], w=[xtile])
                                gt = g2b
                                for half in range(2):
                                    ps = PS_AC.next()
                                    if latent:
                                        for ex_ in range(NE):
                                            for k in range(2):
                                                S.op("pe", lambda e, ex_=ex_, k=k: e.matmul(
                                                    ps[:, :], lhsT=PT[:, ex_, k, tt:tt + 128], rhs=Yl[:, ex_, k, half * 512:(half + 1) * 512],
                                                    start=(ex_ == 0 and k == 0), stop=(ex_ == NE - 1 and k == 1)), r=[PT, Yl], w=[ps])
                                    else:
                                        for ex_ in range(NE):
                                            S.op("pe", lambda e, ex_=ex_: e.matmul(
                                                ps[:, :], lhsT=PTc[:, ex_, tt:tt + 128], rhs=Yc[:, ex_, half * 512:(half + 1) * 512],
                                                start=(ex_ == 0), stop=(ex_ == NE - 1)), r=[PTc, Yc], w=[ps])
                                    tm = GB.next()
                                    S.op("dve", lambda e: e.tensor_tensor(out=tm[:], in0=ps[:, :], in1=gt[:, half * 512:(half + 1) * 512],
                                                                          op=ALU.mult), r=[ps, gt], w=[tm])
                                    S.op("pool", lambda e: e.tensor_tensor(out=xtile[:, half * 512:(half + 1) * 512],
                                                                           in0=xtile[:, half * 512:(half + 1) * 512], in1=tm[:],
                                                                           op=ALU.add), r=[xtile, tm], w=[xtile])
                                if debug and b == 0 and l == 0:
                                    S.dma("sp", dbg["xres"][i], xtile[:], r=[xtile])
                                if not last:
                                    S.dma("sp", XRES[b, i], xtile[:], r=[xtile])
                                else:
                                    ot = OT3.next()
                                    ss = ST3.next()
                                    S.op("pool", lambda e: e.memset(ss[:], 0.0), w=[ss])
                                    S.op("act", lambda e: e.activation(out=ot[:], in_=xtile[:], func=AF.Square, accum_out=ss[:, 0:1]),
                                         r=[xtile], w=[ot, ss])
                                    S.op("dve", lambda e: e.tensor_scalar(out=ss[:, 1:2], in0=ss[:, 0:1], scalar1=1.0 / D, scalar2=EPS,
                                                                          op0=ALU.mult, op1=ALU.add), r=[ss], w=[ss])
                                    S.op("dve", lambda e: e.reciprocal(out=ss[:, 2:3], in_=ss[:, 1:2]), r=[ss], w=[ss])
                                    S.op("act", lambda e: e.activation(out=ss[:, 3:4], in_=ss[:, 2:3], func=AF.Sqrt), r=[ss], w=[ss])
                                    S.op("dve", lambda e: e.scalar_tensor_tensor(out=ot[:], in0=xtile[:], scalar=ss[:, 3:4], in1=fgb[:],
                                                                                 op0=ALU.mult, op1=ALU.mult), r=[xtile, ss, fgb], w=[ot])
                                    S.dma("sp", out[b, i * 128:(i + 1) * 128, :], ot[:], r=[ot])
                    S.barrier()
        except _Stop:
            S.barrier()
        S.barrier(engines=["sp"])
        print("instr counts", S.cnt, "wait instrs", S.n_wait_ins, "ndma", sum(S.dma_val) // 16, flush=True)
    return nc


def _consts():
    identb = np.eye(128, dtype=np.float32).astype(ml_dtypes.bfloat16)
    identf = np.eye(128, dtype=np.float32)
    j = np.arange(128)[:, None]
    q = np.arange(128)[None, :]
    masks = np.stack([(j <= q), (j >= q)], axis=1).astype(np.float32).astype(ml_dtypes.bfloat16)
    iota = np.zeros((128, 258), np.float32)
    iota[:, 0:256] = np.arange(1, 257, dtype=np.float32)[None, :]
    iota[:, 256] = np.arange(1, 129)
    iota[:, 257] = np.arange(129, 257)
    pos = np.arange(T)
    row = (pos // 64).astype(np.float32)
    colp = (pos % 64).astype(np.float32)
    inv = (10000.0 ** (-np.arange(16, dtype=np.float32) / 16)).astype(np.float32)
    ar = row[:, None] * inv
    ac = colp[:, None] * inv
    ang = np.concatenate([ar, ar, ac, ac], axis=-1).astype(np.float32)
    cos = np.cos(ang).astype(np.float32)
    sin = np.sin(ang).astype(np.float32)
    sgn = np.tile(np.concatenate([-np.ones(16), np.ones(16)]), 2).astype(np.float32)
    ss = sin * sgn[None, :]
    cos6 = np.tile(cos, (1, 6))
    ss6 = np.tile(ss, (1, 6))
    rope = np.stack([cos6, ss6], axis=1).reshape(NTL, 128, 2, 384).astype(np.float32)
    sel = np.zeros((16, 16, 128), np.float32)
    for e in range(16):
        sel[e, e, :] = 1.0
    return dict(c_identb=identb, c_identf=identf, c_masks=masks, c_iota=iota, c_rope=rope,
                c_sel=sel.reshape(16, 16 * 128))


def _prep_shared(inp, DEPTH):
    f = lambda a: np.ascontiguousarray(np.asarray(a, dtype=np.float32))
    sh = {}
    sh["ada_w"] = f(inp["ada_w"][:DEPTH])
    sh["ada_b"] = f(inp["ada_b"][:DEPTH])
    rep = lambda g: f(np.repeat(np.asarray(g)[:DEPTH].reshape(DEPTH, 8, 128).transpose(0, 2, 1)[..., None], 5, axis=-1))
    sh["n1rep"] = rep(inp["norm1_g"])
    sh["n2rep"] = rep(inp["norm2_g"])
    sh["w_in"] = f(inp["w_in"][:DEPTH])
    cp = np.zeros((DEPTH, 128, 2, 40), np.float32)
    ca = np.asarray(inp["conv_a_w"])[:DEPTH]
    cb = np.asarray(inp["conv_b_w"])[:DEPTH]
    for h in range(2):
        cp[:, :, h, 0:3] = ca[:, :, h * 128:(h + 1) * 128].transpose(0, 2, 1)
        cp[:, :, h, 3:34] = cb[:, :, h * 128:(h + 1) * 128].transpose(0, 2, 1)
        cp[:, :, h, 34] = np.asarray(inp["conv_b_b"])[:DEPTH, h * 128:(h + 1) * 128]
        cp[:, :, h, 35] = np.asarray(inp["conv_ln_g"])[:DEPTH, h * 128:(h + 1) * 128]
        cp[:, :, h, 36] = np.asarray(inp["conv_ln_b"])[:DEPTH, h * 128:(h + 1) * 128]
    sh["convp"] = cp
    sh["sink"] = f(np.asarray(inp["sink"])[:DEPTH].reshape(1, DEPTH * 4))
    sh["qng"] = f(inp["q_norm_g"][:DEPTH])
    sh["kng"] = f(inp["k_norm_g"][:DEPTH])
    sh["w_out"] = f(inp["w_out"][:DEPTH])
    sh["router"] = f(inp["router_w"][:DEPTH])
    sh["w_gate"] = f(inp["w_gate"][:DEPTH])
    sh["w_up"] = f(inp["w_up"][:DEPTH])
    sh["w_down"] = f(inp["w_down"][:DEPTH])
    sh["final_g"] = f(np.asarray(inp["final_g"]).reshape(1, D))
    sh.update(_consts())
    return sh


def _core_inputs(inp, sh, b0, NS):
    m = dict(sh)
    m["x"] = np.ascontiguousarray(np.asarray(inp["x"][b0:b0 + NS], dtype=np.float32))
    m["ctx"] = np.ascontiguousarray(np.asarray(inp["ctx"][b0:b0 + NS], dtype=np.float32))
    call = np.zeros((5, D), np.float32)
    call[0:NS] = np.asarray(inp["c"][b0:b0 + NS])
    call[4] = np.asarray(inp["c_ctx"])
    m["cT"] = np.ascontiguousarray(call.reshape(5, 8, 128).transpose(2, 1, 0))
    return m


_NC_CACHE = {}


def kernel(**inputs):
    NS, DEPTH = 4, 4
    key = (NS, DEPTH)
    if key not in _NC_CACHE:
        _NC_CACHE[key] = build_program(NS, DEPTH)
    nc = _NC_CACHE[key]
    sh = _prep_shared(inputs, DEPTH)
    in_maps = [_core_inputs(inputs, sh, c * NS, NS) for c in range(N_CORES)]
    res = run_bass_kernel_spmd(nc, in_maps, core_ids=list(range(N_CORES)))
    outs = [np.asarray(r["out"]) for r in res.results]
    return np.concatenate(outs, axis=0).astype(np.float32)
```

```python
import numpy as np
import ml_dtypes
from contextlib import ExitStack
import concourse.bass as bass
import concourse.mybir as mybir
from concourse.bass_utils import run_bass_kernel_spmd

F32 = mybir.dt.float32
BF16 = mybir.dt.bfloat16
AF = mybir.ActivationFunctionType
ALU = mybir.AluOpType
AX = mybir.AxisListType

D = 1024
T = 2048
C = 256
NTOK = T + C
NTL = T // 128
NTT = NTOK // 128
NE = 16
CAP = 256
CAPC = 32
IN_W = 2304
EPS = 1e-6
N_CORES = 8
SAME_ENG_SYNC = True


class Buf:
    __slots__ = ("name", "w", "r")

    def __init__(self, name=""):
        self.name = name
        self.w = []
        self.r = {}


class Tl:
    def __init__(self, t, name=""):
        self.t = t
        self.b = Buf(name)

    def __getitem__(self, k):
        return self.t[k]


class Sync:
    W = 30000
    NDMA = 20

    def __init__(self, nc, es):
        self.nc = nc
        self.es = es
        self.engs = {"pe": nc.tensor, "act": nc.scalar, "dve": nc.vector, "pool": nc.gpsimd, "sp": nc.sync}
        self.cnt = {e: 0 for e in self.engs}
        self.sems = {e: [] for e in self.engs}
        self.known = {e: {} for e in self.engs}
        self.semid = {}
        self.dma_sems = [self._newsem("dma%d" % i) for i in range(self.NDMA)]
        self.dma_val = [0] * self.NDMA
        self.dma_i = 0
        self.n_wait_ins = 0
        self.dead = False

    def _newsem(self, name):
        s = self.es.enter_context(self.nc.semaphore(name))
        self.semid[id(s)] = len(self.semid)
        return s

    def _sid(self, s):
        return self.semid[id(s)]

    def _next_tok(self, eng):
        n = self.cnt[eng]
        k = n // self.W
        while len(self.sems[eng]) <= k:
            self.sems[eng].append(self._newsem("%s_m%d" % (eng, len(self.sems[eng]))))
        self.cnt[eng] = n + 1
        return (self.sems[eng][k], n % self.W + 1, eng)

    def _last_tok(self, eng):
        n = self.cnt[eng]
        if n == 0:
            return None
        n -= 1
        return (self.sems[eng][n // self.W], n % self.W + 1, eng)

    def _deps(self, r, w, add=False):
        toks = []
        for b in r:
            toks.extend(b.w)
        for b in w:
            if not add:
                toks.extend(b.w)
            toks.extend(b.r.values())
        return toks

    def _need(self, eng, toks):
        kn = self.known[eng]
        need = {}
        for (s, v, src) in toks:
            if src == eng and (eng == "pe" or not SAME_ENG_SYNC):
                continue
            sid = self._sid(s)
            if kn.get(sid, 0) >= v:
                continue
            if sid not in need or need[sid][1] < v:
                need[sid] = (s, v)
        return list(need.values())

    def _emit_waits(self, eng, waits, ins_fn):
        E = self.engs[eng]
        for (s, v) in waits[:-1]:
            E.wait_ge(s, v)
            self.n_wait_ins += 1
        ins = ins_fn(E)
        if waits:
            s, v = waits[-1]
            ins._wait_ge(s, v)
        kn = self.known[eng]
        for (s, v) in waits:
            sid = self._sid(s)
            if kn.get(sid, 0) < v:
                kn[sid] = v
        return ins

    def op(self, eng, fn, r=(), w=()):
        if self.dead:
            return None
        r = [x.b if isinstance(x, Tl) else x for x in r]
        w = [x.b if isinstance(x, Tl) else x for x in w]
        waits = self._need(eng, self._deps(r, w))
        ins = self._emit_waits(eng, waits, fn)
        tok = self._next_tok(eng)
        ins.then_inc(tok[0], 1)
        for b in r:
            b.r[eng] = tok
        for b in w:
            b.w = [tok]
            b.r = {}
        return ins

    def dma(self, eng, out, in_, r=(), w=(), add=False, **kw):
        if self.dead:
            return None
        r = [x.b if isinstance(x, Tl) else x for x in r]
        w = [x.b if isinstance(x, Tl) else x for x in w]
        toks = self._deps(r, w, add)
        i = self.dma_i
        self.dma_i = (i + 1) % self.NDMA
        s = self.dma_sems[i]
        if self.dma_val[i] > 0:
            toks.append((s, self.dma_val[i], "dma"))
        waits = self._need(eng, toks)
        ins = self._emit_waits(eng, waits, lambda E: E.dma_start(out=out, in_=in_, **kw))
        self.dma_val[i] += 16
        tok = (s, self.dma_val[i], "dma")
        ins.then_inc(s, 16)
        for b in r:
            b.r[("dma", i)] = tok
        for b in w:
            if add:
                b.w.append(tok)
            else:
                b.w = [tok]
                b.r = {}
        return ins

    def barrier(self, engines=None):
        toks = []
        for e in self.engs:
            t = self._last_tok(e)
            if t is not None:
                toks.append(t)
        for i in range(self.NDMA):
            if self.dma_val[i] > 0:
                toks.append((self.dma_sems[i], self.dma_val[i], "dma"))
        for e in (engines or list(self.engs)):
            kn = self.known[e]
            E = self.engs[e]
            for (s, v, src) in toks:
                if src == e:
                    continue
                sid = self._sid(s)
                if kn.get(sid, 0) >= v:
                    continue
                E.wait_ge(s, v)
                self.n_wait_ins += 1
                kn[sid] = v


class Pool:
    def __init__(self, tiles):
        self.tiles = tiles
        self.i = 0

    def next(self):
        t = self.tiles[self.i]
        self.i = (self.i + 1) % len(self.tiles)
        return t


class _Stop(Exception):
    pass


def build_program(NS=4, DEPTH=4, debug=False, stop=None):
    _sref = []

    _cur = [0]

    def chk(tag):
        if stop == tag or stop == "%s@%d" % (tag, _cur[0]):
            _sref[0].dead = True
    nc = bass.Bass("TRN2", target_bir_lowering=False)
    dt = lambda name, shape, dtype, kind="Internal": nc.dram_tensor(name, list(shape), dtype, kind=kind).ap()
    x_in = dt("x", [NS, T, D], F32, "ExternalInput")
    ctx_in = dt("ctx", [NS, C, D], F32, "ExternalInput")
    cT_in = dt("cT", [128, 8, 5], F32, "ExternalInput")
    ada_w = dt("ada_w", [DEPTH, D, 6 * D], F32, "ExternalInput")
    ada_b = dt("ada_b", [DEPTH, 6 * D], F32, "ExternalInput")
    n1rep = dt("n1rep", [DEPTH, 128, 8, 5], F32, "ExternalInput")
    n2rep = dt("n2rep", [DEPTH, 128, 8, 5], F32, "ExternalInput")
    w_in = dt("w_in", [DEPTH, D, IN_W], F32, "ExternalInput")
    convp = dt("convp", [DEPTH, 128, 2, 40], F32, "ExternalInput")
    sink_in = dt("sink", [1, DEPTH * 4], F32, "ExternalInput")
    qng = dt("qng", [DEPTH, 64], F32, "ExternalInput")
    kng = dt("kng", [DEPTH, 64], F32, "ExternalInput")
    w_out = dt("w_out", [DEPTH, D, D], F32, "ExternalInput")
    router = dt("router", [DEPTH, D, NE], F32, "ExternalInput")
    w_gate = dt("w_gate", [DEPTH, NE, D, D], F32, "ExternalInput")
    w_up = dt("w_up", [DEPTH, NE, D, D], F32, "ExternalInput")
    w_down = dt("w_down", [DEPTH, NE, D, D], F32, "ExternalInput")
    final_g = dt("final_g", [1, D], F32, "ExternalInput")
    c_identb = dt("c_identb", [128, 128], BF16, "ExternalInput")
    c_identf = dt("c_identf", [128, 128], F32, "ExternalInput")
    c_masks = dt("c_masks", [128, 2, 128], BF16, "ExternalInput")
    c_iota = dt("c_iota", [128, 258], F32, "ExternalInput")
    c_rope = dt("c_rope", [NTL, 128, 2, 384], F32, "ExternalInput")
    c_sel = dt("c_sel", [16, 16 * 128], F32, "ExternalInput")
    out = dt("out", [NS, T, D], F32, "ExternalOutput")
    dbg = {}
    if debug:
        dbg["xmid"] = dt("d_xmid", [NTT, 128, D], F32, "ExternalOutput")
        dbg["aff"] = dt("d_aff", [128, NTT, 64], F32, "ExternalOutput")
        dbg["yt"] = dt("d_yt", [8, 128, NTOK], BF16, "ExternalOutput")
        dbg["xres"] = dt("d_xres", [NTT, 128, D], F32, "ExternalOutput")
        dbg["slotg"] = dt("d_slotg", [64, 2, NTOK], F32, "ExternalOutput")
        dbg["qT"] = dt("d_qT", [128, 2, NTOK], BF16, "ExternalOutput")
        dbg["kT"] = dt("d_kT", [128, 1, NTOK], BF16, "ExternalOutput")
    MODS = dt("MODS", [DEPTH, 5, 6 * D], F32)
    XMID = dt("XMID", [NS, NTT, 128, D], F32)
    XRES = dt("XRES", [NS, NTT, 128, D], F32)
    XN2 = dt("XN2", [NS, NTT, 128, D], BF16)
    YT = dt("YT", [NS, 8, 128, NTOK], BF16)
    NSL = NS * CAP + NS * CAPC
    XSG = dt("XSG", [NE, 8, 128, NSL], BF16)
    YEX = dt("YEX", [NE, NSL, D], BF16)
    SLOTG = dt("SLOTG", [64, 2, NTOK], F32)

    with ExitStack() as es:
        S = Sync(nc, es)
        _sref.append(S)

        _uid = [0]

        def sb(name, shape, dtype, scope=es):
            _uid[0] += 1
            name = "%s_u%d" % (name, _uid[0])
            return Tl(scope.enter_context(nc.sbuf_tensor(name, list(shape), dtype)), name)

        def dma_k(eng, tile, src2d, ncols=None):
            for k in range(8):
                dst = tile[:, k, :] if ncols is None else tile[:, k, 0:ncols]
                S.dma(eng, dst, src2d[k * 128:(k + 1) * 128, :], w=[tile], add=(k > 0))

        psb = [Tl(es.enter_context(nc.psum_tensor("ps%d" % i, [128, 512], F32)), "ps%d" % i) for i in range(8)]
        PS_MM = Pool(psb[0:4])
        PS_TR = Pool(psb[4:6])
        PS_AC = Pool(psb[6:8])

        def bfv(ps):
            return ps.t.bitcast(BF16)

        identb = sb("identb", [128, 128], BF16)
        identf = sb("identf", [128, 128], F32)
        masks = sb("masks", [128, 2, 128], BF16)
        iota = sb("iota", [128, 258], F32)
        onesf = sb("onesf", [128, 128], F32)
        onesb = sb("onesb", [128, 128], BF16)
        modsT = sb("modsT", [128, DEPTH, 48, 5], F32)
        affTok = sb("affTok", [128, NTT, 64], F32)
        esink = sb("esink", [128, DEPTH * 4], F32)
        S.dma("sp", identb[:], c_identb[:, :], w=[identb])
        S.dma("sp", identf[:], c_identf[:, :], w=[identf])
        S.dma("sp", masks[:], c_masks[:, :, :], w=[masks])
        S.dma("sp", iota[:], c_iota[:, :], w=[iota])
        S.dma("sp", esink[:], sink_in[0:1, :].to_broadcast([128, DEPTH * 4]), w=[esink])
        S.op("dve", lambda e: e.memset(onesf[:], 1.0), w=[onesf])
        S.op("dve", lambda e: e.memset(onesb[:], 1.0), w=[onesb])
        S.op("dve", lambda e: e.memset(affTok[:], 0.0), w=[affTok])
        S.op("act", lambda e: e.activation(out=esink[:], in_=esink[:], func=AF.Exp), r=[esink], w=[esink])

        with ExitStack() as ps0:
            scT = sb("scT", [128, 8, 5], F32, ps0)
            adw = [sb("adw%d" % i, [128, 8, 512], F32, ps0) for i in range(2)]
            adb = [sb("adb%d" % i, [5, 512], F32, ps0) for i in range(2)]
            mrow = [sb("mrow%d" % i, [5, 512], F32, ps0) for i in range(2)]
            S.dma("sp", scT[:], cT_in[:, :, :], w=[scT])
            S.op("act", lambda e: e.activation(out=scT[:], in_=scT[:], func=AF.Silu), r=[scT], w=[scT])
            it = 0
            for l in range(DEPTH):
                for n in range(12):
                    wt, bt, mr = adw[it % 2], adb[it % 2], mrow[it % 2]
                    it += 1
                    dma_k("sp", wt, ada_w[l][:, n * 512:(n + 1) * 512])
                    S.dma("sp", bt[:], ada_b[l:l + 1, n * 512:(n + 1) * 512].to_broadcast([5, 512]), w=[bt])
                    ps = PS_MM.next()
                    for k in range(8):
                        S.op("pe", lambda e, k=k: e.matmul(ps[0:5, :], lhsT=scT[:, k, :], rhs=wt[:, k, :],
                                                          start=(k == 0), stop=(k == 7)), r=[scT, wt], w=[ps])
                    S.op("dve", lambda e: e.tensor_tensor(out=mr[:], in0=ps[0:5, :], in1=bt[:], op=ALU.add),
                         r=[ps, bt], w=[mr])
                    S.dma("sp", MODS[l][:, n * 512:(n + 1) * 512], mr[:], r=[mr])
                    pt = PS_TR.next()
                    for j in range(4):
                        S.op("pe", lambda e, j=j: e.transpose(pt[:, j * 5:(j + 1) * 5], mr[:, j * 128:(j + 1) * 128],
                                                              identf[0:5, 0:5]), r=[mr, identf], w=[pt])
                    S.op("act", lambda e: e.activation(
                        out=modsT[:, l, n * 4:(n + 1) * 4, :],
                        in_=pt[:, 0:20].rearrange("p (j s) -> p j s", s=5), func=AF.Copy), r=[pt], w=[modsT])
        S.barrier()

        try:
            chk("pro")
            def x_src(l, b, i):
                if l == 0:
                    return x_in[b, i * 128:(i + 1) * 128, :] if i < NTL else ctx_in[b, (i - NTL) * 128:(i - NTL + 1) * 128, :]
                return XRES[b, i]

            A1 = sb("A1", [128, 8, 5], F32)
            A2 = sb("A2", [128, 8, 5], F32)
            for l in range(DEPTH):
                _cur[0] = l
                with ExitStack() as p1:
                    YAG = Pool([sb("yag%d" % i, [128, 256], BF16, p1) for i in range(4)])
                    H2T = Pool([sb("h2t%d" % i, [128, 8, 128], BF16, p1) for i in range(2)])
                    YTB = [Buf("ytb%d" % i) for i in range(NS)]
                    nrep = sb("nrep", [128, 2, 8, 5], F32, p1)
                    cvp = sb("cvp", [128, 2, 40], F32, p1)
                    qg = sb("qg", [128, 64], F32, p1)
                    kg = sb("kg", [128, 64], F32, p1)
                    woutb = sb("woutb", [128, 8, D], BF16, p1)
                    rtb = sb("rtb", [128, 8, NE], BF16, p1)
                    hT = sb("hT", [128, 8, NTOK], BF16, p1)
                    xt = [sb("xt%d" % i, [128, D], F32, p1) for i in range(2)]
                    xnb = [sb("xnb%d" % i, [128, D], BF16, p1) for i in range(2)]
                    st = [sb("st%d" % i, [128, 8], F32, p1) for i in range(4)]
                    XT, XNB, ST = Pool(xt), Pool(xnb), Pool(st)
                    wch = [sb("wch%d" % i, [128, 8, 512], BF16, p1) for i in range(2)]
                    WCH = Pool(wch)
                    cw = [sb("cw%d" % i, [128, NTOK + 32], F32, p1) for i in range(3)]
                    upc = sb("upc", [128, C + 32], F32, p1)
                    ystg = [sb("ystg%d" % i, [128, NTOK], BF16, p1) for i in range(2)]
                    YSTG = Pool(ystg)
                    qT = sb("qT", [128, 2, NTOK], BF16, p1)
                    kT = sb("kT", [128, 1, NTOK], BF16, p1)
                    Vt = sb("Vt", [128, NTT, 2, 65], BF16, p1)
                    rope = [sb("rope%d" % i, [128, 2, 384], F32, p1) for i in range(2)]
                    ROPE = Pool(rope)
                    qk = [sb("qk%d" % i, [128, 384], F32, p1) for i in range(2)]
                    QK = Pool(qk)
                    qk2 = [sb("qkb%d" % i, [128, 384], F32, p1) for i in range(2)]
                    QK2 = Pool(qk2)
                    qkr = [sb("qkr%d" % i, [128, 384], BF16, p1) for i in range(2)]
                    QKR = Pool(qkr)
                    ptl = [sb("ptl%d" % i, [128, 512], BF16, p1) for i in range(4)]
                    PTL = Pool(ptl)
                    yat = [sb("yat%d" % i, [128, 256], BF16, p1) for i in range(2)]
                    YAT = Pool(yat)
                    g1b = sb("g1b", [128, D], F32, p1)
                    YTL = WCH
                    tmpf = [sb("tmpf%d" % i, [128, 512], F32, p1) for i in range(6)]
                    TMPF = Pool(tmpf)

                    S.dma("sp", nrep[:, 0], n1rep[l], w=[nrep])
                    S.dma("sp", nrep[:, 1], n2rep[l], w=[nrep])
                    S.dma("sp", cvp[:], convp[l], w=[cvp])
                    S.dma("sp", qg[:], qng[l:l + 1, :].to_broadcast([128, 64]), w=[qg])
                    S.dma("sp", kg[:], kng[l:l + 1, :].to_broadcast([128, 64]), w=[kg])
                    dma_k("pool", woutb, w_out[l])
                    S.dma("pool", rtb[:], router[l].rearrange("(k p) f -> p k f", p=128), w=[rtb])
                    S.op("dve", lambda e: e.scalar_tensor_tensor(out=A1[:], in0=modsT[:, l, 8:16, :], scalar=1.0,
                                                                 in1=nrep[:, 0], op0=ALU.add, op1=ALU.mult),
                         r=[modsT, nrep], w=[A1])
                    S.op("dve", lambda e: e.scalar_tensor_tensor(out=A2[:], in0=modsT[:, l, 32:40, :], scalar=1.0,
                                                                 in1=nrep[:, 1], op0=ALU.add, op1=ALU.mult),
                         r=[modsT, nrep], w=[A2])
                    S.op("dve", lambda e: e.memset(Vt[:], 1.0), w=[Vt])

                    def norm_to_T(src_tile, dstT, dcols, A, Bofs, j, xn_keep=None):
                        ss = ST.next()
                        xn = xn_keep if xn_keep is not None else XNB.next()
                        S.op("pool", lambda e: e.memset(ss[:], 0.0), w=[ss])
                        S.op("act", lambda e: e.activation(out=xn[:], in_=src_tile[:], func=AF.Square,
                                                           accum_out=ss[:, 0:1]), r=[src_tile], w=[xn, ss])
                        S.op("dve", lambda e: e.tensor_scalar(out=ss[:, 1:2], in0=ss[:, 0:1], scalar1=1.0 / D, scalar2=EPS,
                                                              op0=ALU.mult, op1=ALU.add), r=[ss], w=[ss])
                        S.op("dve", lambda e: e.reciprocal(out=ss[:, 2:3], in_=ss[:, 1:2]), r=[ss], w=[ss])
                        S.op("act", lambda e: e.activation(out=ss[:, 3:4], in_=ss[:, 2:3], func=AF.Sqrt), r=[ss], w=[ss])
                        S.op("act", lambda e: e.activation(out=xn[:], in_=src_tile[:], func=AF.Copy, scale=ss[:, 3:4]),
                             r=[src_tile, ss], w=[xn])
                        pt = PS_TR.next()
                        ptb = bfv(pt)
                        for c in range(8):
                            S.op("pe", lambda e, c=c: e.transpose(ptb[:, c * 128:(c + 1) * 128], xn[:, c * 128:(c + 1) * 128],
                                                                  identb[:]), r=[xn, identb], w=[pt])
                        for c in range(8):
                            if c % 2 == 0:
                                S.op("act", lambda e, c=c: e.activation(
                                    out=dstT[:, c, dcols], in_=ptb[:, c * 128:(c + 1) * 128], func=AF.Identity,
                                    scale=A[:, c, j:j + 1], bias=modsT[:, l, Bofs + c, j:j + 1]), r=[pt, A, modsT], w=[dstT])
                            else:
                                S.op("dve", lambda e, c=c: e.tensor_scalar(
                                    out=dstT[:, c, dcols], in0=ptb[:, c * 128:(c + 1) * 128],
                                    scalar1=A[:, c, j:j + 1], scalar2=modsT[:, l, Bofs + c, j:j + 1],
                                    op0=ALU.mult, op1=ALU.add), r=[pt, A, modsT], w=[dstT])
                        return xn

                    TCH = [(0, 512), (512, 512), (1024, 512), (1536, 512), (2048, 256)]
                    SEGS = [(0, T), (T, C)]

                    def colp(t):
                        return 16 + t if t < T else 16 + t + 0

                    for b in range(NS):
                        for i in range(NTT):
                            xtile = XT.next()
                            S.dma("sp", xtile[:], x_src(l, b, i), w=[xtile])
                            norm_to_T(xtile, hT, slice(i * 128, (i + 1) * 128), A1, 0, b if i < NTL else 4)

                        chk("p1_norm")
                        def inproj_fm(fc, consume):
                            wt = WCH.next()
                            dma_k("pool", wt, w_in[l][:, fc * 128:(fc + 1) * 128], 128)
                            for (t0, n) in TCH:
                                ps = PS_MM.next()
                                for k in range(8):
                                    S.op("pe", lambda e, k=k: e.matmul(ps[:, 0:n], lhsT=wt[:, k, 0:128], rhs=hT[:, k, t0:t0 + n],
                                                                      start=(k == 0), stop=(k == 7)), r=[wt, hT], w=[ps])
                                consume(ps, t0, n)

                        for h in range(2):
                            prod, cv = cw[0], cw[1]
                            S.op("pool", lambda e: e.memset(prod[:, 0:1], 0.0), w=[prod])
                            S.op("pool", lambda e: e.memset(prod[:, T + 1:T + 3], 0.0), w=[prod])
                            S.op("pool", lambda e: e.memset(prod[:, T + 3 + C:T + 4 + C], 0.0), w=[prod])

                            def offA(t0):
                                return 1 + t0 if t0 < T else T + 3 + (t0 - T)
                            inproj_fm(2 + h, lambda ps, t0, n: S.op(
                                "act", lambda e: e.activation(out=prod[:, offA(t0):offA(t0) + n], in_=ps[:, 0:n], func=AF.Copy),
                                r=[ps], w=[prod]))
                            inproj_fm(4 + h, lambda ps, t0, n: S.op(
                                "dve", lambda e: e.tensor_tensor(out=prod[:, offA(t0):offA(t0) + n], in0=ps[:, 0:n],
                                                                 in1=prod[:, offA(t0):offA(t0) + n], op=ALU.mult), r=[ps, prod], w=[prod]))
                            for (s0, sn) in SEGS:
                                o0 = offA(s0)
                                S.op("dve", lambda e: e.tensor_scalar(out=cv[:, s0:s0 + sn], in0=prod[:, o0 - 1:o0 - 1 + sn],
                                                                      scalar1=cvp[:, h, 0:1], scalar2=None, op0=ALU.mult),
                                     r=[prod, cvp], w=[cv])
                                for kk in (1, 2):
                                    S.op("dve", lambda e, kk=kk: e.scalar_tensor_tensor(
                                        out=cv[:, s0:s0 + sn], in0=prod[:, o0 - 1 + kk:o0 - 1 + kk + sn], scalar=cvp[:, h, kk:kk + 1],
                                        in1=cv[:, s0:s0 + sn], op0=ALU.mult, op1=ALU.add), r=[prod, cvp, cv], w=[cv])
                            ys = YSTG.next()
                            inproj_fm(0 + h, lambda ps, t0, n: S.op(
                                "dve", lambda e: e.tensor_tensor(out=ys[:, t0:t0 + n], in0=ps[:, 0:n], in1=cv[:, t0:t0 + n],
                                                                 op=ALU.mult), r=[ps, cv], w=[ys]))
                            S.dma("sp", YT[b, h], ys[:], r=[ys], w=[YTB[b]], add=True)
                        chk("p1_A")
                        ucs = [cw[1], cw[2]]
                        for h in range(2):
                            upad, uc = cw[0], ucs[h]
                            S.op("pool", lambda e: e.memset(upad[:, 0:15], 0.0), w=[upad])
                            S.op("pool", lambda e: e.memset(upad[:, 15 + T:30 + T], 0.0), w=[upad])
                            S.op("pool", lambda e: e.memset(upc[:, 0:15], 0.0), w=[upc])
                            S.op("pool", lambda e: e.memset(upc[:, 15 + C:30 + C], 0.0), w=[upc])

                            def dstB(t0, n):
                                return (upad, upad[:, 15 + t0:15 + t0 + n]) if t0 < T else (upc, upc[:, 15:15 + n])

                            def consB1(ps, t0, n):
                                tl, ap = dstB(t0, n)
                                S.op("act", lambda e: e.activation(out=ap, in_=ps[:, 0:n], func=AF.Sigmoid), r=[ps], w=[tl])

                            def consB2(ps, t0, n):
                                tl, ap = dstB(t0, n)
                                S.op("dve", lambda e: e.tensor_tensor(out=ap, in0=ps[:, 0:n], in1=ap, op=ALU.mult), r=[ps, tl], w=[tl])
                            inproj_fm(8 + h, consB1)
                            inproj_fm(6 + h, consB2)
                            for (src, s0, sn, eng) in ((upad, 0, T, "dve"), (upc, T, C, "dve")):
                                S.op(eng, lambda e: e.tensor_scalar(out=uc[:, s0:s0 + sn], in0=src[:, 0:sn],
                                                                    scalar1=cvp[:, h, 3:4], scalar2=cvp[:, h, 34:35],
                                                                    op0=ALU.mult, op1=ALU.add), r=[src, cvp], w=[uc])
                                for kk in range(1, 31):
                                    S.op(eng, lambda e, kk=kk: e.scalar_tensor_tensor(
                                        out=uc[:, s0:s0 + sn], in0=src[:, kk:kk + sn], scalar=cvp[:, h, 3 + kk:4 + kk],
                                        in1=uc[:, s0:s0 + sn], op0=ALU.mult, op1=ALU.add), r=[src, cvp, uc], w=[uc])
                        ysb = [YSTG.next(), YSTG.next()]
                        for (t0, n) in TCH:
                            p1s, p2s = PS_MM.next(), PS_MM.next()
                            for h in range(2):
                                S.op("pe", lambda e, h=h: e.matmul(p1s[:, 0:n], lhsT=onesf[:], rhs=ucs[h][:, t0:t0 + n],
                                                                  start=(h == 0), stop=(h == 1)), r=[onesf, ucs[h]], w=[p1s])
                            for h in range(2):
                                sqt = TMPF.next()
                                S.op("act", lambda e, h=h: e.activation(out=sqt[:, 0:n], in_=ucs[h][:, t0:t0 + n], func=AF.Square),
                                     r=[ucs[h]], w=[sqt])
                                S.op("pe", lambda e, h=h: e.matmul(p2s[:, 0:n], lhsT=onesf[:], rhs=sqt[:, 0:n],
                                                                  start=(h == 0), stop=(h == 1)), r=[onesf, sqt], w=[p2s])
                            mean, var, dd = TMPF.next(), TMPF.next(), TMPF.next()
                            S.op("act", lambda e: e.activation(out=mean[:, 0:n], in_=p1s[:, 0:n], func=AF.Copy, scale=1.0 / 256),
                                 r=[p1s], w=[mean])
                            S.op("dve", lambda e: e.tensor_tensor(out=var[:, 0:n], in0=mean[:, 0:n], in1=mean[:, 0:n], op=ALU.mult),
                                 r=[mean], w=[var])
                            S.op("dve", lambda e: e.scalar_tensor_tensor(out=var[:, 0:n], in0=p2s[:, 0:n], scalar=1.0 / 256,
                                                                         in1=var[:, 0:n], op0=ALU.mult, op1=ALU.subtract),
                                 r=[p2s, var], w=[var])
                            S.op("dve", lambda e: e.tensor_scalar(out=var[:, 0:n], in0=var[:, 0:n], scalar1=EPS, scalar2=None,
                                                                  op0=ALU.add), r=[var], w=[var])
                            S.op("dve", lambda e: e.reciprocal(out=var[:, 0:n], in_=var[:, 0:n]), r=[var], w=[var])
                            S.op("act", lambda e: e.activation(out=var[:, 0:n], in_=var[:, 0:n], func=AF.Sqrt), r=[var], w=[var])
                            for h in range(2):
                                S.op("pool", lambda e, h=h: e.tensor_tensor(out=dd[:, 0:n], in0=ucs[h][:, t0:t0 + n], in1=mean[:, 0:n],
                                                                            op=ALU.subtract), r=[ucs[h], mean], w=[dd])
                                S.op("pool", lambda e: e.tensor_tensor(out=dd[:, 0:n], in0=dd[:, 0:n], in1=var[:, 0:n], op=ALU.mult),
                                     r=[dd, var], w=[dd])
                                S.op("act", lambda e, h=h: e.activation(out=ysb[h][:, t0:t0 + n], in_=dd[:, 0:n], func=AF.Silu,
                                                                        scale=cvp[:, h, 35:36], bias=cvp[:, h, 36:37]),
                                     r=[dd, cvp], w=[ysb[h]])
                        for h in range(2):
                            S.dma("sp", YT[b, 2 + h], ysb[h][:], r=[ysb[h]], w=[YTB[b]], add=True)

                        chk("p1_B")
                        for grp in range(2):
                            wt = WCH.next()
                            c0 = 1280 + grp * 512
                            dma_k("pool", wt, w_in[l][:, c0:c0 + 512])
                            for i in range(NTT):
                                ps = PS_MM.next()
                                for k in range(8):
                                    S.op("pe", lambda e, k=k: e.matmul(ps[:, :], lhsT=hT[:, k, i * 128:(i + 1) * 128], rhs=wt[:, k, :],
                                                                      start=(k == 0), stop=(k == 7)), r=[hT, wt], w=[ps])
                                S.op("act", lambda e: e.activation(out=Vt[:, i, :, 0:64],
                                                                   in_=ps[:, 384:512].rearrange("p (a d) -> p a d", d=64),
                                                                   func=AF.Copy), r=[ps], w=[Vt])
                                q1 = QK.next()
                                S.op("act", lambda e: e.activation(out=q1[:], in_=ps[:, 0:384], func=AF.Copy), r=[ps], w=[q1])
                                if grp == 1:
                                    sq = QK2.next()
                                    ss = ST.next()
                                    S.op("dve", lambda e: e.tensor_tensor(out=sq[:], in0=q1[:], in1=q1[:], op=ALU.mult), r=[q1], w=[sq])
                                    S.op("dve", lambda e: e.tensor_reduce(out=ss[:, 0:6], in_=sq[:].rearrange("p (a d) -> p a d", d=64),
                                                                          axis=AX.X, op=ALU.add), r=[sq], w=[ss])
                                    S.op("dve", lambda e: e.tensor_scalar(out=ss[:, 0:6], in0=ss[:, 0:6], scalar1=1.0 / 64, scalar2=EPS,
                                                                          op0=ALU.mult, op1=ALU.add), r=[ss], w=[ss])
                                    S.op("dve", lambda e: e.reciprocal(out=ss[:, 0:6], in_=ss[:, 0:6]), r=[ss], w=[ss])
                                    S.op("act", lambda e: e.activation(out=ss[:, 0:6], in_=ss[:, 0:6], func=AF.Sqrt), r=[ss], w=[ss])
                                    for hh in range(6):
                                        gt = qg if hh < 4 else kg
                                        S.op("dve", lambda e, hh=hh, gt=gt: e.scalar_tensor_tensor(
                                            out=q1[:, hh * 64:(hh + 1) * 64], in0=q1[:, hh * 64:(hh + 1) * 64], scalar=ss[:, hh:hh + 1],
                                            in1=gt[:], op0=ALU.mult, op1=ALU.mult), r=[q1, ss, gt], w=[q1])
                                qr = QKR.next()
                                if i < NTL:
                                    rp = ROPE.next()
                                    S.dma("sp", rp[:], c_rope[i], w=[rp])
                                    t1, t2 = QK2.next(), QK2.next()
                                    v4 = lambda ap: ap.rearrange("p (a j q) -> p a j q", j=2, q=16)
                                    S.op("dve", lambda e: e.tensor_tensor(out=t1[:], in0=q1[:], in1=rp[:, 0, :], op=ALU.mult),
                                         r=[q1, rp], w=[t1])
                                    S.op("pool", lambda e: e.tensor_tensor(out=v4(t2[:])[:, :, 0, :], in0=v4(q1[:])[:, :, 1, :],
                                                                           in1=v4(rp[:, 1, :])[:, :, 0, :], op=ALU.mult), r=[q1, rp], w=[t2])
                                    S.op("pool", lambda e: e.tensor_tensor(out=v4(t2[:])[:, :, 1, :], in0=v4(q1[:])[:, :, 0, :],
                                                                           in1=v4(rp[:, 1, :])[:, :, 1, :], op=ALU.mult), r=[q1, rp], w=[t2])
                                    pq_o = lambda ap: ap[:, 0:256].rearrange("p (g k d) -> p k g d", g=2, k=2)
                                    pq_i = lambda ap: ap[:, 0:256].rearrange("p (k g d) -> p k g d", g=2, k=2)
                                    S.op("dve", lambda e: e.tensor_tensor(out=pq_o(qr[:]), in0=pq_i(t1[:]), in1=pq_i(t2[:]), op=ALU.add),
                                         r=[t1, t2], w=[qr])
                                    S.op("dve", lambda e: e.tensor_tensor(out=qr[:, 256:384], in0=t1[:, 256:384], in1=t2[:, 256:384],
                                                                          op=ALU.add), r=[t1, t2], w=[qr])
                                else:
                                    pq_o = lambda ap: ap[:, 0:256].rearrange("p (g k d) -> p k g d", g=2, k=2)
                                    pq_i = lambda ap: ap[:, 0:256].rearrange("p (k g d) -> p k g d", g=2, k=2)
                                    S.op("dve", lambda e: e.tensor_copy(out=pq_o(qr[:]), in_=pq_i(q1[:])), r=[q1], w=[qr])
                                    S.op("dve", lambda e: e.tensor_copy(out=qr[:, 256:384], in_=q1[:, 256:384]), r=[q1], w=[qr])
                                pt = PS_TR.next()
                                ptb = bfv(pt)
                                for hh in range(3):
                                    S.op("pe", lambda e, hh=hh: e.transpose(ptb[:, hh * 128:(hh + 1) * 128], qr[:, hh * 128:(hh + 1) * 128],
                                                                            identb[:]), r=[qr, identb], w=[pt])
                                S.op("act", lambda e: e.activation(out=qT[:, :, i * 128:(i + 1) * 128],
                                                                   in_=ptb[:, 0:256].rearrange("p (a t) -> p a t", t=128),
                                                                   func=AF.Copy), r=[pt], w=[qT])
                                S.op("dve", lambda e: e.tensor_copy(out=kT[:, 0, i * 128:(i + 1) * 128], in_=ptb[:, 256:384]),
                                     r=[pt], w=[kT])
                            if debug and grp == 1 and b == 0 and l == 0:
                                S.dma("sp", dbg["qT"][:, :, :], qT[:], r=[qT])
                                S.dma("sp", dbg["kT"][:, :, :], kT[:], r=[kT])
                            if grp == 0:
                                chk("p1_C0")
                            else:
                                chk("p1_D0")
                            yts = [YSTG.next(), YSTG.next()]

                            def normalize_head(pa, co, h, ya):
                                den = ST.next()
                                if grp == 0:
                                    S.op("act", lambda e: e.activation(out=den[:, 0:1], in_=pa[:, co + 64:co + 65], func=AF.Identity,
                                                                       bias=esink[:, l * 4 + h:l * 4 + h + 1]),
                                         r=[pa, esink], w=[den])
                                    S.op("dve", lambda e: e.reciprocal(out=den[:, 1:2], in_=den[:, 0:1]), r=[den], w=[den])
                                else:
                                    S.op("act", lambda e: e.activation(out=den[:, 0:1], in_=pa[:, co + 64:co + 65], func=AF.Copy),
                                         r=[pa], w=[den])
                                    S.op("dve", lambda e: e.reciprocal(out=den[:, 1:2], in_=den[:, 0:1]), r=[den], w=[den])
                                S.op("act", lambda e: e.activation(out=ya[:, h * 64:(h + 1) * 64], in_=pa[:, co:co + 64],
                                                                   func=AF.Copy, scale=den[:, 1:2]), r=[pa, den], w=[ya])

                            def finish_ya(ya, qb):
                                pt = PS_TR.next()
                                ptb = bfv(pt)
                                for c in range(2):
                                    S.op("pe", lambda e, c=c: e.transpose(ptb[:, c * 128:(c + 1) * 128], ya[:, c * 128:(c + 1) * 128],
                                                                          identb[:]), r=[ya, identb], w=[pt])
                                for c in range(2):
                                    S.op("dve", lambda e, c=c: e.tensor_copy(out=yts[c][:, qb * 128:(qb + 1) * 128],
                                                                             in_=ptb[:, c * 128:(c + 1) * 128]), r=[pt], w=[yts[c]])

                            items = []
                            for qb in range(NTT):
                                if qb >= NTL:
                                    kts = [NTL, NTL + 1]
                                elif grp == 0:
                                    kts = [m for m in (qb - 1, qb, qb + 1) if 0 <= m < NTL] + [NTL, NTL + 1]
                                else:
                                    kts = list(range(NTT))
                                nk = len(kts)
                                for h in range(4):
                                    for g0 in range(0, nk, 4):
                                        items.append((qb, h, g0, kts[g0:g0 + 4], nk))

                            def emit_st(item):
                                qb, h, g0, grpk, nk = item
                                kh = h // 2
                                ps = PS_MM.next()
                                for ki, m in enumerate(grpk):
                                    S.op("pe", lambda e, m=m, ki=ki: e.matmul(
                                        ps[:, ki * 128:(ki + 1) * 128], lhsT=kT[kh * 64:(kh + 1) * 64, 0, m * 128:(m + 1) * 128],
                                        rhs=qT[kh * 64:(kh + 1) * 64, h % 2, qb * 128:(qb + 1) * 128], start=True, stop=True),
                                        r=[kT, qT], w=[ps])
                                pt_ = PTL.next()
                                nn_ = len(grpk) * 128
                                S.op("act", lambda e: e.activation(out=pt_[:, 0:nn_], in_=ps[:, 0:nn_], func=AF.Exp, scale=0.125),
                                     r=[ps], w=[pt_])
                                if grp == 0 and qb < NTL:
                                    for ki, m in enumerate(grpk):
                                        if (m == qb - 1 or m == qb + 1) and m < NTL:
                                            mi = 1 if m == qb - 1 else 0
                                            S.op("pool", lambda e, ki=ki, mi=mi: e.tensor_tensor(
                                                out=pt_[:, ki * 128:(ki + 1) * 128], in0=pt_[:, ki * 128:(ki + 1) * 128],
                                                in1=masks[:, mi, :], op=ALU.mult), r=[pt_, masks], w=[pt_])
                                return pt_

                            cur = {}

                            def emit_pv(item, pt_):
                                qb, h, g0, grpk, nk = item
                                kh = h // 2
                                if h == 0 and g0 == 0:
                                    cur["pacc"] = PS_AC.next()
                                    cur["ya"] = YAT.next()
                                pacc = cur["pacc"]
                                for ki, m in enumerate(grpk):
                                    S.op("pe", lambda e, ki=ki, m=m: e.matmul(
                                        pacc[:, h * 65:(h + 1) * 65], lhsT=pt_[:, ki * 128:(ki + 1) * 128], rhs=Vt[:, m, kh, :],
                                        start=(g0 + ki == 0), stop=(g0 + ki == nk - 1)), r=[pt_, Vt], w=[pacc])
                                if h == 3 and g0 + len(grpk) == nk:
                                    for hh in range(4):
                                        normalize_head(pacc, hh * 65, hh, cur["ya"])
                                    finish_ya(cur["ya"], qb)

                            pend = emit_st(items[0])
                            for k_ in range(len(items)):
                                nxt = emit_st(items[k_ + 1]) if k_ + 1 < len(items) else None
                                emit_pv(items[k_], pend)
                                pend = nxt
                            if grp == 0:
                                chk("p1_C")
                            for c in range(2):
                                S.dma("sp", YT[b, 4 + grp * 2 + c], yts[c][:], r=[yts[c]], w=[YTB[b]], add=True)

                        chk("p1_CD")
                        S.dma("sp", g1b[:], MODS[l][b:b + 1, 2048:3072].to_broadcast([128, D]), w=[g1b])
                        for (t0, n) in TCH:
                            if t0 >= T:
                                S.dma("sp", g1b[:], MODS[l][4:5, 2048:3072].to_broadcast([128, D]), w=[g1b])
                            yl = YTL.next()
                            for c in range(8):
                                S.dma("sp", yl[:, c, 0:n], YT[b, c][:, t0:t0 + n], r=[YTB[b]], w=[yl], add=(c > 0))
                            for i in range(t0 // 128, (t0 + n) // 128):
                                tt = i * 128 - t0
                                j = b if i < NTL else 4
                                gt = g1b
                                xtile = XT.next()
                                S.dma("sp", xtile[:], x_src(l, b, i), w=[xtile])
                                for half in range(2):
                                    ps = PS_MM.next()
                                    for k in range(8):
                                        S.op("pe", lambda e, k=k: e.matmul(ps[:, :], lhsT=yl[:, k, tt:tt + 128],
                                                                          rhs=woutb[:, k, half * 512:(half + 1) * 512],
                                                                          start=(k == 0), stop=(k == 7)), r=[yl, woutb], w=[ps])
                                    tm = TMPF.next()
                                    S.op("dve", lambda e: e.tensor_tensor(out=tm[:], in0=ps[:, :], in1=gt[:, half * 512:(half + 1) * 512],
                                                                          op=ALU.mult), r=[ps, gt], w=[tm])
                                    S.op("pool", lambda e: e.tensor_tensor(out=xtile[:, half * 512:(half + 1) * 512],
                                                                           in0=xtile[:, half * 512:(half + 1) * 512], in1=tm[:],
                                                                           op=ALU.add), r=[xtile, tm], w=[xtile])
                                S.dma("sp", XMID[b, i], xtile[:], r=[xtile])
                                if debug and b == 0 and l == 0:
                                    S.dma("sp", dbg["xmid"][i], xtile[:], r=[xtile])
                                xn = XNB.next()
                                h2 = H2T.next()
                                norm_to_T(xtile, h2, slice(0, 128), A2, 24, j, xn_keep=xn)
                                S.dma("sp", XN2[b, i], xn[:], r=[xn])
                                ps = PS_MM.next()
                                for k in range(8):
                                    S.op("pe", lambda e, k=k: e.matmul(ps[:, 0:NE], lhsT=h2[:, k, :], rhs=rtb[:, k, :],
                                                                      start=(k == 0), stop=(k == 7)), r=[h2, rtb], w=[ps])
                                ss = ST.next()
                                ex = TMPF.next()
                                S.op("pool", lambda e: e.memset(ss[:], 0.0), w=[ss])
                                S.op("act", lambda e: e.activation(out=ex[:, 0:NE], in_=ps[:, 0:NE], func=AF.Exp,
                                                                   accum_out=ss[:, 0:1]), r=[ps], w=[ex, ss])
                                S.op("dve", lambda e: e.reciprocal(out=ss[:, 1:2], in_=ss[:, 0:1]), r=[ss], w=[ss])
                                S.op("dve", lambda e: e.tensor_scalar(out=affTok[:, i, b * 16:(b + 1) * 16], in0=ex[:, 0:NE],
                                                                      scalar1=ss[:, 1:2], scalar2=None, op0=ALU.mult),
                                     r=[ex, ss], w=[affTok])
                    if debug and l == 0:
                        S.dma("sp", dbg["aff"][:, :, :], affTok[:], r=[affTok])
                        S.barrier()
                        for c in range(8):
                            t_ = YSTG.next()
                            S.dma("sp", t_[:], YT[0, c], w=[t_])
                            S.dma("sp", dbg["yt"][c], t_[:], r=[t_])
                    S.barrier()

                chk("p1")
                with ExitStack() as m1:
                    affT = sb("affT", [64, NTOK], F32, m1)
                    work = sb("work", [64, NTOK], F32, m1)
                    Gx = sb("Gx", [64, NTOK], F32, m1)
                    gtok = sb("gtok", [128, NTT, 64], F32, m1)
                    maskb = sb("maskb", [128, NTT, 64], BF16, m1)
                    slotTok = sb("slotTok", [128, NTT, 64], F32, m1)
                    MX = Pool([sb("mx%d" % i, [64, 8], F32, m1) for i in range(2)])
                    xn2 = sb("xn2", [128, NTT, D], BF16, m1)
                    PB = Pool([sb("pb%d" % i, [128, 256], BF16, m1) for i in range(NTL)])
                    PBC = Pool([sb("pbc%d" % i, [128, 32], BF16, m1) for i in range(2)])
                    XSTG = Pool([sb("xstg%d" % i, [128, 8, 256], BF16, m1) for i in range(2)])
                    XSTC = Pool([sb("xstc%d" % i, [128, 8, 32], BF16, m1) for i in range(2)])

                    def tr_in(dst, src_fn, rows_out, nt, ident_n, rbufs):
                        pass

                    for i0 in range(0, NTT, 4):
                        pt = PS_TR.next()
                        nn = min(4, NTT - i0)
                        for q in range(nn):
                            S.op("pe", lambda e, q=q: e.transpose(pt[0:64, q * 128:(q + 1) * 128], affTok[:, i0 + q, :], identf[:]),
                                 r=[affTok, identf], w=[pt])
                        S.op("act", lambda e: e.activation(out=affT[:, i0 * 128:(i0 + nn) * 128], in_=pt[0:64, 0:nn * 128], func=AF.Copy),
                             r=[pt], w=[affT])
                    S.op("dve", lambda e: e.tensor_copy(out=work[:], in_=affT[:]), r=[affT], w=[work])
                    for (s0, sn, rounds) in ((0, T, CAP // 8), (T, C, CAPC // 8)):
                        for r_ in range(rounds):
                            m8 = MX.next()
                            S.op("dve", lambda e: e.max(out=m8[:], in_=work[:, s0:s0 + sn]), r=[work], w=[m8])
                            S.op("dve", lambda e: e.match_replace(out=work[:, s0:s0 + sn], in_to_replace=m8[:],
                                                                  in_values=work[:, s0:s0 + sn], imm_value=0.0), r=[work, m8], w=[work])
                    S.op("dve", lambda e: e.tensor_tensor(out=Gx[:], in0=affT[:], in1=work[:], op=ALU.subtract), r=[affT, work], w=[Gx])
                    S.dma("sp", SLOTG[:, 1, :], Gx[:], r=[Gx])
                    for i0 in range(0, NTT, 4):
                        pt = PS_TR.next()
                        nn = min(4, NTT - i0)
                        for q in range(nn):
                            S.op("pe", lambda e, q=q: e.transpose(pt[:, q * 64:(q + 1) * 64], Gx[:, (i0 + q) * 128:(i0 + q + 1) * 128],
                                                                  identf[0:64, 0:64]), r=[Gx, identf], w=[pt])
                        S.op("act", lambda e: e.activation(out=gtok[:, i0:i0 + nn, :],
                                                           in_=pt[:, 0:nn * 64].rearrange("p (a c) -> p a c", c=64), func=AF.Copy),
                             r=[pt], w=[gtok])
                    S.op("dve", lambda e: e.tensor_scalar(out=maskb[:], in0=gtok[:], scalar1=0.0, scalar2=None, op0=ALU.is_gt),
                         r=[gtok], w=[maskb])
                    for i in range(NTT):
                        prev = list(range(0, i)) if i < NTL else list(range(NTL, i))
                        ps = PS_MM.next()
                        for ii, ip in enumerate(prev):
                            S.op("pe", lambda e, ip=ip, ii=ii: e.matmul(ps[:, 0:64], lhsT=onesb[:], rhs=maskb[:, ip, :],
                                                                        start=(ii == 0), stop=False), r=[onesb, maskb], w=[ps])
                        S.op("pe", lambda e: e.matmul(ps[:, 0:64], lhsT=masks[:, 0, :], rhs=maskb[:, i, :],
                                                      start=(len(prev) == 0), stop=True), r=[masks, maskb], w=[ps])
                        S.op("dve", lambda e: e.tensor_tensor(out=slotTok[:, i, :], in0=ps[:, 0:64], in1=maskb[:, i, :], op=ALU.mult),
                             r=[ps, maskb], w=[slotTok])
                    for i0 in range(0, NTT, 4):
                        pt = PS_TR.next()
                        nn = min(4, NTT - i0)
                        for q in range(nn):
                            S.op("pe", lambda e, q=q: e.transpose(pt[0:64, q * 128:(q + 1) * 128], slotTok[:, i0 + q, :], identf[:]),
                                 r=[slotTok, identf], w=[pt])
                        S.op("act", lambda e: e.activation(out=work[:, i0 * 128:(i0 + nn) * 128], in_=pt[0:64, 0:nn * 128], func=AF.Copy),
                             r=[pt], w=[work])
                    S.dma("sp", SLOTG[:, 0, :], work[:], r=[work])
                    if debug and l == 0:
                        S.dma("sp", dbg["slotg"][:, 0, :], work[:], r=[work])
                        S.dma("sp", dbg["slotg"][:, 1, :], Gx[:], r=[Gx])
                    chk("m1_topk")
                    for b in range(NS):
                        for i in range(NTT):
                            S.dma("sp", xn2[:, i, :], XN2[b, i], w=[xn2], add=(i > 0))
                        for ex_ in range(NE):
                            col = b * 16 + ex_
                            Ps = []
                            for i in range(NTL):
                                P = PB.next()
                                S.op("dve", lambda e: e.tensor_scalar(
                                    out=P[:], in0=iota[:, 0:256], scalar1=slotTok[:, i, col:col + 1], scalar2=None, op0=ALU.is_equal),
                                    r=[iota, slotTok], w=[P])
                                Ps.append(P)
                            stg = XSTG.next()
                            for cg in range(4):
                                pss = [PS_MM.next() for _ in range(2)]
                                for i in range(NTL):
                                    for cc in range(2):
                                        c = cg * 2 + cc
                                        S.op("pe", lambda e, c=c, cc=cc: e.matmul(pss[cc][:, 0:256],
                                                                                  lhsT=xn2[:, i, c * 128:(c + 1) * 128], rhs=Ps[i][:],
                                                                                  start=(i == 0), stop=(i == NTL - 1)),
                                             r=[xn2, Ps[i]], w=[pss[cc]])
                                for cc in range(2):
                                    c = cg * 2 + cc
                                    src = pss[cc][:, 0:256]
                                    if c % 2 == 0:
                                        S.op("act", lambda e, c=c, src=src: e.activation(
                                            out=stg[:, c, :], in_=src, func=AF.Identity, scale=A2[:, c, b:b + 1],
                                            bias=modsT[:, l, 24 + c, b:b + 1]), r=[pss[cc], A2, modsT], w=[stg])
                                    else:
                                        S.op("dve", lambda e, c=c, src=src: e.tensor_scalar(
                                            out=stg[:, c, :], in0=src, scalar1=A2[:, c, b:b + 1], scalar2=modsT[:, l, 24 + c, b:b + 1],
                                            op0=ALU.mult, op1=ALU.add), r=[pss[cc], A2, modsT], w=[stg])
                            for c in range(8):
                                S.dma("sp", XSG[ex_, c][:, b * CAP:(b + 1) * CAP], stg[:, c, :], r=[stg])
                            psc = PS_AC.next()
                            Pcs = []
                            for i in range(NTL, NTT):
                                Pc = PBC.next()
                                S.op("dve", lambda e: e.tensor_scalar(out=Pc[:], in0=iota[:, 0:32], scalar1=slotTok[:, i, col:col + 1],
                                                                       scalar2=None, op0=ALU.is_equal), r=[iota, slotTok], w=[Pc])
                                Pcs.append(Pc)
                            for c in range(8):
                                for ii, i in enumerate(range(NTL, NTT)):
                                    S.op("pe", lambda e, c=c, i=i, ii=ii: e.matmul(psc[:, c * 32:(c + 1) * 32],
                                                                                   lhsT=xn2[:, i, c * 128:(c + 1) * 128], rhs=Pcs[ii][:],
                                                                                   start=(i == NTL), stop=(i == NTT - 1)),
                                         r=[xn2, Pcs[ii]], w=[psc])
                            stc = XSTC.next()
                            for c in range(8):
                                S.op("dve", lambda e, c=c: e.tensor_scalar(
                                    out=stc[:, c, :], in0=psc[:, c * 32:(c + 1) * 32], scalar1=A2[:, c, 4:5],
                                    scalar2=modsT[:, l, 24 + c, 4:5], op0=ALU.mult, op1=ALU.add), r=[psc, A2, modsT], w=[stc])
                            o0 = NS * CAP + b * CAPC
                            S.dma("sp", XSG[ex_][:, :, o0:o0 + CAPC].rearrange("c p s -> p c s"), stc[:], r=[stc])
                    S.barrier()

                chk("m1")
                with ExitStack() as m2:
                    WG = [sb("wg%d" % i, [128, 8, D], BF16, m2) for i in range(2)]
                    WU = [sb("wu%d" % i, [128, 8, D], BF16, m2) for i in range(2)]
                    WD = [sb("wd%d" % i, [128, 8, D], BF16, m2) for i in range(2)]
                    XS = [sb("xs%d" % i, [128, 8, NSL], BF16, m2) for i in range(2)]
                    actT = sb("actT", [128, 8, NSL], BF16, m2)
                    SA = Pool([sb("sa%d" % i, [128, 512], F32, m2) for i in range(3)])
                    YS = Pool([sb("ys%d" % i, [128, D], BF16, m2) for i in range(2)])
                    SCH = [(s0, min(512, NSL - s0)) for s0 in range(0, NSL, 512)]
                    STL = [(s0, min(128, NSL - s0)) for s0 in range(0, NSL, 128)]
                    for ex_ in range(NE):
                        wg, wu, wd, xs = WG[ex_ % 2], WU[ex_ % 2], WD[ex_ % 2], XS[ex_ % 2]
                        dma_k("pool", wg, w_gate[l, ex_])
                        dma_k("pool", wu, w_up[l, ex_])
                        dma_k("pool", wd, w_down[l, ex_])
                        for c in range(8):
                            S.dma("sp", xs[:, c, :], XSG[ex_, c], w=[xs], add=(c > 0))
                        for (s0, n) in SCH:
                            for fc in range(8):
                                pA, pU = PS_MM.next(), PS_MM.next()
                                for k in range(8):
                                    S.op("pe", lambda e, k=k: e.matmul(pA[:, 0:n], lhsT=wg[:, k, fc * 128:(fc + 1) * 128],
                                                                      rhs=xs[:, k, s0:s0 + n], start=(k == 0), stop=(k == 7)),
                                         r=[wg, xs], w=[pA])
                                for k in range(8):
                                    S.op("pe", lambda e, k=k: e.matmul(pU[:, 0:n], lhsT=wu[:, k, fc * 128:(fc + 1) * 128],
                                                                      rhs=xs[:, k, s0:s0 + n], start=(k == 0), stop=(k == 7)),
                                         r=[wu, xs], w=[pU])
                                sa = SA.next()
                                S.op("act", lambda e: e.activation(out=sa[:, 0:n], in_=pA[:, 0:n], func=AF.Silu), r=[pA], w=[sa])
                                S.op("dve", lambda e: e.tensor_tensor(out=actT[:, fc, s0:s0 + n], in0=sa[:, 0:n], in1=pU[:, 0:n],
                                                                      op=ALU.mult), r=[sa, pU], w=[actT])
                        for (s0, rows) in STL:
                            ys = YS.next()
                            for half in range(2):
                                ps = PS_MM.next()
                                for fc in range(8):
                                    S.op("pe", lambda e, fc=fc: e.matmul(ps[0:rows, :], lhsT=actT[:, fc, s0:s0 + rows],
                                                                        rhs=wd[:, fc, half * 512:(half + 1) * 512],
                                                                        start=(fc == 0), stop=(fc == 7)), r=[actT, wd], w=[ps])
                                if half == 0:
                                    S.op("act", lambda e: e.activation(out=ys[0:rows, 0:512], in_=ps[0:rows, :], func=AF.Copy),
                                         r=[ps], w=[ys])
                                else:
                                    S.op("dve", lambda e: e.tensor_copy(out=ys[0:rows, 512:1024], in_=ps[0:rows, :]), r=[ps], w=[ys])
                            S.dma("sp", YEX[ex_][s0:s0 + rows, :], ys[0:rows, :], r=[ys])
                    S.barrier()

                chk("m2")
                with ExitStack() as m3:
                    selb = sb("selb", [16, 16 * 128], BF16, m3)
                    slot = sb("slot", [16, NTOK], BF16, m3)
                    gg = sb("gg", [16, NTOK], BF16, m3)
                    Yl = sb("Yl", [128, NE, 2, D], BF16, m3)
                    Yc = sb("Yc", [32, NE, D], BF16, m3)
                    PT = sb("PT", [128, NE, 2, 512], BF16, m3)
                    PTc = sb("PTc", [32, NE, 256], BF16, m3)
                    GB = Pool([sb("gb%d" % i, [128, 512], F32, m3) for i in range(3)])
                    g2b = sb("g2b", [128, D], F32, m3)
                    XT3 = Pool([sb("xt3_%d" % i, [128, D], F32, m3) for i in range(2)])
                    OT3 = Pool([sb("ot3_%d" % i, [128, D], F32, m3) for i in range(1)])
                    ST3 = Pool([sb("st3_%d" % i, [128, 8], F32, m3) for i in range(4)])
                    fgb = sb("fgb", [128, D], F32, m3)
                    S.dma("sp", fgb[:], final_g[0:1, :].to_broadcast([128, D]), w=[fgb])
                    S.dma("pool", selb[:], c_sel[:, :], w=[selb])
                    last = (l == DEPTH - 1)
                    for b in range(NS):
                        S.dma("pool", slot[:], SLOTG[b * 16:(b + 1) * 16, 0, :], w=[slot])
                        S.dma("pool", gg[:], SLOTG[b * 16:(b + 1) * 16, 1, :], w=[gg])
                        for ex_ in range(NE):
                            S.dma("sp", Yl[:, ex_], YEX[ex_][b * CAP:(b + 1) * CAP, :].rearrange("(k p) d -> p k d", p=128), w=[Yl],
                                  add=(ex_ > 0))
                        o0 = NS * CAP + b * CAPC
                        for ex_ in range(NE):
                            S.dma("sp", Yc[:, ex_, :], YEX[ex_][o0:o0 + CAPC, :], w=[Yc], add=(ex_ > 0))
                        S.dma("sp", g2b[:], MODS[l][b:b + 1, 5120:6144].to_broadcast([128, D]), w=[g2b])
                        for (t0, n) in TCH:
                            latent = t0 < T
                            if last and not latent:
                                continue
                            R = 128 if latent else 32
                            if not latent:
                                S.dma("sp", g2b[:], MODS[l][4:5, 5120:6144].to_broadcast([128, D]), w=[g2b])
                            for ex_ in range(NE):
                                psS, psG = PS_MM.next(), PS_MM.next()
                                S.op("pe", lambda e: e.matmul(psS[0:R, 0:n], lhsT=selb[:, ex_ * 128:ex_ * 128 + R], rhs=slot[:, t0:t0 + n],
                                                              start=True, stop=True), r=[selb, slot], w=[psS])
                                S.op("pe", lambda e: e.matmul(psG[0:R, 0:n], lhsT=selb[:, ex_ * 128:ex_ * 128 + R], rhs=gg[:, t0:t0 + n],
                                                              start=True, stop=True), r=[selb, gg], w=[psG])
                                gb = GB.next()
                                S.op("act", lambda e: e.activation(out=gb[0:R, 0:n], in_=psG[0:R, 0:n], func=AF.Copy), r=[psG], w=[gb])
                                if latent:
                                    for k in range(2):
                                        S.op("dve", lambda e, k=k: e.scalar_tensor_tensor(
                                            out=PT[:, ex_, k, 0:n], in0=psS[:, 0:n], scalar=iota[:, 256 + k:257 + k], in1=gb[:, 0:n],
                                            op0=ALU.is_equal, op1=ALU.mult), r=[psS, iota, gb], w=[PT])
                                else:
                                    S.op("dve", lambda e: e.scalar_tensor_tensor(
                                        out=PTc[:, ex_, 0:n], in0=psS[0:32, 0:n], scalar=iota[0:32, 256:257], in1=gb[0:32, 0:n],
                                        op0=ALU.is_equal, op1=ALU.mult), r=[psS, iota, gb], w=[PTc])
                            for i in range(t0 // 128, (t0 + n) // 128):
                                tt = i * 128 - t0
                                xtile = XT3.next()
                                S.dma("sp", xtile[:], XMID[b, i], w=[xtile])
                                gt = g2b
                                for half in range(2):
                                    ps = PS_AC.next()
                                    if latent:
                                        for ex_ in range(NE):
                                            for k in range(2):
                                                S.op("pe", lambda e, ex_=ex_, k=k: e.matmul(
                                                    ps[:, :], lhsT=PT[:, ex_, k, tt:tt + 128], rhs=Yl[:, ex_, k, half * 512:(half + 1) * 512],
                                                    start=(ex_ == 0 and k == 0), stop=(ex_ == NE - 1 and k == 1)), r=[PT, Yl], w=[ps])
                                    else:
                                        for ex_ in range(NE):
                                            S.op("pe", lambda e, ex_=ex_: e.matmul(
                                                ps[:, :], lhsT=PTc[:, ex_, tt:tt + 128], rhs=Yc[:, ex_, half * 512:(half + 1) * 512],
                                                start=(ex_ == 0), stop=(ex_ == NE - 1)), r=[PTc, Yc], w=[ps])
                                    tm = GB.next()
                                    S.op("dve", lambda e: e.tensor_tensor(out=tm[:], in0=ps[:, :], in1=gt[:, half * 512:(half + 1) * 512],
                                                                          op=ALU.mult), r=[ps, gt], w=[tm])
                                    S.op("pool", lambda e: e.tensor_tensor(out=xtile[:, half * 512:(half + 1) * 512],
                                                                           in0=xtile[:, half * 512:(half + 1) * 512], in1=tm[:],
                                                                           op=ALU.add), r=[xtile, tm], w=[xtile])
                                if debug and b == 0 and l == 0:
                                    S.dma("sp", dbg["xres"][i], xtile[:], r=[xtile])
                                if not last:
                                    S.dma("sp", XRES[b, i], xtile[:], r=[xtile])
                                else:
                                    ot = OT3.next()
                                    ss = ST3.next()
                                    S.op("pool", lambda e: e.memset(ss[:], 0.0), w=[ss])
                                    S.op("act", lambda e: e.activation(out=ot[:], in_=xtile[:], func=AF.Square, accum_out=ss[:, 0:1]),
                                         r=[xtile], w=[ot, ss])
                                    S.op("dve", lambda e: e.tensor_scalar(out=ss[:, 1:2], in0=ss[:, 0:1], scalar1=1.0 / D, scalar2=EPS,
                                                                          op0=ALU.mult, op1=ALU.add), r=[ss], w=[ss])
                                    S.op("dve", lambda e: e.reciprocal(out=ss[:, 2:3], in_=ss[:, 1:2]), r=[ss], w=[ss])
                                    S.op("act", lambda e: e.activation(out=ss[:, 3:4], in_=ss[:, 2:3], func=AF.Sqrt), r=[ss], w=[ss])
                                    S.op("dve", lambda e: e.scalar_tensor_tensor(out=ot[:], in0=xtile[:], scalar=ss[:, 3:4], in1=fgb[:],
                                                                                 op0=ALU.mult, op1=ALU.mult), r=[xtile, ss, fgb], w=[ot])
                                    S.dma("sp", out[b, i * 128:(i + 1) * 128, :], ot[:], r=[ot])
                    S.barrier()
        except _Stop:
            S.barrier()
        S.barrier(engines=["sp"])
        print("instr counts", S.cnt, "wait instrs", S.n_wait_ins, "ndma", sum(S.dma_val) // 16, flush=True)
    return nc


def _consts():
    identb = np.eye(128, dtype=np.float32).astype(ml_dtypes.bfloat16)
    identf = np.eye(128, dtype=np.float32)
    j = np.arange(128)[:, None]
    q = np.arange(128)[None, :]
    masks = np.stack([(j <= q), (j >= q)], axis=1).astype(np.float32).astype(ml_dtypes.bfloat16)
    iota = np.zeros((128, 258), np.float32)
    iota[:, 0:256] = np.arange(1, 257, dtype=np.float32)[None, :]
    iota[:, 256] = np.arange(1, 129)
    iota[:, 257] = np.arange(129, 257)
    pos = np.arange(T)
    row = (pos // 64).astype(np.float32)
    colp = (pos % 64).astype(np.float32)
    inv = (10000.0 ** (-np.arange(16, dtype=np.float32) / 16)).astype(np.float32)
    ar = row[:, None] * inv
    ac = colp[:, None] * inv
    ang = np.concatenate([ar, ar, ac, ac], axis=-1).astype(np.float32)
    cos = np.cos(ang).astype(np.float32)
    sin = np.sin(ang).astype(np.float32)
    sgn = np.tile(np.concatenate([-np.ones(16), np.ones(16)]), 2).astype(np.float32)
    ss = sin * sgn[None, :]
    cos6 = np.tile(cos, (1, 6))
    ss6 = np.tile(ss, (1, 6))
    rope = np.stack([cos6, ss6], axis=1).reshape(NTL, 128, 2, 384).astype(np.float32)
    sel = np.zeros((16, 16, 128), np.float32)
    for e in range(16):
        sel[e, e, :] = 1.0
    return dict(c_identb=identb, c_identf=identf, c_masks=masks, c_iota=iota, c_rope=rope,
                c_sel=sel.reshape(16, 16 * 128))


def _prep_shared(inp, DEPTH):
    f = lambda a: np.ascontiguousarray(np.asarray(a, dtype=np.float32))
    sh = {}
    sh["ada_w"] = f(inp["ada_w"][:DEPTH])
    sh["ada_b"] = f(inp["ada_b"][:DEPTH])
    rep = lambda g: f(np.repeat(np.asarray(g)[:DEPTH].reshape(DEPTH, 8, 128).transpose(0, 2, 1)[..., None], 5, axis=-1))
    sh["n1rep"] = rep(inp["norm1_g"])
    sh["n2rep"] = rep(inp["norm2_g"])
    sh["w_in"] = f(inp["w_in"][:DEPTH])
    cp = np.zeros((DEPTH, 128, 2, 40), np.float32)
    ca = np.asarray(inp["conv_a_w"])[:DEPTH]
    cb = np.asarray(inp["conv_b_w"])[:DEPTH]
    for h in range(2):
        cp[:, :, h, 0:3] = ca[:, :, h * 128:(h + 1) * 128].transpose(0, 2, 1)
        cp[:, :, h, 3:34] = cb[:, :, h * 128:(h + 1) * 128].transpose(0, 2, 1)
        cp[:, :, h, 34] = np.asarray(inp["conv_b_b"])[:DEPTH, h * 128:(h + 1) * 128]
        cp[:, :, h, 35] = np.asarray(inp["conv_ln_g"])[:DEPTH, h * 128:(h + 1) * 128]
        cp[:, :, h, 36] = np.asarray(inp["conv_ln_b"])[:DEPTH, h * 128:(h + 1) * 128]
    sh["convp"] = cp
    sh["sink"] = f(np.asarray(inp["sink"])[:DEPTH].reshape(1, DEPTH * 4))
    sh["qng"] = f(inp["q_norm_g"][:DEPTH])
    sh["kng"] = f(inp["k_norm_g"][:DEPTH])
    sh["w_out"] = f(inp["w_out"][:DEPTH])
    sh["router"] = f(inp["router_w"][:DEPTH])
    sh["w_gate"] = f(inp["w_gate"][:DEPTH])
    sh["w_up"] = f(inp["w_up"][:DEPTH])
    sh["w_down"] = f(inp["w_down"][:DEPTH])
    sh["final_g"] = f(np.asarray(inp["final_g"]).reshape(1, D))
    sh.update(_consts())
    return sh


def _core_inputs(inp, sh, b0, NS):
    m = dict(sh)
    m["x"] = np.ascontiguousarray(np.asarray(inp["x"][b0:b0 + NS], dtype=np.float32))
    m["ctx"] = np.ascontiguousarray(np.asarray(inp["ctx"][b0:b0 + NS], dtype=np.float32))
    call = np.zeros((5, D), np.float32)
    call[0:NS] = np.asarray(inp["c"][b0:b0 + NS])
    call[4] = np.asarray(inp["c_ctx"])
    m["cT"] = np.ascontiguousarray(call.reshape(5, 8, 128).transpose(2, 1, 0))
    return m


_NC_CACHE = {}


def kernel(**inputs):
    NS, DEPTH = 4, 4
    key = (NS, DEPTH)
    if key not in _NC_CACHE:
        _NC_CACHE[key] = build_program(NS, DEPTH)
    nc = _NC_CACHE[key]
    sh = _prep_shared(inputs, DEPTH)
    in_maps = [_core_inputs(inputs, sh, c * NS, NS) for c in range(N_CORES)]
    res = run_bass_kernel_spmd(nc, in_maps, core_ids=list(range(N_CORES)))
    outs = [np.asarray(r["out"]) for r in res.results]
    return np.concatenate(outs, axis=0).astype(np.float32)
```

```python
import numpy as np
import ml_dtypes
from contextlib import ExitStack
import concourse.bass as bass
import concourse.mybir as mybir
from concourse.bass_utils import run_bass_kernel_spmd

F32 = mybir.dt.float32
BF16 = mybir.dt.bfloat16
AF = mybir.ActivationFunctionType
ALU = mybir.AluOpType
AX = mybir.AxisListType

D = 1024
T = 2048
C = 256
NTOK = T + C
NTL = T // 128
NTT = NTOK // 128
NE = 16
CAP = 256
CAPC = 32
IN_W = 2304
EPS = 1e-6
N_CORES = 8
SAME_ENG_SYNC = True


class Buf:
    __slots__ = ("name", "w", "r")

    def __init__(self, name=""):
        self.name = name
        self.w = []
        self.r = {}


class Tl:
    def __init__(self, t, name=""):
        self.t = t
        self.b = Buf(name)

    def __getitem__(self, k):
        return self.t[k]


class Sync:
    W = 30000
    NDMA = 20

    def __init__(self, nc, es):
        self.nc = nc
        self.es = es
        self.engs = {"pe": nc.tensor, "act": nc.scalar, "dve": nc.vector, "pool": nc.gpsimd, "sp": nc.sync}
        self.cnt = {e: 0 for e in self.engs}
        self.sems = {e: [] for e in self.engs}
        self.known = {e: {} for e in self.engs}
        self.semid = {}
        self.dma_sems = [self._newsem("dma%d" % i) for i in range(self.NDMA)]
        self.dma_val = [0] * self.NDMA
        self.dma_i = 0
        self.n_wait_ins = 0
        self.dead = False

    def _newsem(self, name):
        s = self.es.enter_context(self.nc.semaphore(name))
        self.semid[id(s)] = len(self.semid)
        return s

    def _sid(self, s):
        return self.semid[id(s)]

    def _next_tok(self, eng):
        n = self.cnt[eng]
        k = n // self.W
        while len(self.sems[eng]) <= k:
            self.sems[eng].append(self._newsem("%s_m%d" % (eng, len(self.sems[eng]))))
        self.cnt[eng] = n + 1
        return (self.sems[eng][k], n % self.W + 1, eng)

    def _last_tok(self, eng):
        n = self.cnt[eng]
        if n == 0:
            return None
        n -= 1
        return (self.sems[eng][n // self.W], n % self.W + 1, eng)

    def _deps(self, r, w, add=False):
        toks = []
        for b in r:
            toks.extend(b.w)
        for b in w:
            if not add:
                toks.extend(b.w)
            toks.extend(b.r.values())
        return toks

    def _need(self, eng, toks):
        kn = self.known[eng]
        need = {}
        for (s, v, src) in toks:
            if src == eng and (eng == "pe" or not SAME_ENG_SYNC):
                continue
            sid = self._sid(s)
            if kn.get(sid, 0) >= v:
                continue
            if sid not in need or need[sid][1] < v:
                need[sid] = (s, v)
        return list(need.values())

    def _emit_waits(self, eng, waits, ins_fn):
        E = self.engs[eng]
        for (s, v) in waits[:-1]:
            E.wait_ge(s, v)
            self.n_wait_ins += 1
        ins = ins_fn(E)
        if waits:
            s, v = waits[-1]
            ins._wait_ge(s, v)
        kn = self.known[eng]
        for (s, v) in waits:
            sid = self._sid(s)
            if kn.get(sid, 0) < v:
                kn[sid] = v
        return ins

    def op(self, eng, fn, r=(), w=()):
        if self.dead:
            return None
        r = [x.b if isinstance(x, Tl) else x for x in r]
        w = [x.b if isinstance(x, Tl) else x for x in w]
        waits = self._need(eng, self._deps(r, w))
        ins = self._emit_waits(eng, waits, fn)
        tok = self._next_tok(eng)
        ins.then_inc(tok[0], 1)
        for b in r:
            b.r[eng] = tok
        for b in w:
            b.w = [tok]
            b.r = {}
        return ins

    def dma(self, eng, out, in_, r=(), w=(), add=False, **kw):
        if self.dead:
            return None
        r = [x.b if isinstance(x, Tl) else x for x in r]
        w = [x.b if isinstance(x, Tl) else x for x in w]
        toks = self._deps(r, w, add)
        i = self.dma_i
        self.dma_i = (i + 1) % self.NDMA
        s = self.dma_sems[i]
        if self.dma_val[i] > 0:
            toks.append((s, self.dma_val[i], "dma"))
        waits = self._need(eng, toks)
        ins = self._emit_waits(eng, waits, lambda E: E.dma_start(out=out, in_=in_, **kw))
        self.dma_val[i] += 16
        tok = (s, self.dma_val[i], "dma")
        ins.then_inc(s, 16)
        for b in r:
            b.r[("dma", i)] = tok
        for b in w:
            if add:
                b.w.append(tok)
            else:
                b.w = [tok]
                b.r = {}
        return ins

    def barrier(self, engines=None):
        toks = []
        for e in self.engs:
            t = self._last_tok(e)
            if t is not None:
                toks.append(t)
        for i in range(self.NDMA):
            if self.dma_val[i] > 0:
                toks.append((self.dma_sems[i], self.dma_val[i], "dma"))
        for e in (engines or list(self.engs)):
            kn = self.known[e]
            E = self.engs[e]
            for (s, v, src) in toks:
                if src == e:
                    continue
                sid = self._sid(s)
                if kn.get(sid, 0) >= v:
                    continue
                E.wait_ge(s, v)
                self.n_wait_ins += 1
                kn[sid] = v


class Pool:
    def __init__(self, tiles):
        self.tiles = tiles
        self.i = 0

    def next(self):
        t = self.tiles[self.i]
        self.i = (self.i + 1) % len(self.tiles)
        return t


class _Stop(Exception):
    pass


def build_program(NS=4, DEPTH=4, debug=False, stop=None):
    _sref = []

    _cur = [0]

    def chk(tag):
        if stop == tag or stop == "%s@%d" % (tag, _cur[0]):
            _sref[0].dead = True
    nc = bass.Bass("TRN2", target_bir_lowering=False)
    dt = lambda name, shape, dtype, kind="Internal": nc.dram_tensor(name, list(shape), dtype, kind=kind).ap()
    x_in = dt("x", [NS, T, D], F32, "ExternalInput")
    ctx_in = dt("ctx", [NS, C, D], F32, "ExternalInput")
    cT_in = dt("cT", [128, 8, 5], F32, "ExternalInput")
    ada_w = dt("ada_w", [DEPTH, D, 6 * D], F32, "ExternalInput")
    ada_b = dt("ada_b", [DEPTH, 6 * D], F32, "ExternalInput")
    n1rep = dt("n1rep", [DEPTH, 128, 8, 5], F32, "ExternalInput")
    n2rep = dt("n2rep", [DEPTH, 128, 8, 5], F32, "ExternalInput")
    w_in = dt("w_in", [DEPTH, D, IN_W], F32, "ExternalInput")
    convp = dt("convp", [DEPTH, 128, 2, 40], F32, "ExternalInput")
    sink_in = dt("sink", [1, DEPTH * 4], F32, "ExternalInput")
    qng = dt("qng", [DEPTH, 64], F32, "ExternalInput")
    kng = dt("kng", [DEPTH, 64], F32, "ExternalInput")
    w_out = dt("w_out", [DEPTH, D, D], F32, "ExternalInput")
    router = dt("router", [DEPTH, D, NE], F32, "ExternalInput")
    w_gate = dt("w_gate", [DEPTH, NE, D, D], F32, "ExternalInput")
    w_up = dt("w_up", [DEPTH, NE, D, D], F32, "ExternalInput")
    w_down = dt("w_down", [DEPTH, NE, D, D], F32, "ExternalInput")
    final_g = dt("final_g", [1, D], F32, "ExternalInput")
    c_identb = dt("c_identb", [128, 128], BF16, "ExternalInput")
    c_identf = dt("c_identf", [128, 128], F32, "ExternalInput")
    c_masks = dt("c_masks", [128, 2, 128], BF16, "ExternalInput")
    c_iota = dt("c_iota", [128, 258], F32, "ExternalInput")
    c_rope = dt("c_rope", [NTL, 128, 2, 384], F32, "ExternalInput")
    c_sel = dt("c_sel", [16, 16 * 128], F32, "ExternalInput")
    out = dt("out", [NS, T, D], F32, "ExternalOutput")
    dbg = {}
    if debug:
        dbg["xmid"] = dt("d_xmid", [NTT, 128, D], F32, "ExternalOutput")
        dbg["aff"] = dt("d_aff", [128, NTT, 64], F32, "ExternalOutput")
        dbg["yt"] = dt("d_yt", [8, 128, NTOK], BF16, "ExternalOutput")
        dbg["xres"] = dt("d_xres", [NTT, 128, D], F32, "ExternalOutput")
        dbg["slotg"] = dt("d_slotg", [64, 2, NTOK], F32, "ExternalOutput")
        dbg["qT"] = dt("d_qT", [128, 2, NTOK], BF16, "ExternalOutput")
        dbg["kT"] = dt("d_kT", [128, 1, NTOK], BF16, "ExternalOutput")
    MODS = dt("MODS", [DEPTH, 5, 6 * D], F32)
    XMID = dt("XMID", [NS, NTT, 128, D], F32)
    XRES = dt("XRES", [NS, NTT, 128, D], F32)
    XN2 = dt("XN2", [NS, NTT, 128, D], BF16)
    YT = dt("YT", [NS, 8, 128, NTOK], BF16)
    NSL = NS * CAP + NS * CAPC
    XSG = dt("XSG", [NE, 8, 128, NSL], BF16)
    YEX = dt("YEX", [NE, NSL, D], BF16)
    SLOTG = dt("SLOTG", [64, 2, NTOK], F32)

    with ExitStack() as es:
        S = Sync(nc, es)
        _sref.append(S)

        _uid = [0]

        def sb(name, shape, dtype, scope=es):
            _uid[0] += 1
            name = "%s_u%d" % (name, _uid[0])
            return Tl(scope.enter_context(nc.sbuf_tensor(name, list(shape), dtype)), name)

        def dma_k(eng, tile, src2d, ncols=None):
            for k in range(8):
                dst = tile[:, k, :] if ncols is None else tile[:, k, 0:ncols]
                S.dma(eng, dst, src2d[k * 128:(k + 1) * 128, :], w=[tile], add=(k > 0))

        psb = [Tl(es.enter_context(nc.psum_tensor("ps%d" % i, [128, 512], F32)), "ps%d" % i) for i in range(8)]
        PS_MM = Pool(psb[0:4])
        PS_TR = Pool(psb[4:6])
        PS_AC = Pool(psb[6:8])

        def bfv(ps):
            return ps.t.bitcast(BF16)

        identb = sb("identb", [128, 128], BF16)
        identf = sb("identf", [128, 128], F32)
        masks = sb("masks", [128, 2, 128], BF16)
        iota = sb("iota", [128, 258], F32)
        onesf = sb("onesf", [128, 128], F32)
        onesb = sb("onesb", [128, 128], BF16)
        modsT = sb("modsT", [128, DEPTH, 48, 5], F32)
        affTok = sb("affTok", [128, NTT, 64], F32)
        esink = sb("esink", [128, DEPTH * 4], F32)
        S.dma("sp", identb[:], c_identb[:, :], w=[identb])
        S.dma("sp", identf[:], c_identf[:, :], w=[identf])
        S.dma("sp", masks[:], c_masks[:, :, :], w=[masks])
        S.dma("sp", iota[:], c_iota[:, :], w=[iota])
        S.dma("sp", esink[:], sink_in[0:1, :].to_broadcast([128, DEPTH * 4]), w=[esink])
        S.op("dve", lambda e: e.memset(onesf[:], 1.0), w=[onesf])
        S.op("dve", lambda e: e.memset(onesb[:], 1.0), w=[onesb])
        S.op("dve", lambda e: e.memset(affTok[:], 0.0), w=[affTok])
        S.op("act", lambda e: e.activation(out=esink[:], in_=esink[:], func=AF.Exp), r=[esink], w=[esink])

        with ExitStack() as ps0:
            scT = sb("scT", [128, 8, 5], F32, ps0)
            adw = [sb("adw%d" % i, [128, 8, 512], F32, ps0) for i in range(2)]
            adb = [sb("adb%d" % i, [5, 512], F32, ps0) for i in range(2)]
            mrow = [sb("mrow%d" % i, [5, 512], F32, ps0) for i in range(2)]
            S.dma("sp", scT[:], cT_in[:, :, :], w=[scT])
            S.op("act", lambda e: e.activation(out=scT[:], in_=scT[:], func=AF.Silu), r=[scT], w=[scT])
            it = 0
            for l in range(DEPTH):
                for n in range(12):
                    wt, bt, mr = adw[it % 2], adb[it % 2], mrow[it % 2]
                    it += 1
                    dma_k("sp", wt, ada_w[l][:, n * 512:(n + 1) * 512])
                    S.dma("sp", bt[:], ada_b[l:l + 1, n * 512:(n + 1) * 512].to_broadcast([5, 512]), w=[bt])
                    ps = PS_MM.next()
                    for k in range(8):
                        S.op("pe", lambda e, k=k: e.matmul(ps[0:5, :], lhsT=scT[:, k, :], rhs=wt[:, k, :],
                                                          start=(k == 0), stop=(k == 7)), r=[scT, wt], w=[ps])
                    S.op("dve", lambda e: e.tensor_tensor(out=mr[:], in0=ps[0:5, :], in1=bt[:], op=ALU.add),
                         r=[ps, bt], w=[mr])
                    S.dma("sp", MODS[l][:, n * 512:(n + 1) * 512], mr[:], r=[mr])
                    pt = PS_TR.next()
                    for j in range(4):
                        S.op("pe", lambda e, j=j: e.transpose(pt[:, j * 5:(j + 1) * 5], mr[:, j * 128:(j + 1) * 128],
                                                              identf[0:5, 0:5]), r=[mr, identf], w=[pt])
                    S.op("act", lambda e: e.activation(
                        out=modsT[:, l, n * 4:(n + 1) * 4, :],
                        in_=pt[:, 0:20].rearrange("p (j s) -> p j s", s=5), func=AF.Copy), r=[pt], w=[modsT])
        S.barrier()

        try:
            chk("pro")
            def x_src(l, b, i):
                if l == 0:
                    return x_in[b, i * 128:(i + 1) * 128, :] if i < NTL else ctx_in[b, (i - NTL) * 128:(i - NTL + 1) * 128, :]
                return XRES[b, i]

            A1 = sb("A1", [128, 8, 5], F32)
            A2 = sb("A2", [128, 8, 5], F32)
            for l in range(DEPTH):
                _cur[0] = l
                with ExitStack() as p1:
                    YAG = Pool([sb("yag%d" % i, [128, 256], BF16, p1) for i in range(4)])
                    H2T = Pool([sb("h2t%d" % i, [128, 8, 128], BF16, p1) for i in range(2)])
                    YTB = [Buf("ytb%d" % i) for i in range(NS)]
                    nrep = sb("nrep", [128, 2, 8, 5], F32, p1)
                    cvp = sb("cvp", [128, 2, 40], F32, p1)
                    qg = sb("qg", [128, 64], F32, p1)
                    kg = sb("kg", [128, 64], F32, p1)
                    woutb = sb("woutb", [128, 8, D], BF16, p1)
                    rtb = sb("rtb", [128, 8, NE], BF16, p1)
                    hT = sb("hT", [128, 8, NTOK], BF16, p1)
                    xt = [sb("xt%d" % i, [128, D], F32, p1) for i in range(2)]
                    xnb = [sb("xnb%d" % i, [128, D], BF16, p1) for i in range(2)]
                    st = [sb("st%d" % i, [128, 8], F32, p1) for i in range(4)]
                    XT, XNB, ST = Pool(xt), Pool(xnb), Pool(st)
                    wch = [sb("wch%d" % i, [128, 8, 512], BF16, p1) for i in range(2)]
                    WCH = Pool(wch)
                    cw = [sb("cw%d" % i, [128, NTOK + 32], F32, p1) for i in range(3)]
                    upc = sb("upc", [128, C + 32], F32, p1)
                    ystg = [sb("ystg%d" % i, [128, NTOK], BF16, p1) for i in range(2)]
                    YSTG = Pool(ystg)
                    qT = sb("qT", [128, 2, NTOK], BF16, p1)
                    kT = sb("kT", [128, 1, NTOK], BF16, p1)
                    Vt = sb("Vt", [128, NTT, 2, 65], BF16, p1)
                    rope = [sb("rope%d" % i, [128, 2, 384], F32, p1) for i in range(2)]
                    ROPE = Pool(rope)
                    qk = [sb("qk%d" % i, [128, 384], F32, p1) for i in range(2)]
                    QK = Pool(qk)
                    qk2 = [sb("qkb%d" % i, [128, 384], F32, p1) for i in range(2)]
                    QK2 = Pool(qk2)
                    qkr = [sb("qkr%d" % i, [128, 384], BF16, p1) for i in range(2)]
                    QKR = Pool(qkr)
                    ptl = [sb("ptl%d" % i, [128, 512], BF16, p1) for i in range(4)]
                    PTL = Pool(ptl)
                    yat = [sb("yat%d" % i, [128, 256], BF16, p1) for i in range(2)]
                    YAT = Pool(yat)
                    g1b = sb("g1b", [128, D], F32, p1)
                    YTL = WCH
                    tmpf = [sb("tmpf%d" % i, [128, 512], F32, p1) for i in range(6)]
                    TMPF = Pool(tmpf)

                    S.dma("sp", nrep[:, 0], n1rep[l], w=[nrep])
                    S.dma("sp", nrep[:, 1], n2rep[l], w=[nrep])
                    S.dma("sp", cvp[:], convp[l], w=[cvp])
                    S.dma("sp", qg[:], qng[l:l + 1, :].to_broadcast([128, 64]), w=[qg])
                    S.dma("sp", kg[:], kng[l:l + 1, :].to_broadcast([128, 64]), w=[kg])
                    dma_k("pool", woutb, w_out[l])
                    S.dma("pool", rtb[:], router[l].rearrange("(k p) f -> p k f", p=128), w=[rtb])
                    S.op("dve", lambda e: e.scalar_tensor_tensor(out=A1[:], in0=modsT[:, l, 8:16, :], scalar=1.0,
                                                                 in1=nrep[:, 0], op0=ALU.add, op1=ALU.mult),
                         r=[modsT, nrep], w=[A1])
                    S.op("dve", lambda e: e.scalar_tensor_tensor(out=A2[:], in0=modsT[:, l, 32:40, :], scalar=1.0,
                                                                 in1=nrep[:, 1], op0=ALU.add, op1=ALU.mult),
                         r=[modsT, nrep], w=[A2])
                    S.op("dve", lambda e: e.memset(Vt[:], 1.0), w=[Vt])

                    def norm_to_T(src_tile, dstT, dcols, A, Bofs, j, xn_keep=None):
                        ss = ST.next()
                        xn = xn_keep if xn_keep is not None else XNB.next()
                        S.op("pool", lambda e: e.memset(ss[:], 0.0), w=[ss])
                        S.op("act", lambda e: e.activation(out=xn[:], in_=src_tile[:], func=AF.Square,
                                                           accum_out=ss[:, 0:1]), r=[src_tile], w=[xn, ss])
                        S.op("dve", lambda e: e.tensor_scalar(out=ss[:, 1:2], in0=ss[:, 0:1], scalar1=1.0 / D, scalar2=EPS,
                                                              op0=ALU.mult, op1=ALU.add), r=[ss], w=[ss])
                        S.op("dve", lambda e: e.reciprocal(out=ss[:, 2:3], in_=ss[:, 1:2]), r=[ss], w=[ss])
                        S.op("act", lambda e: e.activation(out=ss[:, 3:4], in_=ss[:, 2:3], func=AF.Sqrt), r=[ss], w=[ss])
                        S.op("act", lambda e: e.activation(out=xn[:], in_=src_tile[:], func=AF.Copy, scale=ss[:, 3:4]),
                             r=[src_tile, ss], w=[xn])
                        pt = PS_TR.next()
                        ptb = bfv(pt)
                        for c in range(8):
                            S.op("pe", lambda e, c=c: e.transpose(ptb[:, c * 128:(c + 1) * 128], xn[:, c * 128:(c + 1) * 128],
                                                                  identb[:]), r=[xn, identb], w=[pt])
                        for c in range(8):
                            if c % 2 == 0:
                                S.op("act", lambda e, c=c: e.activation(
                                    out=dstT[:, c, dcols], in_=ptb[:, c * 128:(c + 1) * 128], func=AF.Identity,
                                    scale=A[:, c, j:j + 1], bias=modsT[:, l, Bofs + c, j:j + 1]), r=[pt, A, modsT], w=[dstT])
                            else:
                                S.op("dve", lambda e, c=c: e.tensor_scalar(
                                    out=dstT[:, c, dcols], in0=ptb[:, c * 128:(c + 1) * 128],
                                    scalar1=A[:, c, j:j + 1], scalar2=modsT[:, l, Bofs + c, j:j + 1],
                                    op0=ALU.mult, op1=ALU.add), r=[pt, A, modsT], w=[dstT])
                        return xn

                    TCH = [(0, 512), (512, 512), (1024, 512), (1536, 512), (2048, 256)]
                    SEGS = [(0, T), (T, C)]

                    def colp(t):
                        return 16 + t if t < T else 16 + t + 0

                    for b in range(NS):
                        for i in range(NTT):
                            xtile = XT.next()
                            S.dma("sp", xtile[:], x_src(l, b, i), w=[xtile])
                            norm_to_T(xtile, hT, slice(i * 128, (i + 1) * 128), A1, 0, b if i < NTL else 4)

                        chk("p1_norm")
                        def inproj_fm(fc, consume):
                            wt = WCH.next()
                            dma_k("pool", wt, w_in[l][:, fc * 128:(fc + 1) * 128], 128)
                            for (t0, n) in TCH:
                                ps = PS_MM.next()
                                for k in range(8):
                                    S.op("pe", lambda e, k=k: e.matmul(ps[:, 0:n], lhsT=wt[:, k, 0:128], rhs=hT[:, k, t0:t0 + n],
                                                                      start=(k == 0), stop=(k == 7)), r=[wt, hT], w=[ps])
                                consume(ps, t0, n)

                        for h in range(2):
                            prod, cv = cw[0], cw[1]
                            S.op("pool", lambda e: e.memset(prod[:, 0:1], 0.0), w=[prod])
                            S.op("pool", lambda e: e.memset(prod[:, T + 1:T + 3], 0.0), w=[prod])
                            S.op("pool", lambda e: e.memset(prod[:, T + 3 + C:T + 4 + C], 0.0), w=[prod])

                            def offA(t0):
                                return 1 + t0 if t0 < T else T + 3 + (t0 - T)
                            inproj_fm(2 + h, lambda ps, t0, n: S.op(
                                "act", lambda e: e.activation(out=prod[:, offA(t0):offA(t0) + n], in_=ps[:, 0:n], func=AF.Copy),
                                r=[ps], w=[prod]))
                            inproj_fm(4 + h, lambda ps, t0, n: S.op(
                                "dve", lambda e: e.tensor_tensor(out=prod[:, offA(t0):offA(t0) + n], in0=ps[:, 0:n],
                                                                 in1=prod[:, offA(t0):offA(t0) + n], op=ALU.mult), r=[ps, prod], w=[prod]))
                            for (s0, sn) in SEGS:
                                o0 = offA(s0)
                                S.op("dve", lambda e: e.tensor_scalar(out=cv[:, s0:s0 + sn], in0=prod[:, o0 - 1:o0 - 1 + sn],
                                                                      scalar1=cvp[:, h, 0:1], scalar2=None, op0=ALU.mult),
                                     r=[prod, cvp], w=[cv])
                                for kk in (1, 2):
                                    S.op("dve", lambda e, kk=kk: e.scalar_tensor_tensor(
                                        out=cv[:, s0:s0 + sn], in0=prod[:, o0 - 1 + kk:o0 - 1 + kk + sn], scalar=cvp[:, h, kk:kk + 1],
                                        in1=cv[:, s0:s0 + sn], op0=ALU.mult, op1=ALU.add), r=[prod, cvp, cv], w=[cv])
                            ys = YSTG.next()
                            inproj_fm(0 + h, lambda ps, t0, n: S.op(
                                "dve", lambda e: e.tensor_tensor(out=ys[:, t0:t0 + n], in0=ps[:, 0:n], in1=cv[:, t0:t0 + n],
                                                                 op=ALU.mult), r=[ps, cv], w=[ys]))
                            S.dma("sp", YT[b, h], ys[:], r=[ys], w=[YTB[b]], add=True)
                        chk("p1_A")
                        ucs = [cw[1], cw[2]]
                        for h in range(2):
                            upad, uc = cw[0], ucs[h]
                            S.op("pool", lambda e: e.memset(upad[:, 0:15], 0.0), w=[upad])
                            S.op("pool", lambda e: e.memset(upad[:, 15 + T:30 + T], 0.0), w=[upad])
                            S.op("pool", lambda e: e.memset(upc[:, 0:15], 0.0), w=[upc])
                            S.op("pool", lambda e: e.memset(upc[:, 15 + C:30 + C], 0.0), w=[upc])

                            def dstB(t0, n):
                                return (upad, upad[:, 15 + t0:15 + t0 + n]) if t0 < T else (upc, upc[:, 15:15 + n])

                            def consB1(ps, t0, n):
                                tl, ap = dstB(t0, n)
                                S.op("act", lambda e: e.activation(out=ap, in_=ps[:, 0:n], func=AF.Sigmoid), r=[ps], w=[tl])

                            def consB2(ps, t0, n):
                                tl, ap = dstB(t0, n)
                                S.op("dve", lambda e: e.tensor_tensor(out=ap, in0=ps[:, 0:n], in1=ap, op=ALU.mult), r=[ps, tl], w=[tl])
                            inproj_fm(8 + h, consB1)
                            inproj_fm(6 + h, consB2)
                            for (src, s0, sn, eng) in ((upad, 0, T, "dve"), (upc, T, C, "dve")):
                                S.op(eng, lambda e: e.tensor_scalar(out=uc[:, s0:s0 + sn], in0=src[:, 0:sn],
                                                                    scalar1=cvp[:, h, 3:4], scalar2=cvp[:, h, 34:35],
                                                                    op0=ALU.mult, op1=ALU.add), r=[src, cvp], w=[uc])
                                for kk in range(1, 31):
                                    S.op(eng, lambda e, kk=kk: e.scalar_tensor_tensor(
                                        out=uc[:, s0:s0 + sn], in0=src[:, kk:kk + sn], scalar=cvp[:, h, 3 + kk:4 + kk],
                                        in1=uc[:, s0:s0 + sn], op0=ALU.mult, op1=ALU.add), r=[src, cvp, uc], w=[uc])
                        ysb = [YSTG.next(), YSTG.next()]
                        for (t0, n) in TCH:
                            p1s, p2s = PS_MM.next(), PS_MM.next()
                            for h in range(2):
                                S.op("pe", lambda e, h=h: e.matmul(p1s[:, 0:n], lhsT=onesf[:], rhs=ucs[h][:, t0:t0 + n],
                                                                  start=(h == 0), stop=(h == 1)), r=[onesf, ucs[h]], w=[p1s])
                            for h in range(2):
                                sqt = TMPF.next()
                                S.op("act", lambda e, h=h: e.activation(out=sqt[:, 0:n], in_=ucs[h][:, t0:t0 + n], func=AF.Square),
                                     r=[ucs[h]], w=[sqt])
                                S.op("pe", lambda e, h=h: e.matmul(p2s[:, 0:n], lhsT=onesf[:], rhs=sqt[:, 0:n],
                                                                  start=(h == 0), stop=(h == 1)), r=[onesf, sqt], w=[p2s])
                            mean, var, dd = TMPF.next(), TMPF.next(), TMPF.next()
                            S.op("act", lambda e: e.activation(out=mean[:, 0:n], in_=p1s[:, 0:n], func=AF.Copy, scale=1.0 / 256),
                                 r=[p1s], w=[mean])
                            S.op("dve", lambda e: e.tensor_tensor(out=var[:, 0:n], in0=mean[:, 0:n], in1=mean[:, 0:n], op=ALU.mult),
                                 r=[mean], w=[var])
                            S.op("dve", lambda e: e.scalar_tensor_tensor(out=var[:, 0:n], in0=p2s[:, 0:n], scalar=1.0 / 256,
                                                                         in1=var[:, 0:n], op0=ALU.mult, op1=ALU.subtract),
                                 r=[p2s, var], w=[var])
                            S.op("dve", lambda e: e.tensor_scalar(out=var[:, 0:n], in0=var[:, 0:n], scalar1=EPS, scalar2=None,
                                                                  op0=ALU.add), r=[var], w=[var])
                            S.op("dve", lambda e: e.reciprocal(out=var[:, 0:n], in_=var[:, 0:n]), r=[var], w=[var])
                            S.op("act", lambda e: e.activation(out=var[:, 0:n], in_=var[:, 0:n], func=AF.Sqrt), r=[var], w=[var])
                            for h in range(2):
                                S.op("dve", lambda e, h=h: e.tensor_tensor(out=dd[:, 0:n], in0=ucs[h][:, t0:t0 + n], in1=mean[:, 0:n],
                                                                            op=ALU.subtract), r=[ucs[h], mean], w=[dd])
                                S.op("dve", lambda e: e.tensor_tensor(out=dd[:, 0:n], in0=dd[:, 0:n], in1=var[:, 0:n], op=ALU.mult),
                                     r=[dd, var], w=[dd])
                                S.op("act", lambda e, h=h: e.activation(out=ysb[h][:, t0:t0 + n], in_=dd[:, 0:n], func=AF.Silu,
                                                                        scale=cvp[:, h, 35:36], bias=cvp[:, h, 36:37]),
                                     r=[dd, cvp], w=[ysb[h]])
                        for h in range(2):
                            S.dma("sp", YT[b, 2 + h], ysb[h][:], r=[ysb[h]], w=[YTB[b]], add=True)

                        chk("p1_B")
                        for grp in range(2):
                            wt = WCH.next()
                            c0 = 1280 + grp * 512
                            dma_k("pool", wt, w_in[l][:, c0:c0 + 512])
                            for i in range(NTT):
                                ps = PS_MM.next()
                                for k in range(8):
                                    S.op("pe", lambda e, k=k: e.matmul(ps[:, :], lhsT=hT[:, k, i * 128:(i + 1) * 128], rhs=wt[:, k, :],
                                                                      start=(k == 0), stop=(k == 7)), r=[hT, wt], w=[ps])
                                S.op("act", lambda e: e.activation(out=Vt[:, i, :, 0:64],
                                                                   in_=ps[:, 384:512].rearrange("p (a d) -> p a d", d=64),
                                                                   func=AF.Copy), r=[ps], w=[Vt])
                                q1 = QK.next()
                                S.op("act", lambda e: e.activation(out=q1[:], in_=ps[:, 0:384], func=AF.Copy), r=[ps], w=[q1])
                                if grp == 1:
                                    sq = QK2.next()
                                    ss = ST.next()
                                    S.op("dve", lambda e: e.tensor_tensor(out=sq[:], in0=q1[:], in1=q1[:], op=ALU.mult), r=[q1], w=[sq])
                                    S.op("dve", lambda e: e.tensor_reduce(out=ss[:, 0:6], in_=sq[:].rearrange("p (a d) -> p a d", d=64),
                                                                          axis=AX.X, op=ALU.add), r=[sq], w=[ss])
                                    S.op("dve", lambda e: e.tensor_scalar(out=ss[:, 0:6], in0=ss[:, 0:6], scalar1=1.0 / 64, scalar2=EPS,
                                                                          op0=ALU.mult, op1=ALU.add), r=[ss], w=[ss])
                                    S.op("dve", lambda e: e.reciprocal(out=ss[:, 0:6], in_=ss[:, 0:6]), r=[ss], w=[ss])
                                    S.op("act", lambda e: e.activation(out=ss[:, 0:6], in_=ss[:, 0:6], func=AF.Sqrt), r=[ss], w=[ss])
                                    for hh in range(6):
                                        gt = qg if hh < 4 else kg
                                        S.op("dve", lambda e, hh=hh, gt=gt: e.scalar_tensor_tensor(
                                            out=q1[:, hh * 64:(hh + 1) * 64], in0=q1[:, hh * 64:(hh + 1) * 64], scalar=ss[:, hh:hh + 1],
                                            in1=gt[:], op0=ALU.mult, op1=ALU.mult), r=[q1, ss, gt], w=[q1])
                                qr = QKR.next()
                                if i < NTL:
                                    rp = ROPE.next()
                                    S.dma("sp", rp[:], c_rope[i], w=[rp])
                                    t1, t2 = QK2.next(), QK2.next()
                                    v4 = lambda ap: ap.rearrange("p (a j q) -> p a j q", j=2, q=16)
                                    S.op("dve", lambda e: e.tensor_tensor(out=t1[:], in0=q1[:], in1=rp[:, 0, :], op=ALU.mult),
                                         r=[q1, rp], w=[t1])
                                    S.op("dve", lambda e: e.tensor_tensor(out=v4(t2[:])[:, :, 0, :], in0=v4(q1[:])[:, :, 1, :],
                                                                           in1=v4(rp[:, 1, :])[:, :, 0, :], op=ALU.mult), r=[q1, rp], w=[t2])
                                    S.op("dve", lambda e: e.tensor_tensor(out=v4(t2[:])[:, :, 1, :], in0=v4(q1[:])[:, :, 0, :],
                                                                           in1=v4(rp[:, 1, :])[:, :, 1, :], op=ALU.mult), r=[q1, rp], w=[t2])
                                    pq_o = lambda ap: ap[:, 0:256].rearrange("p (g k d) -> p k g d", g=2, k=2)
                                    pq_i = lambda ap: ap[:, 0:256].rearrange("p (k g d) -> p k g d", g=2, k=2)
                                    S.op("dve", lambda e: e.tensor_tensor(out=pq_o(qr[:]), in0=pq_i(t1[:]), in1=pq_i(t2[:]), op=ALU.add),
                                         r=[t1, t2], w=[qr])
                                    S.op("dve", lambda e: e.tensor_tensor(out=qr[:, 256:384], in0=t1[:, 256:384], in1=t2[:, 256:384],
                                                                          op=ALU.add), r=[t1, t2], w=[qr])
                                else:
                                    pq_o = lambda ap: ap[:, 0:256].rearrange("p (g k d) -> p k g d", g=2, k=2)
                                    pq_i = lambda ap: ap[:, 0:256].rearrange("p (k g d) -> p k g d", g=2, k=2)
                                    S.op("dve", lambda e: e.tensor_copy(out=pq_o(qr[:]), in_=pq_i(q1[:])), r=[q1], w=[qr])
                                    S.op("dve", lambda e: e.tensor_copy(out=qr[:, 256:384], in_=q1[:, 256:384]), r=[q1], w=[qr])
                                pt = PS_TR.next()
                                ptb = bfv(pt)
                                for hh in range(3):
                                    S.op("pe", lambda e, hh=hh: e.transpose(ptb[:, hh * 128:(hh + 1) * 128], qr[:, hh * 128:(hh + 1) * 128],
                                                                            identb[:]), r=[qr, identb], w=[pt])
                                S.op("act", lambda e: e.activation(out=qT[:, :, i * 128:(i + 1) * 128],
                                                                   in_=ptb[:, 0:256].rearrange("p (a t) -> p a t", t=128),
                                                                   func=AF.Copy), r=[pt], w=[qT])
                                S.op("dve", lambda e: e.tensor_copy(out=kT[:, 0, i * 128:(i + 1) * 128], in_=ptb[:, 256:384]),
                                     r=[pt], w=[kT])
                            if debug and grp == 1 and b == 0 and l == 0:
                                S.dma("sp", dbg["qT"][:, :, :], qT[:], r=[qT])
                                S.dma("sp", dbg["kT"][:, :, :], kT[:], r=[kT])
                            if grp == 0:
                                chk("p1_C0")
                            else:
                                chk("p1_D0")
                            yts = [YSTG.next(), YSTG.next()]

                            def normalize_head(pa, co, h, ya):
                                den = ST.next()
                                if grp == 0:
                                    S.op("act", lambda e: e.activation(out=den[:, 0:1], in_=pa[:, co + 64:co + 65], func=AF.Identity,
                                                                       bias=esink[:, l * 4 + h:l * 4 + h + 1]),
                                         r=[pa, esink], w=[den])
                                    S.op("dve", lambda e: e.reciprocal(out=den[:, 1:2], in_=den[:, 0:1]), r=[den], w=[den])
                                else:
                                    S.op("act", lambda e: e.activation(out=den[:, 0:1], in_=pa[:, co + 64:co + 65], func=AF.Copy),
                                         r=[pa], w=[den])
                                    S.op("dve", lambda e: e.reciprocal(out=den[:, 1:2], in_=den[:, 0:1]), r=[den], w=[den])
                                S.op("act", lambda e: e.activation(out=ya[:, h * 64:(h + 1) * 64], in_=pa[:, co:co + 64],
                                                                   func=AF.Copy, scale=den[:, 1:2]), r=[pa, den], w=[ya])

                            def finish_ya(ya, qb):
                                pt = PS_TR.next()
                                ptb = bfv(pt)
                                for c in range(2):
                                    S.op("pe", lambda e, c=c: e.transpose(ptb[:, c * 128:(c + 1) * 128], ya[:, c * 128:(c + 1) * 128],
                                                                          identb[:]), r=[ya, identb], w=[pt])
                                for c in range(2):
                                    S.op("dve", lambda e, c=c: e.tensor_copy(out=yts[c][:, qb * 128:(qb + 1) * 128],
                                                                             in_=ptb[:, c * 128:(c + 1) * 128]), r=[pt], w=[yts[c]])

                            items = []
                            for qb in range(NTT):
                                if qb >= NTL:
                                    kts = [NTL, NTL + 1]
                                elif grp == 0:
                                    kts = [m for m in (qb - 1, qb, qb + 1) if 0 <= m < NTL] + [NTL, NTL + 1]
                                else:
                                    kts = list(range(NTT))
                                nk = len(kts)
                                for h in range(4):
                                    for g0 in range(0, nk, 4):
                                        items.append((qb, h, g0, kts[g0:g0 + 4], nk))

                            def emit_st(item):
                                qb, h, g0, grpk, nk = item
                                kh = h // 2
                                ps = PS_MM.next()
                                for ki, m in enumerate(grpk):
                                    S.op("pe", lambda e, m=m, ki=ki: e.matmul(
                                        ps[:, ki * 128:(ki + 1) * 128], lhsT=kT[kh * 64:(kh + 1) * 64, 0, m * 128:(m + 1) * 128],
                                        rhs=qT[kh * 64:(kh + 1) * 64, h % 2, qb * 128:(qb + 1) * 128], start=True, stop=True),
                                        r=[kT, qT], w=[ps])
                                pt_ = PTL.next()
                                nn_ = len(grpk) * 128
                                S.op("act", lambda e: e.activation(out=pt_[:, 0:nn_], in_=ps[:, 0:nn_], func=AF.Exp, scale=0.125),
                                     r=[ps], w=[pt_])
                                if grp == 0 and qb < NTL:
                                    for ki, m in enumerate(grpk):
                                        if (m == qb - 1 or m == qb + 1) and m < NTL:
                                            mi = 1 if m == qb - 1 else 0
                                            S.op("dve", lambda e, ki=ki, mi=mi: e.tensor_tensor(
                                                out=pt_[:, ki * 128:(ki + 1) * 128], in0=pt_[:, ki * 128:(ki + 1) * 128],
                                                in1=masks[:, mi, :], op=ALU.mult), r=[pt_, masks], w=[pt_])
                                return pt_

                            cur = {}

                            def emit_pv(item, pt_):
                                qb, h, g0, grpk, nk = item
                                kh = h // 2
                                if h == 0 and g0 == 0:
                                    cur["pacc"] = PS_AC.next()
                                    cur["ya"] = YAT.next()
                                pacc = cur["pacc"]
                                for ki, m in enumerate(grpk):
                                    S.op("pe", lambda e, ki=ki, m=m: e.matmul(
                                        pacc[:, h * 65:(h + 1) * 65], lhsT=pt_[:, ki * 128:(ki + 1) * 128], rhs=Vt[:, m, kh, :],
                                        start=(g0 + ki == 0), stop=(g0 + ki == nk - 1)), r=[pt_, Vt], w=[pacc])
                                if h == 3 and g0 + len(grpk) == nk:
                                    for hh in range(4):
                                        normalize_head(pacc, hh * 65, hh, cur["ya"])
                                    finish_ya(cur["ya"], qb)

                            pend = emit_st(items[0])
                            for k_ in range(len(items)):
                                nxt = emit_st(items[k_ + 1]) if k_ + 1 < len(items) else None
                                emit_pv(items[k_], pend)
                                pend = nxt
                            if grp == 0:
                                chk("p1_C")
                            for c in range(2):
                                S.dma("sp", YT[b, 4 + grp * 2 + c], yts[c][:], r=[yts[c]], w=[YTB[b]], add=True)

                        chk("p1_CD")
                        S.dma("sp", g1b[:], MODS[l][b:b + 1, 2048:3072].to_broadcast([128, D]), w=[g1b])
                        for (t0, n) in TCH:
                            if t0 >= T:
                                S.dma("sp", g1b[:], MODS[l][4:5, 2048:3072].to_broadcast([128, D]), w=[g1b])
                            yl = YTL.next()
                            for c in range(8):
                                S.dma("sp", yl[:, c, 0:n], YT[b, c][:, t0:t0 + n], r=[YTB[b]], w=[yl], add=(c > 0))
                            for i in range(t0 // 128, (t0 + n) // 128):
                                tt = i * 128 - t0
                                j = b if i < NTL else 4
                                gt = g1b
                                xtile = XT.next()
                                S.dma("sp", xtile[:], x_src(l, b, i), w=[xtile])
                                for half in range(2):
                                    ps = PS_MM.next()
                                    for k in range(8):
                                        S.op("pe", lambda e, k=k: e.matmul(ps[:, :], lhsT=yl[:, k, tt:tt + 128],
                                                                          rhs=woutb[:, k, half * 512:(half + 1) * 512],
                                                                          start=(k == 0), stop=(k == 7)), r=[yl, woutb], w=[ps])
                                    tm = TMPF.next()
                                    S.op("dve", lambda e: e.tensor_tensor(out=tm[:], in0=ps[:, :], in1=gt[:, half * 512:(half + 1) * 512],
                                                                          op=ALU.mult), r=[ps, gt], w=[tm])
                                    S.op("dve", lambda e: e.tensor_tensor(out=xtile[:, half * 512:(half + 1) * 512],
                                                                           in0=xtile[:, half * 512:(half + 1) * 512], in1=tm[:],
                                                                           op=ALU.add), r=[xtile, tm], w=[xtile])
                                S.dma("sp", XMID[b, i], xtile[:], r=[xtile])
                                if debug and b == 0 and l == 0:
                                    S.dma("sp", dbg["xmid"][i], xtile[:], r=[xtile])
                                xn = XNB.next()
                                h2 = H2T.next()
                                norm_to_T(xtile, h2, slice(0, 128), A2, 24, j, xn_keep=xn)
                                S.dma("sp", XN2[b, i], xn[:], r=[xn])
                                ps = PS_MM.next()
                                for k in range(8):
                                    S.op("pe", lambda e, k=k: e.matmul(ps[:, 0:NE], lhsT=h2[:, k, :], rhs=rtb[:, k, :],
                                                                      start=(k == 0), stop=(k == 7)), r=[h2, rtb], w=[ps])
                                ss = ST.next()
                                ex = TMPF.next()
                                S.op("pool", lambda e: e.memset(ss[:], 0.0), w=[ss])
                                S.op("act", lambda e: e.activation(out=ex[:, 0:NE], in_=ps[:, 0:NE], func=AF.Exp,
                                                                   accum_out=ss[:, 0:1]), r=[ps], w=[ex, ss])
                                S.op("dve", lambda e: e.reciprocal(out=ss[:, 1:2], in_=ss[:, 0:1]), r=[ss], w=[ss])
                                S.op("dve", lambda e: e.tensor_scalar(out=affTok[:, i, b * 16:(b + 1) * 16], in0=ex[:, 0:NE],
                                                                      scalar1=ss[:, 1:2], scalar2=None, op0=ALU.mult),
                                     r=[ex, ss], w=[affTok])
                    if debug and l == 0:
                        S.dma("sp", dbg["aff"][:, :, :], affTok[:], r=[affTok])
                        S.barrier()
                        for c in range(8):
                            t_ = YSTG.next()
                            S.dma("sp", t_[:], YT[0, c], w=[t_])
                            S.dma("sp", dbg["yt"][c], t_[:], r=[t_])
                    S.barrier()

                chk("p1")
                with ExitStack() as m1:
                    affT = sb("affT", [64, NTOK], F32, m1)
                    work = sb("work", [64, NTOK], F32, m1)
                    Gx = sb("Gx", [64, NTOK], F32, m1)
                    gtok = sb("gtok", [128, NTT, 64], F32, m1)
                    maskb = sb("maskb", [128, NTT, 64], BF16, m1)
                    slotTok = sb("slotTok", [128, NTT, 64], F32, m1)
                    MX = Pool([sb("mx%d" % i, [64, 8], F32, m1) for i in range(2)])
                    xn2 = sb("xn2", [128, NTT, D], BF16, m1)
                    PB = Pool([sb("pb%d" % i, [128, 256], BF16, m1) for i in range(NTL)])
                    PBC = Pool([sb("pbc%d" % i, [128, 32], BF16, m1) for i in range(2)])
                    XSTG = Pool([sb("xstg%d" % i, [128, 8, 256], BF16, m1) for i in range(2)])
                    XSTC = Pool([sb("xstc%d" % i, [128, 8, 32], BF16, m1) for i in range(2)])

                    def tr_in(dst, src_fn, rows_out, nt, ident_n, rbufs):
                        pass

                    for i0 in range(0, NTT, 4):
                        pt = PS_TR.next()
                        nn = min(4, NTT - i0)
                        for q in range(nn):
                            S.op("pe", lambda e, q=q: e.transpose(pt[0:64, q * 128:(q + 1) * 128], affTok[:, i0 + q, :], identf[:]),
                                 r=[affTok, identf], w=[pt])
                        S.op("act", lambda e: e.activation(out=affT[:, i0 * 128:(i0 + nn) * 128], in_=pt[0:64, 0:nn * 128], func=AF.Copy),
                             r=[pt], w=[affT])
                    S.op("dve", lambda e: e.tensor_copy(out=work[:], in_=affT[:]), r=[affT], w=[work])
                    for (s0, sn, rounds) in ((0, T, CAP // 8), (T, C, CAPC // 8)):
                        for r_ in range(rounds):
                            m8 = MX.next()
                            S.op("dve", lambda e: e.max(out=m8[:], in_=work[:, s0:s0 + sn]), r=[work], w=[m8])
                            S.op("dve", lambda e: e.match_replace(out=work[:, s0:s0 + sn], in_to_replace=m8[:],
                                                                  in_values=work[:, s0:s0 + sn], imm_value=0.0), r=[work, m8], w=[work])
                    S.op("dve", lambda e: e.tensor_tensor(out=Gx[:], in0=affT[:], in1=work[:], op=ALU.subtract), r=[affT, work], w=[Gx])
                    S.dma("sp", SLOTG[:, 1, :], Gx[:], r=[Gx])
                    for i0 in range(0, NTT, 4):
                        pt = PS_TR.next()
                        nn = min(4, NTT - i0)
                        for q in range(nn):
                            S.op("pe", lambda e, q=q: e.transpose(pt[:, q * 64:(q + 1) * 64], Gx[:, (i0 + q) * 128:(i0 + q + 1) * 128],
                                                                  identf[0:64, 0:64]), r=[Gx, identf], w=[pt])
                        S.op("act", lambda e: e.activation(out=gtok[:, i0:i0 + nn, :],
                                                           in_=pt[:, 0:nn * 64].rearrange("p (a c) -> p a c", c=64), func=AF.Copy),
                             r=[pt], w=[gtok])
                    S.op("dve", lambda e: e.tensor_scalar(out=maskb[:], in0=gtok[:], scalar1=0.0, scalar2=None, op0=ALU.is_gt),
                         r=[gtok], w=[maskb])
                    for i in range(NTT):
                        prev = list(range(0, i)) if i < NTL else list(range(NTL, i))
                        ps = PS_MM.next()
                        for ii, ip in enumerate(prev):
                            S.op("pe", lambda e, ip=ip, ii=ii: e.matmul(ps[:, 0:64], lhsT=onesb[:], rhs=maskb[:, ip, :],
                                                                        start=(ii == 0), stop=False), r=[onesb, maskb], w=[ps])
                        S.op("pe", lambda e: e.matmul(ps[:, 0:64], lhsT=masks[:, 0, :], rhs=maskb[:, i, :],
                                                      start=(len(prev) == 0), stop=True), r=[masks, maskb], w=[ps])
                        S.op("dve", lambda e: e.tensor_tensor(out=slotTok[:, i, :], in0=ps[:, 0:64], in1=maskb[:, i, :], op=ALU.mult),
                             r=[ps, maskb], w=[slotTok])
                    for i0 in range(0, NTT, 4):
                        pt = PS_TR.next()
                        nn = min(4, NTT - i0)
                        for q in range(nn):
                            S.op("pe", lambda e, q=q: e.transpose(pt[0:64, q * 128:(q + 1) * 128], slotTok[:, i0 + q, :], identf[:]),
                                 r=[slotTok, identf], w=[pt])
                        S.op("act", lambda e: e.activation(out=work[:, i0 * 128:(i0 + nn) * 128], in_=pt[0:64, 0:nn * 128], func=AF.Copy),
                             r=[pt], w=[work])
                    S.dma("sp", SLOTG[:, 0, :], work[:], r=[work])
                    if debug and l == 0:
                        S.dma("sp", dbg["slotg"][:, 0, :], work[:], r=[work])
                        S.dma("sp", dbg["slotg"][:, 1, :], Gx[:], r=[Gx])
                    chk("m1_topk")
                    for b in range(NS):
                        for i in range(NTT):
                            S.dma("sp", xn2[:, i, :], XN2[b, i], w=[xn2], add=(i > 0))
                        for ex_ in range(NE):
                            col = b * 16 + ex_
                            Ps = []
                            for i in range(NTL):
                                P = PB.next()
                                S.op("dve", lambda e: e.tensor_scalar(
                                    out=P[:], in0=iota[:, 0:256], scalar1=slotTok[:, i, col:col + 1], scalar2=None, op0=ALU.is_equal),
                                    r=[iota, slotTok], w=[P])
                                Ps.append(P)
                            stg = XSTG.next()
                            for cg in range(4):
                                pss = [PS_MM.next() for _ in range(2)]
                                for i in range(NTL):
                                    for cc in range(2):
                                        c = cg * 2 + cc
                                        S.op("pe", lambda e, c=c, cc=cc: e.matmul(pss[cc][:, 0:256],
                                                                                  lhsT=xn2[:, i, c * 128:(c + 1) * 128], rhs=Ps[i][:],
                                                                                  start=(i == 0), stop=(i == NTL - 1)),
                                             r=[xn2, Ps[i]], w=[pss[cc]])
                                for cc in range(2):
                                    c = cg * 2 + cc
                                    src = pss[cc][:, 0:256]
                                    if c % 2 == 0:
                                        S.op("act", lambda e, c=c, src=src: e.activation(
                                            out=stg[:, c, :], in_=src, func=AF.Identity, scale=A2[:, c, b:b + 1],
                                            bias=modsT[:, l, 24 + c, b:b + 1]), r=[pss[cc], A2, modsT], w=[stg])
                                    else:
                                        S.op("dve", lambda e, c=c, src=src: e.tensor_scalar(
                                            out=stg[:, c, :], in0=src, scalar1=A2[:, c, b:b + 1], scalar2=modsT[:, l, 24 + c, b:b + 1],
                                            op0=ALU.mult, op1=ALU.add), r=[pss[cc], A2, modsT], w=[stg])
                            for c in range(8):
                                S.dma("sp", XSG[ex_, c][:, b * CAP:(b + 1) * CAP], stg[:, c, :], r=[stg])
                            psc = PS_AC.next()
                            Pcs = []
                            for i in range(NTL, NTT):
                                Pc = PBC.next()
                                S.op("dve", lambda e: e.tensor_scalar(out=Pc[:], in0=iota[:, 0:32], scalar1=slotTok[:, i, col:col + 1],
                                                                       scalar2=None, op0=ALU.is_equal), r=[iota, slotTok], w=[Pc])
                                Pcs.append(Pc)
                            for c in range(8):
                                for ii, i in enumerate(range(NTL, NTT)):
                                    S.op("pe", lambda e, c=c, i=i, ii=ii: e.matmul(psc[:, c * 32:(c + 1) * 32],
                                                                                   lhsT=xn2[:, i, c * 128:(c + 1) * 128], rhs=Pcs[ii][:],
                                                                                   start=(i == NTL), stop=(i == NTT - 1)),
                                         r=[xn2, Pcs[ii]], w=[psc])
                            stc = XSTC.next()
                            for c in range(8):
                                S.op("dve", lambda e, c=c: e.tensor_scalar(
                                    out=stc[:, c, :], in0=psc[:, c * 32:(c + 1) * 32], scalar1=A2[:, c, 4:5],
                                    scalar2=modsT[:, l, 24 + c, 4:5], op0=ALU.mult, op1=ALU.add), r=[psc, A2, modsT], w=[stc])
                            o0 = NS * CAP + b * CAPC
                            S.dma("sp", XSG[ex_][:, :, o0:o0 + CAPC].rearrange("c p s -> p c s"), stc[:], r=[stc])
                    S.barrier()

                chk("m1")
                with ExitStack() as m2:
                    WG = [sb("wg%d" % i, [128, 8, D], BF16, m2) for i in range(2)]
                    WU = [sb("wu%d" % i, [128, 8, D], BF16, m2) for i in range(2)]
                    WD = [sb("wd%d" % i, [128, 8, D], BF16, m2) for i in range(2)]
                    XS = [sb("xs%d" % i, [128, 8, NSL], BF16, m2) for i in range(2)]
                    actT = sb("actT", [128, 8, NSL], BF16, m2)
                    SA = Pool([sb("sa%d" % i, [128, 512], F32, m2) for i in range(3)])
                    YS = Pool([sb("ys%d" % i, [128, D], BF16, m2) for i in range(2)])
                    SCH = [(s0, min(512, NSL - s0)) for s0 in range(0, NSL, 512)]
                    STL = [(s0, min(128, NSL - s0)) for s0 in range(0, NSL, 128)]
                    for ex_ in range(NE):
                        wg, wu, wd, xs = WG[ex_ % 2], WU[ex_ % 2], WD[ex_ % 2], XS[ex_ % 2]
                        dma_k("pool", wg, w_gate[l, ex_])
                        dma_k("pool", wu, w_up[l, ex_])
                        dma_k("pool", wd, w_down[l, ex_])
                        for c in range(8):
                            S.dma("sp", xs[:, c, :], XSG[ex_, c], w=[xs], add=(c > 0))
                        for (s0, n) in SCH:
                            for fc in range(8):
                                pA, pU = PS_MM.next(), PS_MM.next()
                                for k in range(8):
                                    S.op("pe", lambda e, k=k: e.matmul(pA[:, 0:n], lhsT=wg[:, k, fc * 128:(fc + 1) * 128],
                                                                      rhs=xs[:, k, s0:s0 + n], start=(k == 0), stop=(k == 7)),
                                         r=[wg, xs], w=[pA])
                                for k in range(8):
                                    S.op("pe", lambda e, k=k: e.matmul(pU[:, 0:n], lhsT=wu[:, k, fc * 128:(fc + 1) * 128],
                                                                      rhs=xs[:, k, s0:s0 + n], start=(k == 0), stop=(k == 7)),
                                         r=[wu, xs], w=[pU])
                                sa = SA.next()
                                S.op("act", lambda e: e.activation(out=sa[:, 0:n], in_=pA[:, 0:n], func=AF.Silu), r=[pA], w=[sa])
                                S.op("dve", lambda e: e.tensor_tensor(out=actT[:, fc, s0:s0 + n], in0=sa[:, 0:n], in1=pU[:, 0:n],
                                                                      op=ALU.mult), r=[sa, pU], w=[actT])
                        for (s0, rows) in STL:
                            ys = YS.next()
                            for half in range(2):
                                ps = PS_MM.next()
                                for fc in range(8):
                                    S.op("pe", lambda e, fc=fc: e.matmul(ps[0:rows, :], lhsT=actT[:, fc, s0:s0 + rows],
                                                                        rhs=wd[:, fc, half * 512:(half + 1) * 512],
                                                                        start=(fc == 0), stop=(fc == 7)), r=[actT, wd], w=[ps])
                                if half == 0:
                                    S.op("act", lambda e: e.activation(out=ys[0:rows, 0:512], in_=ps[0:rows, :], func=AF.Copy),
                                         r=[ps], w=[ys])
                                else:
                                    S.op("dve", lambda e: e.tensor_copy(out=ys[0:rows, 512:1024], in_=ps[0:rows, :]), r=[ps], w=[ys])
                            S.dma("sp", YEX[ex_][s0:s0 + rows, :], ys[0:rows, :], r=[ys])
                    S.barrier()

                chk("m2")
                with ExitStack() as m3:
                    selb = sb("selb", [16, 16 * 128], BF16, m3)
                    slot = sb("slot", [16, NTOK], BF16, m3)
                    gg = sb("gg", [16, NTOK], BF16, m3)
                    Yl = sb("Yl", [128, NE, 2, D], BF16, m3)
                    Yc = sb("Yc", [32, NE, D], BF16, m3)
                    PT = sb("PT", [128, NE, 2, 512], BF16, m3)
                    PTc = sb("PTc", [32, NE, 256], BF16, m3)
                    GB = Pool([sb("gb%d" % i, [128, 512], F32, m3) for i in range(3)])
                    g2b = sb("g2b", [128, D], F32, m3)
                    XT3 = Pool([sb("xt3_%d" % i, [128, D], F32, m3) for i in range(2)])
                    OT3 = Pool([sb("ot3_%d" % i, [128, D], F32, m3) for i in range(1)])
                    ST3 = Pool([sb("st3_%d" % i, [128, 8], F32, m3) for i in range(4)])
                    fgb = sb("fgb", [128, D], F32, m3)
                    S.dma("sp", fgb[:], final_g[0:1, :].to_broadcast([128, D]), w=[fgb])
                    S.dma("pool", selb[:], c_sel[:, :], w=[selb])
                    last = (l == DEPTH - 1)
                    for b in range(NS):
                        S.dma("pool", slot[:], SLOTG[b * 16:(b + 1) * 16, 0, :], w=[slot])
                        S.dma("pool", gg[:], SLOTG[b * 16:(b + 1) * 16, 1, :], w=[gg])
                        for ex_ in range(NE):
                            S.dma("sp", Yl[:, ex_], YEX[ex_][b * CAP:(b + 1) * CAP, :].rearrange("(k p) d -> p k d", p=128), w=[Yl],
                                  add=(ex_ > 0))
                        o0 = NS * CAP + b * CAPC
                        for ex_ in range(NE):
                            S.dma("sp", Yc[:, ex_, :], YEX[ex_][o0:o0 + CAPC, :], w=[Yc], add=(ex_ > 0))
                        S.dma("sp", g2b[:], MODS[l][b:b + 1, 5120:6144].to_broadcast([128, D]), w=[g2b])
                        for (t0, n) in TCH:
                            latent = t0 < T
                            if last and not latent:
                                continue
                            R = 128 if latent else 32
                            if not latent:
                                S.dma("sp", g2b[:], MODS[l][4:5, 5120:6144].to_broadcast([128, D]), w=[g2b])
                            for ex_ in range(NE):
                                psS, psG = PS_MM.next(), PS_MM.next()
                                S.op("pe", lambda e: e.matmul(psS[0:R, 0:n], lhsT=selb[:, ex_ * 128:ex_ * 128 + R], rhs=slot[:, t0:t0 + n],
                                                              start=True, stop=True), r=[selb, slot], w=[psS])
                                S.op("pe", lambda e: e.matmul(psG[0:R, 0:n], lhsT=selb[:, ex_ * 128:ex_ * 128 + R], rhs=gg[:, t0:t0 + n],
                                                              start=True, stop=True), r=[selb, gg], w=[psG])
                                gb = GB.next()
                                S.op("act", lambda e: e.activation(out=gb[0:R, 0:n], in_=psG[0:R, 0:n], func=AF.Copy), r=[psG], w=[gb])
                                if latent:
                                    for k in range(2):
                                        S.op("dve", lambda e, k=k: e.scalar_tensor_tensor(
                                            out=PT[:, ex_, k, 0:n], in0=psS[:, 0:n], scalar=iota[:, 256 + k:257 + k], in1=gb[:, 0:n],
                                            op0=ALU.is_equal, op1=ALU.mult), r=[psS, iota, gb], w=[PT])
                                else:
                                    S.op("dve", lambda e: e.scalar_tensor_tensor(
                                        out=PTc[:, ex_, 0:n], in0=psS[0:32, 0:n], scalar=iota[0:32, 256:257], in1=gb[0:32, 0:n],
                                        op0=ALU.is_equal, op1=ALU.mult), r=[psS, iota, gb], w=[PTc])
                            for i in range(t0 // 128, (t0 + n) // 128):
                                tt = i * 128 - t0
                                xtile = XT3.next()
                                S.dma("sp", xtile[:], XMID[b, i], w=[xtile])
                                gt = g2b
                                for half in range(2):
                                    ps = PS_AC.next()
                                    if latent:
                                        for ex_ in range(NE):
                                            for k in range(2):
                                                S.op("pe", lambda e, ex_=ex_, k=k: e.matmul(
                                                    ps[:, :], lhsT=PT[:, ex_, k, tt:tt + 128], rhs=Yl[:, ex_, k, half * 512:(half + 1) * 512],
                                                    start=(ex_ == 0 and k == 0), stop=(ex_ == NE - 1 and k == 1)), r=[PT, Yl], w=[ps])
                                    else:
                                        for ex_ in range(NE):
                                            S.op("pe", lambda e, ex_=ex_: e.matmul(
                                                ps[:, :], lhsT=PTc[:, ex_, tt:tt + 128], rhs=Yc[:, ex_, half * 512:(half + 1) * 512],
                                                start=(ex_ == 0), stop=(ex_ == NE - 1)), r=[PTc, Yc], w=[ps])
                                    tm = GB.next()
                                    S.op("dve", lambda e: e.tensor_tensor(out=tm[:], in0=ps[:, :], in1=gt[:, half * 512:(half + 1) * 512],
                                                                          op=ALU.mult), r=[ps, gt], w=[tm])
                                    S.op("dve", lambda e: e.tensor_tensor(out=xtile[:, half * 512:(half + 1) * 512],
                                                                           in0=xtile[:, half * 512:(half + 1) * 512], in1=tm[:],
                                                                           op=ALU.add), r=[xtile, tm], w=[xtile])
                                if debug and b == 0 and l == 0:
                                    S.dma("sp", dbg["xres"][i], xtile[:], r=[xtile])
                                if not last:
                                    S.dma("sp", XRES[b, i], xtile[:], r=[xtile])
                                else:
                                    ot = OT3.next()
                                    ss = ST3.next()
                                    S.op("pool", lambda e: e.memset(ss[:], 0.0), w=[ss])
                                    S.op("act", lambda e: e.activation(out=ot[:], in_=xtile[:], func=AF.Square, accum_out=ss[:, 0:1]),
                                         r=[xtile], w=[ot, ss])
                                    S.op("dve", lambda e: e.tensor_scalar(out=ss[:, 1:2], in0=ss[:, 0:1], scalar1=1.0 / D, scalar2=EPS,
                                                                          op0=ALU.mult, op1=ALU.add), r=[ss], w=[ss])
                                    S.op("dve", lambda e: e.reciprocal(out=ss[:, 2:3], in_=ss[:, 1:2]), r=[ss], w=[ss])
                                    S.op("act", lambda e: e.activation(out=ss[:, 3:4], in_=ss[:, 2:3], func=AF.Sqrt), r=[ss], w=[ss])
                                    S.op("dve", lambda e: e.scalar_tensor_tensor(out=ot[:], in0=xtile[:], scalar=ss[:, 3:4], in1=fgb[:],
                                                                                 op0=ALU.mult, op1=ALU.mult), r=[xtile, ss, fgb], w=[ot])
                                    S.dma("sp", out[b, i * 128:(i + 1) * 128, :], ot[:], r=[ot])
                    S.barrier()
        except _Stop:
            S.barrier()
        S.barrier(engines=["sp"])
        print("instr counts", S.cnt, "wait instrs", S.n_wait_ins, "ndma", sum(S.dma_val) // 16, flush=True)
    return nc


def _consts():
    identb = np.eye(128, dtype=np.float32).astype(ml_dtypes.bfloat16)
    identf = np.eye(128, dtype=np.float32)
    j = np.arange(128)[:, None]
    q = np.arange(128)[None, :]
    masks = np.stack([(j <= q), (j >= q)], axis=1).astype(np.float32).astype(ml_dtypes.bfloat16)
    iota = np.zeros((128, 258), np.float32)
    iota[:, 0:256] = np.arange(1, 257, dtype=np.float32)[None, :]
    iota[:, 256] = np.arange(1, 129)
    iota[:, 257] = np.arange(129, 257)
    pos = np.arange(T)
    row = (pos // 64).astype(np.float32)
    colp = (pos % 64).astype(np.float32)
    inv = (10000.0 ** (-np.arange(16, dtype=np.float32) / 16)).astype(np.float32)
    ar = row[:, None] * inv
    ac = colp[:, None] * inv
    ang = np.concatenate([ar, ar, ac, ac], axis=-1).astype(np.float32)
    cos = np.cos(ang).astype(np.float32)
    sin = np.sin(ang).astype(np.float32)
    sgn = np.tile(np.concatenate([-np.ones(16), np.ones(16)]), 2).astype(np.float32)
    ss = sin * sgn[None, :]
    cos6 = np.tile(cos, (1, 6))
    ss6 = np.tile(ss, (1, 6))
    rope = np.stack([cos6, ss6], axis=1).reshape(NTL, 128, 2, 384).astype(np.float32)
    sel = np.zeros((16, 16, 128), np.float32)
    for e in range(16):
        sel[e, e, :] = 1.0
    return dict(c_identb=identb, c_identf=identf, c_masks=masks, c_iota=iota, c_rope=rope,
                c_sel=sel.reshape(16, 16 * 128))


def _prep_shared(inp, DEPTH):
    f = lambda a: np.ascontiguousarray(np.asarray(a, dtype=np.float32))
    sh = {}
    sh["ada_w"] = f(inp["ada_w"][:DEPTH])
    sh["ada_b"] = f(inp["ada_b"][:DEPTH])
    rep = lambda g: f(np.repeat(np.asarray(g)[:DEPTH].reshape(DEPTH, 8, 128).transpose(0, 2, 1)[..., None], 5, axis=-1))
    sh["n1rep"] = rep(inp["norm1_g"])
    sh["n2rep"] = rep(inp["norm2_g"])
    sh["w_in"] = f(inp["w_in"][:DEPTH])
    cp = np.zeros((DEPTH, 128, 2, 40), np.float32)
    ca = np.asarray(inp["conv_a_w"])[:DEPTH]
    cb = np.asarray(inp["conv_b_w"])[:DEPTH]
    for h in range(2):
        cp[:, :, h, 0:3] = ca[:, :, h * 128:(h + 1) * 128].transpose(0, 2, 1)
        cp[:, :, h, 3:34] = cb[:, :, h * 128:(h + 1) * 128].transpose(0, 2, 1)
        cp[:, :, h, 34] = np.asarray(inp["conv_b_b"])[:DEPTH, h * 128:(h + 1) * 128]
        cp[:, :, h, 35] = np.asarray(inp["conv_ln_g"])[:DEPTH, h * 128:(h + 1) * 128]
        cp[:, :, h, 36] = np.asarray(inp["conv_ln_b"])[:DEPTH, h * 128:(h + 1) * 128]
    sh["convp"] = cp
    sh["sink"] = f(np.asarray(inp["sink"])[:DEPTH].reshape(1, DEPTH * 4))
    sh["qng"] = f(inp["q_norm_g"][:DEPTH])
    sh["kng"] = f(inp["k_norm_g"][:DEPTH])
    sh["w_out"] = f(inp["w_out"][:DEPTH])
    sh["router"] = f(inp["router_w"][:DEPTH])
    sh["w_gate"] = f(inp["w_gate"][:DEPTH])
    sh["w_up"] = f(inp["w_up"][:DEPTH])
    sh["w_down"] = f(inp["w_down"][:DEPTH])
    sh["final_g"] = f(np.asarray(inp["final_g"]).reshape(1, D))
    sh.update(_consts())
    return sh


def _core_inputs(inp, sh, b0, NS):
    m = dict(sh)
    m["x"] = np.ascontiguousarray(np.asarray(inp["x"][b0:b0 + NS], dtype=np.float32))
    m["ctx"] = np.ascontiguousarray(np.asarray(inp["ctx"][b0:b0 + NS], dtype=np.float32))
    call = np.zeros((5, D), np.float32)
    call[0:NS] = np.asarray(inp["c"][b0:b0 + NS])
    call[4] = np.asarray(inp["c_ctx"])
    m["cT"] = np.ascontiguousarray(call.reshape(5, 8, 128).transpose(2, 1, 0))
    return m


_NC_CACHE = {}


def kernel(**inputs):
    NS, DEPTH = 4, 4
    key = (NS, DEPTH)
    if key not in _NC_CACHE:
        _NC_CACHE[key] = build_program(NS, DEPTH)
    nc = _NC_CACHE[key]
    sh = _prep_shared(inputs, DEPTH)
    in_maps = [_core_inputs(inputs, sh, c * NS, NS) for c in range(N_CORES)]
    res = run_bass_kernel_spmd(nc, in_maps, core_ids=list(range(N_CORES)))
    outs = [np.asarray(r["out"]) for r in res.results]
    return np.concatenate(outs, axis=0).astype(np.float32)
```

```python
import numpy as np
import ml_dtypes
from contextlib import ExitStack
import concourse.bass as bass
import concourse.mybir as mybir
from concourse.bass_utils import run_bass_kernel_spmd

F32 = mybir.dt.float32
BF16 = mybir.dt.bfloat16
AF = mybir.ActivationFunctionType
ALU = mybir.AluOpType
AX = mybir.AxisListType

D = 1024
T = 2048
C = 256
NTOK = T + C
NTL = T // 128
NTT = NTOK // 128
NE = 16
CAP = 256
CAPC = 32
IN_W = 2304
EPS = 1e-6
N_CORES = 8
SAME_ENG_SYNC = True


class Buf:
    __slots__ = ("name", "w", "r")

    def __init__(self, name=""):
        self.name = name
        self.w = []
        self.r = {}


class Tl:
    def __init__(self, t, name=""):
        self.t = t
        self.b = Buf(name)

    def __getitem__(self, k):
        return self.t[k]


class Sync:
    W = 30000
    NDMA = 20

    def __init__(self, nc, es):
        self.nc = nc
        self.es = es
        self.engs = {"pe": nc.tensor, "act": nc.scalar, "dve": nc.vector, "pool": nc.gpsimd, "sp": nc.sync}
        self.cnt = {e: 0 for e in self.engs}
        self.sems = {e: [] for e in self.engs}
        self.known = {e: {} for e in self.engs}
        self.semid = {}
        self.dma_sems = [self._newsem("dma%d" % i) for i in range(self.NDMA)]
        self.dma_val = [0] * self.NDMA
        self.dma_i = 0
        self.n_wait_ins = 0
        self.dead = False

    def _newsem(self, name):
        s = self.es.enter_context(self.nc.semaphore(name))
        self.semid[id(s)] = len(self.semid)
        return s

    def _sid(self, s):
        return self.semid[id(s)]

    def _next_tok(self, eng):
        n = self.cnt[eng]
        k = n // self.W
        while len(self.sems[eng]) <= k:
            self.sems[eng].append(self._newsem("%s_m%d" % (eng, len(self.sems[eng]))))
        self.cnt[eng] = n + 1
        return (self.sems[eng][k], n % self.W + 1, eng)

    def _last_tok(self, eng):
        n = self.cnt[eng]
        if n == 0:
            return None
        n -= 1
        return (self.sems[eng][n // self.W], n % self.W + 1, eng)

    def _deps(self, r, w, add=False):
        toks = []
        for b in r:
            toks.extend(b.w)
        for b in w:
            if not add:
                toks.extend(b.w)
            toks.extend(b.r.values())
        return toks

    def _need(self, eng, toks):
        kn = self.known[eng]
        need = {}
        for (s, v, src) in toks:
            if src == eng and (eng == "pe" or not SAME_ENG_SYNC):
                continue
            sid = self._sid(s)
            if kn.get(sid, 0) >= v:
                continue
            if sid not in need or need[sid][1] < v:
                need[sid] = (s, v)
        return list(need.values())

    def _emit_waits(self, eng, waits, ins_fn):
        E = self.engs[eng]
        for (s, v) in waits[:-1]:
            E.wait_ge(s, v)
            self.n_wait_ins += 1
        ins = ins_fn(E)
        if waits:
            s, v = waits[-1]
            ins._wait_ge(s, v)
        kn = self.known[eng]
        for (s, v) in waits:
            sid = self._sid(s)
            if kn.get(sid, 0) < v:
                kn[sid] = v
        return ins

    def op(self, eng, fn, r=(), w=()):
        if self.dead:
            return None
        r = [x.b if isinstance(x, Tl) else x for x in r]
        w = [x.b if isinstance(x, Tl) else x for x in w]
        waits = self._need(eng, self._deps(r, w))
        ins = self._emit_waits(eng, waits, fn)
        tok = self._next_tok(eng)
        ins.then_inc(tok[0], 1)
        for b in r:
            b.r[eng] = tok
        for b in w:
            b.w = [tok]
            b.r = {}
        return ins

    def dma(self, eng, out, in_, r=(), w=(), add=False, **kw):
        if self.dead:
            return None
        r = [x.b if isinstance(x, Tl) else x for x in r]
        w = [x.b if isinstance(x, Tl) else x for x in w]
        toks = self._deps(r, w, add)
        i = self.dma_i
        self.dma_i = (i + 1) % self.NDMA
        s = self.dma_sems[i]
        if self.dma_val[i] > 0:
            toks.append((s, self.dma_val[i], "dma"))
        waits = self._need(eng, toks)
        ins = self._emit_waits(eng, waits, lambda E: E.dma_start(out=out, in_=in_, **kw))
        self.dma_val[i] += 16
        tok = (s, self.dma_val[i], "dma")
        ins.then_inc(s, 16)
        for b in r:
            b.r[("dma", i)] = tok
        for b in w:
            if add:
                b.w.append(tok)
            else:
                b.w = [tok]
                b.r = {}
        return ins

    def barrier(self, engines=None):
        toks = []
        for e in self.engs:
            t = self._last_tok(e)
            if t is not None:
                toks.append(t)
        for i in range(self.NDMA):
            if self.dma_val[i] > 0:
                toks.append((self.dma_sems[i], self.dma_val[i], "dma"))
        for e in (engines or list(self.engs)):
            kn = self.known[e]
            E = self.engs[e]
            for (s, v, src) in toks:
                if src == e:
                    continue
                sid = self._sid(s)
                if kn.get(sid, 0) >= v:
                    continue
                E.wait_ge(s, v)
                self.n_wait_ins += 1
                kn[sid] = v


class Pool:
    def __init__(self, tiles):
        self.tiles = tiles
        self.i = 0

    def next(self):
        t = self.tiles[self.i]
        self.i = (self.i + 1) % len(self.tiles)
        return t


class _Stop(Exception):
    pass


def build_program(NS=4, DEPTH=4, debug=False, stop=None):
    _sref = []

    _cur = [0]

    def chk(tag):
        if stop == tag or stop == "%s@%d" % (tag, _cur[0]):
            _sref[0].dead = True
    nc = bass.Bass("TRN2", target_bir_lowering=False)
    dt = lambda name, shape, dtype, kind="Internal": nc.dram_tensor(name, list(shape), dtype, kind=kind).ap()
    x_in = dt("x", [NS, T, D], F32, "ExternalInput")
    ctx_in = dt("ctx", [NS, C, D], F32, "ExternalInput")
    cT_in = dt("cT", [128, 8, 5], F32, "ExternalInput")
    ada_w = dt("ada_w", [DEPTH, D, 6 * D], F32, "ExternalInput")
    ada_b = dt("ada_b", [DEPTH, 6 * D], F32, "ExternalInput")
    n1rep = dt("n1rep", [DEPTH, 128, 8, 5], F32, "ExternalInput")
    n2rep = dt("n2rep", [DEPTH, 128, 8, 5], F32, "ExternalInput")
    w_in = dt("w_in", [DEPTH, D, IN_W], F32, "ExternalInput")
    convp = dt("convp", [DEPTH, 128, 2, 40], F32, "ExternalInput")
    sink_in = dt("sink", [1, DEPTH * 4], F32, "ExternalInput")
    qng = dt("qng", [DEPTH, 64], F32, "ExternalInput")
    kng = dt("kng", [DEPTH, 64], F32, "ExternalInput")
    w_out = dt("w_out", [DEPTH, D, D], F32, "ExternalInput")
    router = dt("router", [DEPTH, D, NE], F32, "ExternalInput")
    w_gate = dt("w_gate", [DEPTH, NE, D, D], F32, "ExternalInput")
    w_up = dt("w_up", [DEPTH, NE, D, D], F32, "ExternalInput")
    w_down = dt("w_down", [DEPTH, NE, D, D], F32, "ExternalInput")
    final_g = dt("final_g", [1, D], F32, "ExternalInput")
    c_identb = dt("c_identb", [128, 128], BF16, "ExternalInput")
    c_identf = dt("c_identf", [128, 128], F32, "ExternalInput")
    c_masks = dt("c_masks", [128, 2, 128], BF16, "ExternalInput")
    c_iota = dt("c_iota", [128, 258], F32, "ExternalInput")
    c_rope = dt("c_rope", [NTL, 128, 2, 384], F32, "ExternalInput")
    c_sel = dt("c_sel", [16, 16 * 128], F32, "ExternalInput")
    out = dt("out", [NS, T, D], F32, "ExternalOutput")
    dbg = {}
    if debug:
        dbg["xmid"] = dt("d_xmid", [NTT, 128, D], F32, "ExternalOutput")
        dbg["aff"] = dt("d_aff", [128, NTT, 64], F32, "ExternalOutput")
        dbg["yt"] = dt("d_yt", [8, 128, NTOK], BF16, "ExternalOutput")
        dbg["xres"] = dt("d_xres", [NTT, 128, D], F32, "ExternalOutput")
        dbg["slotg"] = dt("d_slotg", [64, 2, NTOK], F32, "ExternalOutput")
        dbg["qT"] = dt("d_qT", [128, 2, NTOK], BF16, "ExternalOutput")
        dbg["kT"] = dt("d_kT", [128, 1, NTOK], BF16, "ExternalOutput")
    MODS = dt("MODS", [DEPTH, 5, 6 * D], F32)
    XMID = dt("XMID", [NS, NTT, 128, D], F32)
    XRES = dt("XRES", [NS, NTT, 128, D], F32)
    XN2 = dt("XN2", [NS, NTT, 128, D], BF16)
    YT = dt("YT", [NS, 8, 128, NTOK], BF16)
    NSL = NS * CAP + NS * CAPC
    XSG = dt("XSG", [NE, 8, 128, NSL], BF16)
    YEX = dt("YEX", [NE, NSL, D], BF16)
    SLOTG = dt("SLOTG", [64, 2, NTOK], F32)

    with ExitStack() as es:
        S = Sync(nc, es)
        _sref.append(S)

        _uid = [0]

        def sb(name, shape, dtype, scope=es):
            _uid[0] += 1
            name = "%s_u%d" % (name, _uid[0])
            return Tl(scope.enter_context(nc.sbuf_tensor(name, list(shape), dtype)), name)

        def dma_k(eng, tile, src2d, ncols=None):
            for k in range(8):
                dst = tile[:, k, :] if ncols is None else tile[:, k, 0:ncols]
                S.dma(eng, dst, src2d[k * 128:(k + 1) * 128, :], w=[tile], add=(k > 0))

        psb = [Tl(es.enter_context(nc.psum_tensor("ps%d" % i, [128, 512], F32)), "ps%d" % i) for i in range(8)]
        PS_MM = Pool(psb[0:4])
        PS_TR = Pool(psb[4:6])
        PS_AC = Pool(psb[6:8])

        def bfv(ps):
            return ps.t.bitcast(BF16)

        identb = sb("identb", [128, 128], BF16)
        identf = sb("identf", [128, 128], F32)
        masks = sb("masks", [128, 2, 128], BF16)
        iota = sb("iota", [128, 258], F32)
        onesf = sb("onesf", [128, 128], F32)
        onesb = sb("onesb", [128, 128], BF16)
        modsT = sb("modsT", [128, DEPTH, 48, 5], F32)
        affTok = sb("affTok", [128, NTT, 64], F32)
        esink = sb("esink", [128, DEPTH * 4], F32)
        S.dma("sp", identb[:], c_identb[:, :], w=[identb])
        S.dma("sp", identf[:], c_identf[:, :], w=[identf])
        S.dma("sp", masks[:], c_masks[:, :, :], w=[masks])
        S.dma("sp", iota[:], c_iota[:, :], w=[iota])
        S.dma("sp", esink[:], sink_in[0:1, :].to_broadcast([128, DEPTH * 4]), w=[esink])
        S.op("dve", lambda e: e.memset(onesf[:], 1.0), w=[onesf])
        S.op("dve", lambda e: e.memset(onesb[:], 1.0), w=[onesb])
        S.op("dve", lambda e: e.memset(affTok[:], 0.0), w=[affTok])
        S.op("act", lambda e: e.activation(out=esink[:], in_=esink[:], func=AF.Exp), r=[esink], w=[esink])

        with ExitStack() as ps0:
            scT = sb("scT", [128, 8, 5], F32, ps0)
            adw = [sb("adw%d" % i, [128, 8, 512], F32, ps0) for i in range(2)]
            adb = [sb("adb%d" % i, [5, 512], F32, ps0) for i in range(2)]
            mrow = [sb("mrow%d" % i, [5, 512], F32, ps0) for i in range(2)]
            S.dma("sp", scT[:], cT_in[:, :, :], w=[scT])
            S.op("act", lambda e: e.activation(out=scT[:], in_=scT[:], func=AF.Silu), r=[scT], w=[scT])
            it = 0
            for l in range(DEPTH):
                for n in range(12):
                    wt, bt, mr = adw[it % 2], adb[it % 2], mrow[it % 2]
                    it += 1
                    dma_k("sp", wt, ada_w[l][:, n * 512:(n + 1) * 512])
                    S.dma("sp", bt[:], ada_b[l:l + 1, n * 512:(n + 1) * 512].to_broadcast([5, 512]), w=[bt])
                    ps = PS_MM.next()
                    for k in range(8):
                        S.op("pe", lambda e, k=k: e.matmul(ps[0:5, :], lhsT=scT[:, k, :], rhs=wt[:, k, :],
                                                          start=(k == 0), stop=(k == 7)), r=[scT, wt], w=[ps])
                    S.op("dve", lambda e: e.tensor_tensor(out=mr[:], in0=ps[0:5, :], in1=bt[:], op=ALU.add),
                         r=[ps, bt], w=[mr])
                    S.dma("sp", MODS[l][:, n * 512:(n + 1) * 512], mr[:], r=[mr])
                    pt = PS_TR.next()
                    for j in range(4):
                        S.op("pe", lambda e, j=j: e.transpose(pt[:, j * 5:(j + 1) * 5], mr[:, j * 128:(j + 1) * 128],
                                                              identf[0:5, 0:5]), r=[mr, identf], w=[pt])
                    S.op("act", lambda e: e.activation(
                        out=modsT[:, l, n * 4:(n + 1) * 4, :],
                        in_=pt[:, 0:20].rearrange("p (j s) -> p j s", s=5), func=AF.Copy), r=[pt], w=[modsT])
        S.barrier()

        try:
            chk("pro")
            def x_src(l, b, i):
                if l == 0:
                    return x_in[b, i * 128:(i + 1) * 128, :] if i < NTL else ctx_in[b, (i - NTL) * 128:(i - NTL + 1) * 128, :]
                return XRES[b, i]

            A1 = sb("A1", [128, 8, 5], F32)
            A2 = sb("A2", [128, 8, 5], F32)
            for l in range(DEPTH):
                _cur[0] = l
                with ExitStack() as p1:
                    YAG = Pool([sb("yag%d" % i, [128, 256], BF16, p1) for i in range(4)])
                    H2T = Pool([sb("h2t%d" % i, [128, 8, 128], BF16, p1) for i in range(2)])
                    YTB = [Buf("ytb%d" % i) for i in range(NS)]
                    nrep = sb("nrep", [128, 2, 8, 5], F32, p1)
                    cvp = sb("cvp", [128, 2, 40], F32, p1)
                    qg = sb("qg", [128, 64], F32, p1)
                    kg = sb("kg", [128, 64], F32, p1)
                    woutb = sb("woutb", [128, 8, D], BF16, p1)
                    rtb = sb("rtb", [128, 8, NE], BF16, p1)
                    hT = sb("hT", [128, 8, NTOK], BF16, p1)
                    xt = [sb("xt%d" % i, [128, D], F32, p1) for i in range(2)]
                    xnb = [sb("xnb%d" % i, [128, D], BF16, p1) for i in range(2)]
                    st = [sb("st%d" % i, [128, 8], F32, p1) for i in range(4)]
                    XT, XNB, ST = Pool(xt), Pool(xnb), Pool(st)
                    wch = [sb("wch%d" % i, [128, 8, 512], BF16, p1) for i in range(2)]
                    WCH = Pool(wch)
                    cw = [sb("cw%d" % i, [128, NTOK + 32], F32, p1) for i in range(3)]
                    upc = sb("upc", [128, C + 32], F32, p1)
                    ystg = [sb("ystg%d" % i, [128, NTOK], BF16, p1) for i in range(2)]
                    YSTG = Pool(ystg)
                    qT = sb("qT", [128, 2, NTOK], BF16, p1)
                    kT = sb("kT", [128, 1, NTOK], BF16, p1)
                    Vt = sb("Vt", [128, NTT, 2, 65], BF16, p1)
                    rope = [sb("rope%d" % i, [128, 2, 384], F32, p1) for i in range(2)]
                    ROPE = Pool(rope)
                    qk = [sb("qk%d" % i, [128, 384], F32, p1) for i in range(2)]
                    QK = Pool(qk)
                    qk2 = [sb("qkb%d" % i, [128, 384], F32, p1) for i in range(2)]
                    QK2 = Pool(qk2)
                    qkr = [sb("qkr%d" % i, [128, 384], BF16, p1) for i in range(2)]
                    QKR = Pool(qkr)
                    ptl = [sb("ptl%d" % i, [128, 512], BF16, p1) for i in range(5)]
                    PTL = Pool(ptl)
                    yat = [sb("yat%d" % i, [128, 256], BF16, p1) for i in range(2)]
                    YAT = Pool(yat)
                    g1b = sb("g1b", [128, D], F32, p1)
                    YTL = WCH
                    tmpf = [sb("tmpf%d" % i, [128, 512], F32, p1) for i in range(6)]
                    TMPF = Pool(tmpf)

                    S.dma("sp", nrep[:, 0], n1rep[l], w=[nrep])
                    S.dma("sp", nrep[:, 1], n2rep[l], w=[nrep])
                    S.dma("sp", cvp[:], convp[l], w=[cvp])
                    S.dma("sp", qg[:], qng[l:l + 1, :].to_broadcast([128, 64]), w=[qg])
                    S.dma("sp", kg[:], kng[l:l + 1, :].to_broadcast([128, 64]), w=[kg])
                    dma_k("pool", woutb, w_out[l])
                    S.dma("pool", rtb[:], router[l].rearrange("(k p) f -> p k f", p=128), w=[rtb])
                    S.op("dve", lambda e: e.scalar_tensor_tensor(out=A1[:], in0=modsT[:, l, 8:16, :], scalar=1.0,
                                                                 in1=nrep[:, 0], op0=ALU.add, op1=ALU.mult),
                         r=[modsT, nrep], w=[A1])
                    S.op("dve", lambda e: e.scalar_tensor_tensor(out=A2[:], in0=modsT[:, l, 32:40, :], scalar=1.0,
                                                                 in1=nrep[:, 1], op0=ALU.add, op1=ALU.mult),
                         r=[modsT, nrep], w=[A2])
                    S.op("dve", lambda e: e.memset(Vt[:], 1.0), w=[Vt])

                    def norm_to_T(src_tile, dstT, dcols, A, Bofs, j, xn_keep=None):
                        ss = ST.next()
                        xn = xn_keep if xn_keep is not None else XNB.next()
                        S.op("pool", lambda e: e.memset(ss[:], 0.0), w=[ss])
                        S.op("act", lambda e: e.activation(out=xn[:], in_=src_tile[:], func=AF.Square,
                                                           accum_out=ss[:, 0:1]), r=[src_tile], w=[xn, ss])
                        S.op("dve", lambda e: e.tensor_scalar(out=ss[:, 1:2], in0=ss[:, 0:1], scalar1=1.0 / D, scalar2=EPS,
                                                              op0=ALU.mult, op1=ALU.add), r=[ss], w=[ss])
                        S.op("dve", lambda e: e.reciprocal(out=ss[:, 2:3], in_=ss[:, 1:2]), r=[ss], w=[ss])
                        S.op("act", lambda e: e.activation(out=ss[:, 3:4], in_=ss[:, 2:3], func=AF.Sqrt), r=[ss], w=[ss])
                        S.op("act", lambda e: e.activation(out=xn[:], in_=src_tile[:], func=AF.Copy, scale=ss[:, 3:4]),
                             r=[src_tile, ss], w=[xn])
                        pt = PS_TR.next()
                        ptb = bfv(pt)
                        for c in range(8):
                            S.op("pe", lambda e, c=c: e.transpose(ptb[:, c * 128:(c + 1) * 128], xn[:, c * 128:(c + 1) * 128],
                                                                  identb[:]), r=[xn, identb], w=[pt])
                        for c in range(8):
                            if c % 2 == 0:
                                S.op("act", lambda e, c=c: e.activation(
                                    out=dstT[:, c, dcols], in_=ptb[:, c * 128:(c + 1) * 128], func=AF.Identity,
                                    scale=A[:, c, j:j + 1], bias=modsT[:, l, Bofs + c, j:j + 1]), r=[pt, A, modsT], w=[dstT])
                            else:
                                S.op("dve", lambda e, c=c: e.tensor_scalar(
                                    out=dstT[:, c, dcols], in0=ptb[:, c * 128:(c + 1) * 128],
                                    scalar1=A[:, c, j:j + 1], scalar2=modsT[:, l, Bofs + c, j:j + 1],
                                    op0=ALU.mult, op1=ALU.add), r=[pt, A, modsT], w=[dstT])
                        return xn

                    TCH = [(0, 512), (512, 512), (1024, 512), (1536, 512), (2048, 256)]
                    SEGS = [(0, T), (T, C)]

                    def colp(t):
                        return 16 + t if t < T else 16 + t + 0

                    for b in range(NS):
                        for i in range(NTT):
                            xtile = XT.next()
                            S.dma("sp", xtile[:], x_src(l, b, i), w=[xtile])
                            norm_to_T(xtile, hT, slice(i * 128, (i + 1) * 128), A1, 0, b if i < NTL else 4)

                        chk("p1_norm")
                        def inproj_fm(fc, consume):
                            wt = WCH.next()
                            dma_k("pool", wt, w_in[l][:, fc * 128:(fc + 1) * 128], 128)
                            for (t0, n) in TCH:
                                ps = PS_MM.next()
                                for k in range(8):
                                    S.op("pe", lambda e, k=k: e.matmul(ps[:, 0:n], lhsT=wt[:, k, 0:128], rhs=hT[:, k, t0:t0 + n],
                                                                      start=(k == 0), stop=(k == 7)), r=[wt, hT], w=[ps])
                                consume(ps, t0, n)

                        for h in range(2):
                            prod, cv = cw[0], cw[1]
                            S.op("pool", lambda e: e.memset(prod[:, 0:1], 0.0), w=[prod])
                            S.op("pool", lambda e: e.memset(prod[:, T + 1:T + 3], 0.0), w=[prod])
                            S.op("pool", lambda e: e.memset(prod[:, T + 3 + C:T + 4 + C], 0.0), w=[prod])

                            def offA(t0):
                                return 1 + t0 if t0 < T else T + 3 + (t0 - T)
                            inproj_fm(2 + h, lambda ps, t0, n: S.op(
                                "act", lambda e: e.activation(out=prod[:, offA(t0):offA(t0) + n], in_=ps[:, 0:n], func=AF.Copy),
                                r=[ps], w=[prod]))
                            inproj_fm(4 + h, lambda ps, t0, n: S.op(
                                "dve", lambda e: e.tensor_tensor(out=prod[:, offA(t0):offA(t0) + n], in0=ps[:, 0:n],
                                                                 in1=prod[:, offA(t0):offA(t0) + n], op=ALU.mult), r=[ps, prod], w=[prod]))
                            for (s0, sn) in SEGS:
                                o0 = offA(s0)
                                S.op("dve", lambda e: e.tensor_scalar(out=cv[:, s0:s0 + sn], in0=prod[:, o0 - 1:o0 - 1 + sn],
                                                                      scalar1=cvp[:, h, 0:1], scalar2=None, op0=ALU.mult),
                                     r=[prod, cvp], w=[cv])
                                for kk in (1, 2):
                                    S.op("dve", lambda e, kk=kk: e.scalar_tensor_tensor(
                                        out=cv[:, s0:s0 + sn], in0=prod[:, o0 - 1 + kk:o0 - 1 + kk + sn], scalar=cvp[:, h, kk:kk + 1],
                                        in1=cv[:, s0:s0 + sn], op0=ALU.mult, op1=ALU.add), r=[prod, cvp, cv], w=[cv])
                            ys = YSTG.next()
                            inproj_fm(0 + h, lambda ps, t0, n: S.op(
                                "dve", lambda e: e.tensor_tensor(out=ys[:, t0:t0 + n], in0=ps[:, 0:n], in1=cv[:, t0:t0 + n],
                                                                 op=ALU.mult), r=[ps, cv], w=[ys]))
                            S.dma("sp", YT[b, h], ys[:], r=[ys], w=[YTB[b]], add=True)
                        chk("p1_A")
                        ucs = [cw[1], cw[2]]
                        for h in range(2):
                            upad, uc = cw[0], ucs[h]
                            S.op("pool", lambda e: e.memset(upad[:, 0:15], 0.0), w=[upad])
                            S.op("pool", lambda e: e.memset(upad[:, 15 + T:30 + T], 0.0), w=[upad])
                            S.op("pool", lambda e: e.memset(upc[:, 0:15], 0.0), w=[upc])
                            S.op("pool", lambda e: e.memset(upc[:, 15 + C:30 + C], 0.0), w=[upc])

                            def dstB(t0, n):
                                return (upad, upad[:, 15 + t0:15 + t0 + n]) if t0 < T else (upc, upc[:, 15:15 + n])

                            def consB1(ps, t0, n):
                                tl, ap = dstB(t0, n)
                                S.op("act", lambda e: e.activation(out=ap, in_=ps[:, 0:n], func=AF.Sigmoid), r=[ps], w=[tl])

                            def consB2(ps, t0, n):
                                tl, ap = dstB(t0, n)
                                S.op("dve", lambda e: e.tensor_tensor(out=ap, in0=ps[:, 0:n], in1=ap, op=ALU.mult), r=[ps, tl], w=[tl])
                            inproj_fm(8 + h, consB1)
                            inproj_fm(6 + h, consB2)
                            for (src, s0, sn, eng) in ((upad, 0, T, "dve"), (upc, T, C, "dve")):
                                S.op(eng, lambda e: e.tensor_scalar(out=uc[:, s0:s0 + sn], in0=src[:, 0:sn],
                                                                    scalar1=cvp[:, h, 3:4], scalar2=cvp[:, h, 34:35],
                                                                    op0=ALU.mult, op1=ALU.add), r=[src, cvp], w=[uc])
                                for kk in range(1, 31):
                                    S.op(eng, lambda e, kk=kk: e.scalar_tensor_tensor(
                                        out=uc[:, s0:s0 + sn], in0=src[:, kk:kk + sn], scalar=cvp[:, h, 3 + kk:4 + kk],
                                        in1=uc[:, s0:s0 + sn], op0=ALU.mult, op1=ALU.add), r=[src, cvp, uc], w=[uc])
                        ysb = [YSTG.next(), YSTG.next()]
                        for (t0, n) in TCH:
                            p1s, p2s = PS_MM.next(), PS_MM.next()
                            for h in range(2):
                                S.op("pe", lambda e, h=h: e.matmul(p1s[:, 0:n], lhsT=onesf[:], rhs=ucs[h][:, t0:t0 + n],
                                                                  start=(h == 0), stop=(h == 1)), r=[onesf, ucs[h]], w=[p1s])
                            for h in range(2):
                                sqt = TMPF.next()
                                S.op("act", lambda e, h=h: e.activation(out=sqt[:, 0:n], in_=ucs[h][:, t0:t0 + n], func=AF.Square),
                                     r=[ucs[h]], w=[sqt])
                                S.op("pe", lambda e, h=h: e.matmul(p2s[:, 0:n], lhsT=onesf[:], rhs=sqt[:, 0:n],
                                                                  start=(h == 0), stop=(h == 1)), r=[onesf, sqt], w=[p2s])
                            mean, var, dd = TMPF.next(), TMPF.next(), TMPF.next()
                            S.op("act", lambda e: e.activation(out=mean[:, 0:n], in_=p1s[:, 0:n], func=AF.Copy, scale=1.0 / 256),
                                 r=[p1s], w=[mean])
                            S.op("dve", lambda e: e.tensor_tensor(out=var[:, 0:n], in0=mean[:, 0:n], in1=mean[:, 0:n], op=ALU.mult),
                                 r=[mean], w=[var])
                            S.op("dve", lambda e: e.scalar_tensor_tensor(out=var[:, 0:n], in0=p2s[:, 0:n], scalar=1.0 / 256,
                                                                         in1=var[:, 0:n], op0=ALU.mult, op1=ALU.subtract),
                                 r=[p2s, var], w=[var])
                            S.op("dve", lambda e: e.tensor_scalar(out=var[:, 0:n], in0=var[:, 0:n], scalar1=EPS, scalar2=None,
                                                                  op0=ALU.add), r=[var], w=[var])
                            S.op("dve", lambda e: e.reciprocal(out=var[:, 0:n], in_=var[:, 0:n]), r=[var], w=[var])
                            S.op("act", lambda e: e.activation(out=var[:, 0:n], in_=var[:, 0:n], func=AF.Sqrt), r=[var], w=[var])
                            for h in range(2):
                                S.op("dve", lambda e, h=h: e.tensor_tensor(out=dd[:, 0:n], in0=ucs[h][:, t0:t0 + n], in1=mean[:, 0:n],
                                                                            op=ALU.subtract), r=[ucs[h], mean], w=[dd])
                                S.op("dve", lambda e: e.tensor_tensor(out=dd[:, 0:n], in0=dd[:, 0:n], in1=var[:, 0:n], op=ALU.mult),
                                     r=[dd, var], w=[dd])
                                S.op("act", lambda e, h=h: e.activation(out=ysb[h][:, t0:t0 + n], in_=dd[:, 0:n], func=AF.Silu,
                                                                        scale=cvp[:, h, 35:36], bias=cvp[:, h, 36:37]),
                                     r=[dd, cvp], w=[ysb[h]])
                        for h in range(2):
                            S.dma("sp", YT[b, 2 + h], ysb[h][:], r=[ysb[h]], w=[YTB[b]], add=True)

                        chk("p1_B")
                        for grp in range(2):
                            wt = WCH.next()
                            c0 = 1280 + grp * 512
                            dma_k("pool", wt, w_in[l][:, c0:c0 + 512])
                            for i in range(NTT):
                                ps = PS_MM.next()
                                for k in range(8):
                                    S.op("pe", lambda e, k=k: e.matmul(ps[:, :], lhsT=hT[:, k, i * 128:(i + 1) * 128], rhs=wt[:, k, :],
                                                                      start=(k == 0), stop=(k == 7)), r=[hT, wt], w=[ps])
                                S.op("act", lambda e: e.activation(out=Vt[:, i, :, 0:64],
                                                                   in_=ps[:, 384:512].rearrange("p (a d) -> p a d", d=64),
                                                                   func=AF.Copy), r=[ps], w=[Vt])
                                q1 = QK.next()
                                S.op("act", lambda e: e.activation(out=q1[:], in_=ps[:, 0:384], func=AF.Copy), r=[ps], w=[q1])
                                if grp == 1:
                                    sq = QK2.next()
                                    ss = ST.next()
                                    S.op("dve", lambda e: e.tensor_tensor(out=sq[:], in0=q1[:], in1=q1[:], op=ALU.mult), r=[q1], w=[sq])
                                    S.op("dve", lambda e: e.tensor_reduce(out=ss[:, 0:6], in_=sq[:].rearrange("p (a d) -> p a d", d=64),
                                                                          axis=AX.X, op=ALU.add), r=[sq], w=[ss])
                                    S.op("dve", lambda e: e.tensor_scalar(out=ss[:, 0:6], in0=ss[:, 0:6], scalar1=1.0 / 64, scalar2=EPS,
                                                                          op0=ALU.mult, op1=ALU.add), r=[ss], w=[ss])
                                    S.op("dve", lambda e: e.reciprocal(out=ss[:, 0:6], in_=ss[:, 0:6]), r=[ss], w=[ss])
                                    S.op("act", lambda e: e.activation(out=ss[:, 0:6], in_=ss[:, 0:6], func=AF.Sqrt), r=[ss], w=[ss])
                                    for hh in range(6):
                                        gt = qg if hh < 4 else kg
                                        S.op("dve", lambda e, hh=hh, gt=gt: e.scalar_tensor_tensor(
                                            out=q1[:, hh * 64:(hh + 1) * 64], in0=q1[:, hh * 64:(hh + 1) * 64], scalar=ss[:, hh:hh + 1],
                                            in1=gt[:], op0=ALU.mult, op1=ALU.mult), r=[q1, ss, gt], w=[q1])
                                qr = QKR.next()
                                if i < NTL:
                                    rp = ROPE.next()
                                    S.dma("sp", rp[:], c_rope[i], w=[rp])
                                    t1, t2 = QK2.next(), QK2.next()
                                    v4 = lambda ap: ap.rearrange("p (a j q) -> p a j q", j=2, q=16)
                                    S.op("dve", lambda e: e.tensor_tensor(out=t1[:], in0=q1[:], in1=rp[:, 0, :], op=ALU.mult),
                                         r=[q1, rp], w=[t1])
                                    S.op("dve", lambda e: e.tensor_tensor(out=v4(t2[:])[:, :, 0, :], in0=v4(q1[:])[:, :, 1, :],
                                                                           in1=v4(rp[:, 1, :])[:, :, 0, :], op=ALU.mult), r=[q1, rp], w=[t2])
                                    S.op("dve", lambda e: e.tensor_tensor(out=v4(t2[:])[:, :, 1, :], in0=v4(q1[:])[:, :, 0, :],
                                                                           in1=v4(rp[:, 1, :])[:, :, 1, :], op=ALU.mult), r=[q1, rp], w=[t2])
                                    pq_o = lambda ap: ap[:, 0:256].rearrange("p (g k d) -> p k g d", g=2, k=2)
                                    pq_i = lambda ap: ap[:, 0:256].rearrange("p (k g d) -> p k g d", g=2, k=2)
                                    S.op("dve", lambda e: e.tensor_tensor(out=pq_o(qr[:]), in0=pq_i(t1[:]), in1=pq_i(t2[:]), op=ALU.add),
                                         r=[t1, t2], w=[qr])
                                    S.op("dve", lambda e: e.tensor_tensor(out=qr[:, 256:384], in0=t1[:, 256:384], in1=t2[:, 256:384],
                                                                          op=ALU.add), r=[t1, t2], w=[qr])
                                else:
                                    pq_o = lambda ap: ap[:, 0:256].rearrange("p (g k d) -> p k g d", g=2, k=2)
                                    pq_i = lambda ap: ap[:, 0:256].rearrange("p (k g d) -> p k g d", g=2, k=2)
                                    S.op("dve", lambda e: e.tensor_copy(out=pq_o(qr[:]), in_=pq_i(q1[:])), r=[q1], w=[qr])
                                    S.op("dve", lambda e: e.tensor_copy(out=qr[:, 256:384], in_=q1[:, 256:384]), r=[q1], w=[qr])
                                pt = PS_TR.next()
                                ptb = bfv(pt)
                                for hh in range(3):
                                    S.op("pe", lambda e, hh=hh: e.transpose(ptb[:, hh * 128:(hh + 1) * 128], qr[:, hh * 128:(hh + 1) * 128],
                                                                            identb[:]), r=[qr, identb], w=[pt])
                                S.op("act", lambda e: e.activation(out=qT[:, :, i * 128:(i + 1) * 128],
                                                                   in_=ptb[:, 0:256].rearrange("p (a t) -> p a t", t=128),
                                                                   func=AF.Copy), r=[pt], w=[qT])
                                S.op("dve", lambda e: e.tensor_copy(out=kT[:, 0, i * 128:(i + 1) * 128], in_=ptb[:, 256:384]),
                                     r=[pt], w=[kT])
                            if debug and grp == 1 and b == 0 and l == 0:
                                S.dma("sp", dbg["qT"][:, :, :], qT[:], r=[qT])
                                S.dma("sp", dbg["kT"][:, :, :], kT[:], r=[kT])
                            if grp == 0:
                                chk("p1_C0")
                            else:
                                chk("p1_D0")
                            yts = [YSTG.next(), YSTG.next()]

                            def normalize_head(pa, co, h, ya):
                                den = ST.next()
                                if grp == 0:
                                    S.op("act", lambda e: e.activation(out=den[:, 0:1], in_=pa[:, co + 64:co + 65], func=AF.Identity,
                                                                       bias=esink[:, l * 4 + h:l * 4 + h + 1]),
                                         r=[pa, esink], w=[den])
                                    S.op("dve", lambda e: e.reciprocal(out=den[:, 1:2], in_=den[:, 0:1]), r=[den], w=[den])
                                else:
                                    S.op("act", lambda e: e.activation(out=den[:, 0:1], in_=pa[:, co + 64:co + 65], func=AF.Copy),
                                         r=[pa], w=[den])
                                    S.op("dve", lambda e: e.reciprocal(out=den[:, 1:2], in_=den[:, 0:1]), r=[den], w=[den])
                                S.op("act", lambda e: e.activation(out=ya[:, h * 64:(h + 1) * 64], in_=pa[:, co:co + 64],
                                                                   func=AF.Copy, scale=den[:, 1:2]), r=[pa, den], w=[ya])

                            def finish_ya(ya, qb):
                                pt = PS_TR.next()
                                ptb = bfv(pt)
                                for c in range(2):
                                    S.op("pe", lambda e, c=c: e.transpose(ptb[:, c * 128:(c + 1) * 128], ya[:, c * 128:(c + 1) * 128],
                                                                          identb[:]), r=[ya, identb], w=[pt])
                                for c in range(2):
                                    S.op("dve", lambda e, c=c: e.tensor_copy(out=yts[c][:, qb * 128:(qb + 1) * 128],
                                                                             in_=ptb[:, c * 128:(c + 1) * 128]), r=[pt], w=[yts[c]])

                            items = []
                            for qb in range(NTT):
                                if qb >= NTL:
                                    kts = [NTL, NTL + 1]
                                elif grp == 0:
                                    kts = [m for m in (qb - 1, qb, qb + 1) if 0 <= m < NTL] + [NTL, NTL + 1]
                                else:
                                    kts = list(range(NTT))
                                nk = len(kts)
                                for h in range(4):
                                    for g0 in range(0, nk, 4):
                                        items.append((qb, h, g0, kts[g0:g0 + 4], nk))

                            def emit_st(item):
                                qb, h, g0, grpk, nk = item
                                kh = h // 2
                                ps = PS_MM.next()
                                for ki, m in enumerate(grpk):
                                    S.op("pe", lambda e, m=m, ki=ki: e.matmul(
                                        ps[:, ki * 128:(ki + 1) * 128], lhsT=kT[kh * 64:(kh + 1) * 64, 0, m * 128:(m + 1) * 128],
                                        rhs=qT[kh * 64:(kh + 1) * 64, h % 2, qb * 128:(qb + 1) * 128], start=True, stop=True),
                                        r=[kT, qT], w=[ps])
                                pt_ = PTL.next()
                                nn_ = len(grpk) * 128
                                S.op("act", lambda e: e.activation(out=pt_[:, 0:nn_], in_=ps[:, 0:nn_], func=AF.Exp, scale=0.125),
                                     r=[ps], w=[pt_])
                                if grp == 0 and qb < NTL:
                                    for ki, m in enumerate(grpk):
                                        if (m == qb - 1 or m == qb + 1) and m < NTL:
                                            mi = 1 if m == qb - 1 else 0
                                            S.op("dve", lambda e, ki=ki, mi=mi: e.tensor_tensor(
                                                out=pt_[:, ki * 128:(ki + 1) * 128], in0=pt_[:, ki * 128:(ki + 1) * 128],
                                                in1=masks[:, mi, :], op=ALU.mult), r=[pt_, masks], w=[pt_])
                                return pt_

                            cur = {}

                            def emit_pv(item, pt_):
                                qb, h, g0, grpk, nk = item
                                kh = h // 2
                                if h == 0 and g0 == 0:
                                    cur["pacc"] = PS_AC.next()
                                    cur["ya"] = YAT.next()
                                pacc = cur["pacc"]
                                for ki, m in enumerate(grpk):
                                    S.op("pe", lambda e, ki=ki, m=m: e.matmul(
                                        pacc[:, h * 65:(h + 1) * 65], lhsT=pt_[:, ki * 128:(ki + 1) * 128], rhs=Vt[:, m, kh, :],
                                        start=(g0 + ki == 0), stop=(g0 + ki == nk - 1)), r=[pt_, Vt], w=[pacc])
                                if h == 3 and g0 + len(grpk) == nk:
                                    for hh in range(4):
                                        normalize_head(pacc, hh * 65, hh, cur["ya"])
                                    finish_ya(cur["ya"], qb)

                            LOOK = 2
                            pend = [emit_st(items[k_]) for k_ in range(min(LOOK, len(items)))]
                            for k_ in range(len(items)):
                                if k_ + LOOK < len(items):
                                    pend.append(emit_st(items[k_ + LOOK]))
                                emit_pv(items[k_], pend.pop(0))
                            if grp == 0:
                                chk("p1_C")
                            for c in range(2):
                                S.dma("sp", YT[b, 4 + grp * 2 + c], yts[c][:], r=[yts[c]], w=[YTB[b]], add=True)

                        chk("p1_CD")
                        S.dma("sp", g1b[:], MODS[l][b:b + 1, 2048:3072].to_broadcast([128, D]), w=[g1b])
                        for (t0, n) in TCH:
                            if t0 >= T:
                                S.dma("sp", g1b[:], MODS[l][4:5, 2048:3072].to_broadcast([128, D]), w=[g1b])
                            yl = YTL.next()
                            for c in range(8):
                                S.dma("sp", yl[:, c, 0:n], YT[b, c][:, t0:t0 + n], r=[YTB[b]], w=[yl], add=(c > 0))
                            for i in range(t0 // 128, (t0 + n) // 128):
                                tt = i * 128 - t0
                                j = b if i < NTL else 4
                                gt = g1b
                                xtile = XT.next()
                                S.dma("sp", xtile[:], x_src(l, b, i), w=[xtile])
                                for half in range(2):
                                    ps = PS_MM.next()
                                    for k in range(8):
                                        S.op("pe", lambda e, k=k: e.matmul(ps[:, :], lhsT=yl[:, k, tt:tt + 128],
                                                                          rhs=woutb[:, k, half * 512:(half + 1) * 512],
                                                                          start=(k == 0), stop=(k == 7)), r=[yl, woutb], w=[ps])
                                    tm = TMPF.next()
                                    S.op("dve", lambda e: e.tensor_tensor(out=tm[:], in0=ps[:, :], in1=gt[:, half * 512:(half + 1) * 512],
                                                                          op=ALU.mult), r=[ps, gt], w=[tm])
                                    S.op("dve", lambda e: e.tensor_tensor(out=xtile[:, half * 512:(half + 1) * 512],
                                                                           in0=xtile[:, half * 512:(half + 1) * 512], in1=tm[:],
                                                                           op=ALU.add), r=[xtile, tm], w=[xtile])
                                S.dma("sp", XMID[b, i], xtile[:], r=[xtile])
                                if debug and b == 0 and l == 0:
                                    S.dma("sp", dbg["xmid"][i], xtile[:], r=[xtile])
                                xn = XNB.next()
                                h2 = H2T.next()
                                norm_to_T(xtile, h2, slice(0, 128), A2, 24, j, xn_keep=xn)
                                S.dma("sp", XN2[b, i], xn[:], r=[xn])
                                ps = PS_MM.next()
                                for k in range(8):
                                    S.op("pe", lambda e, k=k: e.matmul(ps[:, 0:NE], lhsT=h2[:, k, :], rhs=rtb[:, k, :],
                                                                      start=(k == 0), stop=(k == 7)), r=[h2, rtb], w=[ps])
                                ss = ST.next()
                                ex = TMPF.next()
                                S.op("pool", lambda e: e.memset(ss[:], 0.0), w=[ss])
                                S.op("act", lambda e: e.activation(out=ex[:, 0:NE], in_=ps[:, 0:NE], func=AF.Exp,
                                                                   accum_out=ss[:, 0:1]), r=[ps], w=[ex, ss])
                                S.op("dve", lambda e: e.reciprocal(out=ss[:, 1:2], in_=ss[:, 0:1]), r=[ss], w=[ss])
                                S.op("dve", lambda e: e.tensor_scalar(out=affTok[:, i, b * 16:(b + 1) * 16], in0=ex[:, 0:NE],
                                                                      scalar1=ss[:, 1:2], scalar2=None, op0=ALU.mult),
                                     r=[ex, ss], w=[affTok])
                    if debug and l == 0:
                        S.dma("sp", dbg["aff"][:, :, :], affTok[:], r=[affTok])
                        S.barrier()
                        for c in range(8):
                            t_ = YSTG.next()
                            S.dma("sp", t_[:], YT[0, c], w=[t_])
                            S.dma("sp", dbg["yt"][c], t_[:], r=[t_])
                    S.barrier()

                chk("p1")
                with ExitStack() as m1:
                    affT = sb("affT", [64, NTOK], F32, m1)
                    work = sb("work", [64, NTOK], F32, m1)
                    Gx = sb("Gx", [64, NTOK], F32, m1)
                    gtok = sb("gtok", [128, NTT, 64], F32, m1)
                    maskb = sb("maskb", [128, NTT, 64], BF16, m1)
                    slotTok = sb("slotTok", [128, NTT, 64], F32, m1)
                    MX = Pool([sb("mx%d" % i, [64, 8], F32, m1) for i in range(2)])
                    xn2 = sb("xn2", [128, NTT, D], BF16, m1)
                    PB = Pool([sb("pb%d" % i, [128, 256], BF16, m1) for i in range(NTL)])
                    PBC = Pool([sb("pbc%d" % i, [128, 32], BF16, m1) for i in range(2)])
                    XSTG = Pool([sb("xstg%d" % i, [128, 8, 256], BF16, m1) for i in range(2)])
                    XSTC = Pool([sb("xstc%d" % i, [128, 8, 32], BF16, m1) for i in range(2)])

                    def tr_in(dst, src_fn, rows_out, nt, ident_n, rbufs):
                        pass

                    for i0 in range(0, NTT, 4):
                        pt = PS_TR.next()
                        nn = min(4, NTT - i0)
                        for q in range(nn):
                            S.op("pe", lambda e, q=q: e.transpose(pt[0:64, q * 128:(q + 1) * 128], affTok[:, i0 + q, :], identf[:]),
                                 r=[affTok, identf], w=[pt])
                        S.op("act", lambda e: e.activation(out=affT[:, i0 * 128:(i0 + nn) * 128], in_=pt[0:64, 0:nn * 128], func=AF.Copy),
                             r=[pt], w=[affT])
                    S.op("dve", lambda e: e.tensor_copy(out=work[:], in_=affT[:]), r=[affT], w=[work])
                    for (s0, sn, rounds) in ((0, T, CAP // 8), (T, C, CAPC // 8)):
                        for r_ in range(rounds):
                            m8 = MX.next()
                            S.op("dve", lambda e: e.max(out=m8[:], in_=work[:, s0:s0 + sn]), r=[work], w=[m8])
                            S.op("dve", lambda e: e.match_replace(out=work[:, s0:s0 + sn], in_to_replace=m8[:],
                                                                  in_values=work[:, s0:s0 + sn], imm_value=0.0), r=[work, m8], w=[work])
                    S.op("dve", lambda e: e.tensor_tensor(out=Gx[:], in0=affT[:], in1=work[:], op=ALU.subtract), r=[affT, work], w=[Gx])
                    S.dma("sp", SLOTG[:, 1, :], Gx[:], r=[Gx])
                    for i0 in range(0, NTT, 4):
                        pt = PS_TR.next()
                        nn = min(4, NTT - i0)
                        for q in range(nn):
                            S.op("pe", lambda e, q=q: e.transpose(pt[:, q * 64:(q + 1) * 64], Gx[:, (i0 + q) * 128:(i0 + q + 1) * 128],
                                                                  identf[0:64, 0:64]), r=[Gx, identf], w=[pt])
                        S.op("act", lambda e: e.activation(out=gtok[:, i0:i0 + nn, :],
                                                           in_=pt[:, 0:nn * 64].rearrange("p (a c) -> p a c", c=64), func=AF.Copy),
                             r=[pt], w=[gtok])
                    S.op("dve", lambda e: e.tensor_scalar(out=maskb[:], in0=gtok[:], scalar1=0.0, scalar2=None, op0=ALU.is_gt),
                         r=[gtok], w=[maskb])
                    for i in range(NTT):
                        prev = list(range(0, i)) if i < NTL else list(range(NTL, i))
                        ps = PS_MM.next()
                        for ii, ip in enumerate(prev):
                            S.op("pe", lambda e, ip=ip, ii=ii: e.matmul(ps[:, 0:64], lhsT=onesb[:], rhs=maskb[:, ip, :],
                                                                        start=(ii == 0), stop=False), r=[onesb, maskb], w=[ps])
                        S.op("pe", lambda e: e.matmul(ps[:, 0:64], lhsT=masks[:, 0, :], rhs=maskb[:, i, :],
                                                      start=(len(prev) == 0), stop=True), r=[masks, maskb], w=[ps])
                        S.op("dve", lambda e: e.tensor_tensor(out=slotTok[:, i, :], in0=ps[:, 0:64], in1=maskb[:, i, :], op=ALU.mult),
                             r=[ps, maskb], w=[slotTok])
                    for i0 in range(0, NTT, 4):
                        pt = PS_TR.next()
                        nn = min(4, NTT - i0)
                        for q in range(nn):
                            S.op("pe", lambda e, q=q: e.transpose(pt[0:64, q * 128:(q + 1) * 128], slotTok[:, i0 + q, :], identf[:]),
                                 r=[slotTok, identf], w=[pt])
                        S.op("act", lambda e: e.activation(out=work[:, i0 * 128:(i0 + nn) * 128], in_=pt[0:64, 0:nn * 128], func=AF.Copy),
                             r=[pt], w=[work])
                    S.dma("sp", SLOTG[:, 0, :], work[:], r=[work])
                    if debug and l == 0:
                        S.dma("sp", dbg["slotg"][:, 0, :], work[:], r=[work])
                        S.dma("sp", dbg["slotg"][:, 1, :], Gx[:], r=[Gx])
                    chk("m1_topk")
                    for b in range(NS):
                        for i in range(NTT):
                            S.dma("sp", xn2[:, i, :], XN2[b, i], w=[xn2], add=(i > 0))
                        for ex_ in range(NE):
                            col = b * 16 + ex_
                            Ps = []
                            for i in range(NTL):
                                P = PB.next()
                                S.op("dve", lambda e: e.tensor_scalar(
                                    out=P[:], in0=iota[:, 0:256], scalar1=slotTok[:, i, col:col + 1], scalar2=None, op0=ALU.is_equal),
                                    r=[iota, slotTok], w=[P])
                                Ps.append(P)
                            stg = XSTG.next()
                            for cg in range(4):
                                pss = [PS_MM.next() for _ in range(2)]
                                for i in range(NTL):
                                    for cc in range(2):
                                        c = cg * 2 + cc
                                        S.op("pe", lambda e, c=c, cc=cc: e.matmul(pss[cc][:, 0:256],
                                                                                  lhsT=xn2[:, i, c * 128:(c + 1) * 128], rhs=Ps[i][:],
                                                                                  start=(i == 0), stop=(i == NTL - 1)),
                                             r=[xn2, Ps[i]], w=[pss[cc]])
                                for cc in range(2):
                                    c = cg * 2 + cc
                                    src = pss[cc][:, 0:256]
                                    if c % 2 == 0:
                                        S.op("act", lambda e, c=c, src=src: e.activation(
                                            out=stg[:, c, :], in_=src, func=AF.Identity, scale=A2[:, c, b:b + 1],
                                            bias=modsT[:, l, 24 + c, b:b + 1]), r=[pss[cc], A2, modsT], w=[stg])
                                    else:
                                        S.op("dve", lambda e, c=c, src=src: e.tensor_scalar(
                                            out=stg[:, c, :], in0=src, scalar1=A2[:, c, b:b + 1], scalar2=modsT[:, l, 24 + c, b:b + 1],
                                            op0=ALU.mult, op1=ALU.add), r=[pss[cc], A2, modsT], w=[stg])
                            for c in range(8):
                                S.dma("sp", XSG[ex_, c][:, b * CAP:(b + 1) * CAP], stg[:, c, :], r=[stg])
                            psc = PS_AC.next()
                            Pcs = []
                            for i in range(NTL, NTT):
                                Pc = PBC.next()
                                S.op("dve", lambda e: e.tensor_scalar(out=Pc[:], in0=iota[:, 0:32], scalar1=slotTok[:, i, col:col + 1],
                                                                       scalar2=None, op0=ALU.is_equal), r=[iota, slotTok], w=[Pc])
                                Pcs.append(Pc)
                            for c in range(8):
                                for ii, i in enumerate(range(NTL, NTT)):
                                    S.op("pe", lambda e, c=c, i=i, ii=ii: e.matmul(psc[:, c * 32:(c + 1) * 32],
                                                                                   lhsT=xn2[:, i, c * 128:(c + 1) * 128], rhs=Pcs[ii][:],
                                                                                   start=(i == NTL), stop=(i == NTT - 1)),
                                         r=[xn2, Pcs[ii]], w=[psc])
                            stc = XSTC.next()
                            for c in range(8):
                                S.op("dve", lambda e, c=c: e.tensor_scalar(
                                    out=stc[:, c, :], in0=psc[:, c * 32:(c + 1) * 32], scalar1=A2[:, c, 4:5],
                                    scalar2=modsT[:, l, 24 + c, 4:5], op0=ALU.mult, op1=ALU.add), r=[psc, A2, modsT], w=[stc])
                            o0 = NS * CAP + b * CAPC
                            S.dma("sp", XSG[ex_][:, :, o0:o0 + CAPC].rearrange("c p s -> p c s"), stc[:], r=[stc])
                    S.barrier()

                chk("m1")
                with ExitStack() as m2:
                    WG = [sb("wg%d" % i, [128, 8, D], BF16, m2) for i in range(2)]
                    WU = [sb("wu%d" % i, [128, 8, D], BF16, m2) for i in range(2)]
                    WD = [sb("wd%d" % i, [128, 8, D], BF16, m2) for i in range(2)]
                    XS = [sb("xs%d" % i, [128, 8, NSL], BF16, m2) for i in range(2)]
                    actT = sb("actT", [128, 8, NSL], BF16, m2)
                    SA = Pool([sb("sa%d" % i, [128, 512], F32, m2) for i in range(3)])
                    YS = Pool([sb("ys%d" % i, [128, D], BF16, m2) for i in range(2)])
                    SCH = [(s0, min(512, NSL - s0)) for s0 in range(0, NSL, 512)]
                    STL = [(s0, min(128, NSL - s0)) for s0 in range(0, NSL, 128)]
                    for ex_ in range(NE):
                        wg, wu, wd, xs = WG[ex_ % 2], WU[ex_ % 2], WD[ex_ % 2], XS[ex_ % 2]
                        dma_k("pool", wg, w_gate[l, ex_])
                        dma_k("pool", wu, w_up[l, ex_])
                        dma_k("pool", wd, w_down[l, ex_])
                        for c in range(8):
                            S.dma("sp", xs[:, c, :], XSG[ex_, c], w=[xs], add=(c > 0))
                        for (s0, n) in SCH:
                            for fc in range(8):
                                pA, pU = PS_MM.next(), PS_MM.next()
                                for k in range(8):
                                    S.op("pe", lambda e, k=k: e.matmul(pA[:, 0:n], lhsT=wg[:, k, fc * 128:(fc + 1) * 128],
                                                                      rhs=xs[:, k, s0:s0 + n], start=(k == 0), stop=(k == 7)),
                                         r=[wg, xs], w=[pA])
                                for k in range(8):
                                    S.op("pe", lambda e, k=k: e.matmul(pU[:, 0:n], lhsT=wu[:, k, fc * 128:(fc + 1) * 128],
                                                                      rhs=xs[:, k, s0:s0 + n], start=(k == 0), stop=(k == 7)),
                                         r=[wu, xs], w=[pU])
                                sa = SA.next()
                                S.op("act", lambda e: e.activation(out=sa[:, 0:n], in_=pA[:, 0:n], func=AF.Silu), r=[pA], w=[sa])
                                S.op("dve", lambda e: e.tensor_tensor(out=actT[:, fc, s0:s0 + n], in0=sa[:, 0:n], in1=pU[:, 0:n],
                                                                      op=ALU.mult), r=[sa, pU], w=[actT])
                        for (s0, rows) in STL:
                            ys = YS.next()
                            for half in range(2):
                                ps = PS_MM.next()
                                for fc in range(8):
                                    S.op("pe", lambda e, fc=fc: e.matmul(ps[0:rows, :], lhsT=actT[:, fc, s0:s0 + rows],
                                                                        rhs=wd[:, fc, half * 512:(half + 1) * 512],
                                                                        start=(fc == 0), stop=(fc == 7)), r=[actT, wd], w=[ps])
                                if half == 0:
                                    S.op("act", lambda e: e.activation(out=ys[0:rows, 0:512], in_=ps[0:rows, :], func=AF.Copy),
                                         r=[ps], w=[ys])
                                else:
                                    S.op("dve", lambda e: e.tensor_copy(out=ys[0:rows, 512:1024], in_=ps[0:rows, :]), r=[ps], w=[ys])
                            S.dma("sp", YEX[ex_][s0:s0 + rows, :], ys[0:rows, :], r=[ys])
                    S.barrier()

                chk("m2")
                with ExitStack() as m3:
                    selb = sb("selb", [16, 16 * 128], BF16, m3)
                    slot = sb("slot", [16, NTOK], BF16, m3)
                    gg = sb("gg", [16, NTOK], BF16, m3)
                    Yl = sb("Yl", [128, NE, 2, D], BF16, m3)
                    Yc = sb("Yc", [32, NE, D], BF16, m3)
                    PT = sb("PT", [128, NE, 2, 512], BF16, m3)
                    PTc = sb("PTc", [32, NE, 256], BF16, m3)
                    GB = Pool([sb("gb%d" % i, [128, 512], F32, m3) for i in range(3)])
                    g2b = sb("g2b", [128, D], F32, m3)
                    XT3 = Pool([sb("xt3_%d" % i, [128, D], F32, m3) for i in range(2)])
                    OT3 = Pool([sb("ot3_%d" % i, [128, D], F32, m3) for i in range(1)])
                    ST3 = Pool([sb("st3_%d" % i, [128, 8], F32, m3) for i in range(4)])
                    fgb = sb("fgb", [128, D], F32, m3)
                    S.dma("sp", fgb[:], final_g[0:1, :].to_broadcast([128, D]), w=[fgb])
                    S.dma("pool", selb[:], c_sel[:, :], w=[selb])
                    last = (l == DEPTH - 1)
                    for b in range(NS):
                        S.dma("pool", slot[:], SLOTG[b * 16:(b + 1) * 16, 0, :], w=[slot])
                        S.dma("pool", gg[:], SLOTG[b * 16:(b + 1) * 16, 1, :], w=[gg])
                        for ex_ in range(NE):
                            S.dma("sp", Yl[:, ex_], YEX[ex_][b * CAP:(b + 1) * CAP, :].rearrange("(k p) d -> p k d", p=128), w=[Yl],
                                  add=(ex_ > 0))
                        o0 = NS * CAP + b * CAPC
                        for ex_ in range(NE):
                            S.dma("sp", Yc[:, ex_, :], YEX[ex_][o0:o0 + CAPC, :], w=[Yc], add=(ex_ > 0))
                        S.dma("sp", g2b[:], MODS[l][b:b + 1, 5120:6144].to_broadcast([128, D]), w=[g2b])
                        for (t0, n) in TCH:
                            latent = t0 < T
                            if last and not latent:
                                continue
                            R = 128 if latent else 32
                            if not latent:
                                S.dma("sp", g2b[:], MODS[l][4:5, 5120:6144].to_broadcast([128, D]), w=[g2b])
                            for ex_ in range(NE):
                                psS, psG = PS_MM.next(), PS_MM.next()
                                S.op("pe", lambda e: e.matmul(psS[0:R, 0:n], lhsT=selb[:, ex_ * 128:ex_ * 128 + R], rhs=slot[:, t0:t0 + n],
                                                              start=True, stop=True), r=[selb, slot], w=[psS])
                                S.op("pe", lambda e: e.matmul(psG[0:R, 0:n], lhsT=selb[:, ex_ * 128:ex_ * 128 + R], rhs=gg[:, t0:t0 + n],
                                                              start=True, stop=True), r=[selb, gg], w=[psG])
                                gb = GB.next()
                                S.op("act", lambda e: e.activation(out=gb[0:R, 0:n], in_=psG[0:R, 0:n], func=AF.Copy), r=[psG], w=[gb])
                                if latent:
                                    for k in range(2):
                                        S.op("dve", lambda e, k=k: e.scalar_tensor_tensor(
                                            out=PT[:, ex_, k, 0:n], in0=psS[:, 0:n], scalar=iota[:, 256 + k:257 + k], in1=gb[:, 0:n],
                                            op0=ALU.is_equal, op1=ALU.mult), r=[psS, iota, gb], w=[PT])
                                else:
                                    S.op("dve", lambda e: e.scalar_tensor_tensor(
                                        out=PTc[:, ex_, 0:n], in0=psS[0:32, 0:n], scalar=iota[0:32, 256:257], in1=gb[0:32, 0:n],
                                        op0=ALU.is_equal, op1=ALU.mult), r=[psS, iota, gb], w=[PTc])
                            for i in range(t0 // 128, (t0 + n) // 128):
                                tt = i * 128 - t0
                                xtile = XT3.next()
                                S.dma("sp", xtile[:], XMID[b, i], w=[xtile])
                                gt = g2b
                                for half in range(2):
                                    ps = PS_AC.next()
                                    if latent:
                                        for ex_ in range(NE):
                                            for k in range(2):
                                                S.op("pe", lambda e, ex_=ex_, k=k: e.matmul(
                                                    ps[:, :], lhsT=PT[:, ex_, k, tt:tt + 128], rhs=Yl[:, ex_, k, half * 512:(half + 1) * 512],
                                                    start=(ex_ == 0 and k == 0), stop=(ex_ == NE - 1 and k == 1)), r=[PT, Yl], w=[ps])
                                    else:
                                        for ex_ in range(NE):
                                            S.op("pe", lambda e, ex_=ex_: e.matmul(
                                                ps[:, :], lhsT=PTc[:, ex_, tt:tt + 128], rhs=Yc[:, ex_, half * 512:(half + 1) * 512],
                                                start=(ex_ == 0), stop=(ex_ == NE - 1)), r=[PTc, Yc], w=[ps])
                                    tm = GB.next()
                                    S.op("dve", lambda e: e.tensor_tensor(out=tm[:], in0=ps[:, :], in1=gt[:, half * 512:(half + 1) * 512],
                                                                          op=ALU.mult), r=[ps, gt], w=[tm])
                                    S.op("dve", lambda e: e.tensor_tensor(out=xtile[:, half * 512:(half + 1) * 512],
                                                                           in0=xtile[:, half * 512:(half + 1) * 512], in1=tm[:],
                                                                           op=ALU.add), r=[xtile, tm], w=[xtile])
                                if debug and b == 0 and l == 0:
                                    S.dma("sp", dbg["xres"][i], xtile[:], r=[xtile])
                                if not last:
                                    S.dma("sp", XRES[b, i], xtile[:], r=[xtile])
                                else:
                                    ot = OT3.next()
                                    ss = ST3.next()
                                    S.op("pool", lambda e: e.memset(ss[:], 0.0), w=[ss])
                                    S.op("act", lambda e: e.activation(out=ot[:], in_=xtile[:], func=AF.Square, accum_out=ss[:, 0:1]),
                                         r=[xtile], w=[ot, ss])
                                    S.op("dve", lambda e: e.tensor_scalar(out=ss[:, 1:2], in0=ss[:, 0:1], scalar1=1.0 / D, scalar2=EPS,
                                                                          op0=ALU.mult, op1=ALU.add), r=[ss], w=[ss])
                                    S.op("dve", lambda e: e.reciprocal(out=ss[:, 2:3], in_=ss[:, 1:2]), r=[ss], w=[ss])
                                    S.op("act", lambda e: e.activation(out=ss[:, 3:4], in_=ss[:, 2:3], func=AF.Sqrt), r=[ss], w=[ss])
                                    S.op("dve", lambda e: e.scalar_tensor_tensor(out=ot[:], in0=xtile[:], scalar=ss[:, 3:4], in1=fgb[:],
                                                                                 op0=ALU.mult, op1=ALU.mult), r=[xtile, ss, fgb], w=[ot])
                                    S.dma("sp", out[b, i * 128:(i + 1) * 128, :], ot[:], r=[ot])
                    S.barrier()
        except _Stop:
            S.barrier()
        S.barrier(engines=["sp"])
        print("instr counts", S.cnt, "wait instrs", S.n_wait_ins, "ndma", sum(S.dma_val) // 16, flush=True)
    return nc


def _consts():
    identb = np.eye(128, dtype=np.float32).astype(ml_dtypes.bfloat16)
    identf = np.eye(128, dtype=np.float32)
    j = np.arange(128)[:, None]
    q = np.arange(128)[None, :]
    masks = np.stack([(j <= q), (j >= q)], axis=1).astype(np.float32).astype(ml_dtypes.bfloat16)
    iota = np.zeros((128, 258), np.float32)
    iota[:, 0:256] = np.arange(1, 257, dtype=np.float32)[None, :]
    iota[:, 256] = np.arange(1, 129)
    iota[:, 257] = np.arange(129, 257)
    pos = np.arange(T)
    row = (pos // 64).astype(np.float32)
    colp = (pos % 64).astype(np.float32)
    inv = (10000.0 ** (-np.arange(16, dtype=np.float32) / 16)).astype(np.float32)
    ar = row[:, None] * inv
    ac = colp[:, None] * inv
    ang = np.concatenate([ar, ar, ac, ac], axis=-1).astype(np.float32)
    cos = np.cos(ang).astype(np.float32)
    sin = np.sin(ang).astype(np.float32)
    sgn = np.tile(np.concatenate([-np.ones(16), np.ones(16)]), 2).astype(np.float32)
    ss = sin * sgn[None, :]
    cos6 = np.tile(cos, (1, 6))
    ss6 = np.tile(ss, (1, 6))
    rope = np.stack([cos6, ss6], axis=1).reshape(NTL, 128, 2, 384).astype(np.float32)
    sel = np.zeros((16, 16, 128), np.float32)
    for e in range(16):
        sel[e, e, :] = 1.0
    return dict(c_identb=identb, c_identf=identf, c_masks=masks, c_iota=iota, c_rope=rope,
                c_sel=sel.reshape(16, 16 * 128))


def _prep_shared(inp, DEPTH):
    f = lambda a: np.ascontiguousarray(np.asarray(a, dtype=np.float32))
    sh = {}
    sh["ada_w"] = f(inp["ada_w"][:DEPTH])
    sh["ada_b"] = f(inp["ada_b"][:DEPTH])
    rep = lambda g: f(np.repeat(np.asarray(g)[:DEPTH].reshape(DEPTH, 8, 128).transpose(0, 2, 1)[..., None], 5, axis=-1))
    sh["n1rep"] = rep(inp["norm1_g"])
    sh["n2rep"] = rep(inp["norm2_g"])
    sh["w_in"] = f(inp["w_in"][:DEPTH])
    cp = np.zeros((DEPTH, 128, 2, 40), np.float32)
    ca = np.asarray(inp["conv_a_w"])[:DEPTH]
    cb = np.asarray(inp["conv_b_w"])[:DEPTH]
    for h in range(2):
        cp[:, :, h, 0:3] = ca[:, :, h * 128:(h + 1) * 128].transpose(0, 2, 1)
        cp[:, :, h, 3:34] = cb[:, :, h * 128:(h + 1) * 128].transpose(0, 2, 1)
        cp[:, :, h, 34] = np.asarray(inp["conv_b_b"])[:DEPTH, h * 128:(h + 1) * 128]
        cp[:, :, h, 35] = np.asarray(inp["conv_ln_g"])[:DEPTH, h * 128:(h + 1) * 128]
        cp[:, :, h, 36] = np.asarray(inp["conv_ln_b"])[:DEPTH, h * 128:(h + 1) * 128]
    sh["convp"] = cp
    sh["sink"] = f(np.asarray(inp["sink"])[:DEPTH].reshape(1, DEPTH * 4))
    sh["qng"] = f(inp["q_norm_g"][:DEPTH])
    sh["kng"] = f(inp["k_norm_g"][:DEPTH])
    sh["w_out"] = f(inp["w_out"][:DEPTH])
    sh["router"] = f(inp["router_w"][:DEPTH])
    sh["w_gate"] = f(inp["w_gate"][:DEPTH])
    sh["w_up"] = f(inp["w_up"][:DEPTH])
    sh["w_down"] = f(inp["w_down"][:DEPTH])
    sh["final_g"] = f(np.asarray(inp["final_g"]).reshape(1, D))
    sh.update(_consts())
    return sh


def _core_inputs(inp, sh, b0, NS):
    m = dict(sh)
    m["x"] = np.ascontiguousarray(np.asarray(inp["x"][b0:b0 + NS], dtype=np.float32))
    m["ctx"] = np.ascontiguousarray(np.asarray(inp["ctx"][b0:b0 + NS], dtype=np.float32))
    call = np.zeros((5, D), np.float32)
    call[0:NS] = np.asarray(inp["c"][b0:b0 + NS])
    call[4] = np.asarray(inp["c_ctx"])
    m["cT"] = np.ascontiguousarray(call.reshape(5, 8, 128).transpose(2, 1, 0))
    return m


_NC_CACHE = {}


def kernel(**inputs):
    NS, DEPTH = 4, 4
    key = (NS, DEPTH)
    if key not in _NC_CACHE:
        _NC_CACHE[key] = build_program(NS, DEPTH)
    nc = _NC_CACHE[key]
    sh = _prep_shared(inputs, DEPTH)
    in_maps = [_core_inputs(inputs, sh, c * NS, NS) for c in range(N_CORES)]
    res = run_bass_kernel_spmd(nc, in_maps, core_ids=list(range(N_CORES)))
    outs = [np.asarray(r["out"]) for r in res.results]
    return np.concatenate(outs, axis=0).astype(np.float32)
```
